# Optimizing a Trainium2 kernel written in Bass

```python
import math
import jax
import jax.numpy as jnp
from jax import lax
import numpy as np

D_MODEL = 1024
BATCH = 16
SEQ = 4096
DEPTH = 2

HEAD_DIM = 64
SSD_WIDTH = D_MODEL // 2
SSD_HEADS = SSD_WIDTH // HEAD_DIM
SSD_GROUPS = 2
SSD_STATE = 128
SSD_CONV = 4
SSD_CHUNK = 128
SB_WIDTH = D_MODEL // 4
SB_HEADS = SB_WIDTH // HEAD_DIM
SB_BLOCK = 128
MOBA_WIDTH = D_MODEL // 4
MOBA_HEADS = MOBA_WIDTH // HEAD_DIM
MOBA_BLOCK = 256
MOBA_TOPK = 3
MOBA_Q_CHUNK = 32

MIX_WIDTH = SSD_WIDTH + SB_WIDTH + MOBA_WIDTH
CONV_CH = SSD_WIDTH + 2 * SSD_GROUPS * SSD_STATE
IN_COLS = SSD_WIDTH + CONV_CH + SSD_HEADS + 3 * SB_WIDTH + 3 * MOBA_WIDTH
FFN_HIDDEN = ((8 * D_MODEL + 3 * 256 - 1) // (3 * 256)) * 256
EPS = 1e-6

kernel_name = 'hybrid_ssd_stickbreak_moba_block'


def _rms(x):
    xf = x.astype(jnp.float32)
    return xf * lax.rsqrt(jnp.mean(xf * xf, axis=-1, keepdims=True) + EPS)


def rms_norm(x, w):
    return (_rms(x) * w.astype(jnp.float32)).astype(x.dtype)


def alibi_slopes(n):
    return jnp.asarray(2.0 ** (-8.0 * np.arange(1, n + 1) / n), dtype=jnp.float32)


def _split_heads(t, n_heads):
    b, s, _ = t.shape
    return jnp.transpose(t.reshape(b, s, n_heads, HEAD_DIM), (0, 2, 1, 3)).astype(jnp.float32)


def _merge_heads(t):
    b, h, s, d = t.shape
    return jnp.transpose(t, (0, 2, 1, 3)).reshape(b, s, h * d)


def causal_depthwise_conv(u, w, b):
    k = w.shape[0]
    out = lax.conv_general_dilated(u, w[:, None, :].astype(u.dtype), window_strides=(1,),
                                   padding=[(k - 1, 0)], dimension_numbers=('NWC', 'WIO', 'NWC'),
                                   feature_group_count=u.shape[-1])
    return out + b.astype(u.dtype)


def ssd_chunked(xs, dt, a, bm, cm):
    bsz, s, h, p = xs.shape
    g, n = bm.shape[2], bm.shape[3]
    k = h // g
    q = SSD_CHUNK
    c = s // q
    xdt = (xs * dt[..., None]).reshape(bsz, c, q, g, k, p)
    adt = jnp.moveaxis((dt * a).reshape(bsz, c, q, g, k), 2, -1)
    acum = jnp.cumsum(adt, axis=-1)
    causal = jnp.tril(jnp.ones((q, q), dtype=bool))
    seg = acum[..., :, None] - acum[..., None, :]
    lmat = jnp.exp(jnp.where(causal, seg, -jnp.inf))
    bc = bm.reshape(bsz, c, q, g, n)
    cc = cm.reshape(bsz, c, q, g, n)
    cb = jnp.einsum('bclgn,bcsgn->bcgls', cc, bc)
    y_diag = jnp.einsum('bcgls,bcgkls,bcsgkp->bclgkp', cb, lmat, xdt)
    decay_to_end = jnp.exp(acum[..., -1:] - acum)
    chunk_states = jnp.einsum('bclgn,bcgkl,bclgkp->bcgkpn', bc, decay_to_end, xdt)
    chunk_decay = jnp.exp(acum[..., -1])

    def step(state, inp):
        dec, cs = inp
        return state * dec[..., None, None] + cs, state

    init = jnp.zeros((bsz, g, k, p, n), jnp.float32)
    _, states_in = lax.scan(step, init, (jnp.moveaxis(chunk_decay, 1, 0), jnp.moveaxis(chunk_states, 1, 0)))
    states_in = jnp.moveaxis(states_in, 0, 1)
    y_off = jnp.einsum('bclgn,bcgkpn,bcgkl->bclgkp', cc, states_in, jnp.exp(acum))
    return (y_diag + y_off).reshape(bsz, s, h, p)


def ssd_mixer(z, xbc, dt_raw, conv_w, conv_b, dt_bias, a_log, d_skip, norm_w):
    bsz, s, _ = z.shape
    xbc = jax.nn.silu(causal_depthwise_conv(xbc, conv_w, conv_b)).astype(jnp.float32)
    gn = SSD_GROUPS * SSD_STATE
    xs = xbc[..., :SSD_WIDTH].reshape(bsz, s, SSD_HEADS, HEAD_DIM)
    bm = xbc[..., SSD_WIDTH:SSD_WIDTH + gn].reshape(bsz, s, SSD_GROUPS, SSD_STATE)
    cm = xbc[..., SSD_WIDTH + gn:].reshape(bsz, s, SSD_GROUPS, SSD_STATE)
    dt = jax.nn.softplus(dt_raw.astype(jnp.float32) + dt_bias.astype(jnp.float32))
    a = -jnp.exp(a_log.astype(jnp.float32))
    y = ssd_chunked(xs, dt, a, bm, cm)
    y = y + d_skip.astype(jnp.float32)[:, None] * xs
    y = y.reshape(bsz, s, SSD_WIDTH) * jax.nn.silu(z.astype(jnp.float32))
    y = _rms(y.reshape(bsz, s, SSD_GROUPS, SSD_WIDTH // SSD_GROUPS)).reshape(bsz, s, SSD_WIDTH)
    return (y * norm_w.astype(jnp.float32)).astype(z.dtype)


def stick_breaking_attention(q, k, v):
    bsz, h, s, d = q.shape
    scale = 1.0 / math.sqrt(d)
    outs = []
    for i in range(s // SB_BLOCK):
        start = i * SB_BLOCK
        end = start + SB_BLOCK
        z = jnp.einsum('bhqd,bhkd->bhqk', q[:, :, start:end], k[:, :, :end]) * scale
        tpos = start + jnp.arange(SB_BLOCK)
        spos = jnp.arange(end)
        strict = spos[None, :] < tpos[:, None]
        log_keep = jnp.where(strict, jax.nn.log_sigmoid(-z), 0.0)
        after = lax.cumsum(log_keep, axis=3, reverse=True) - log_keep
        w = jnp.where(strict, jnp.exp(jax.nn.log_sigmoid(z) + after), 0.0)
        outs.append(jnp.einsum('bhqk,bhkd->bhqd', w, v[:, :, :end]))
    return jnp.concatenate(outs, axis=2)


def moba_attention(q, k, v, slopes):
    bsz, h, s, d = q.shape
    blk = MOBA_BLOCK
    nb = -(-s // blk)
    sp = nb * blk
    pad = ((0, 0), (0, 0), (0, sp - s), (0, 0))
    q = jnp.pad(q, pad)
    k = jnp.pad(k, pad)
    v = jnp.pad(v, pad)
    k_blk = k.reshape(bsz, h, nb, blk, d)
    v_blk = v.reshape(bsz, h, nb, blk, d)
    k_mean = jnp.mean(k_blk, axis=3)
    n_sel = min(MOBA_TOPK, nb - 1)
    scale = 1.0 / math.sqrt(d)
    bi = jnp.arange(bsz)[:, None, None, None]
    hi = jnp.arange(h)[None, :, None, None]

    def chunk(ci):
        start = ci * MOBA_Q_CHUNK
        own = start // blk
        qc = lax.dynamic_slice_in_dim(q, start, MOBA_Q_CHUNK, axis=2)
        tpos = start + jnp.arange(MOBA_Q_CHUNK)
        ko = lax.dynamic_slice_in_dim(k, own * blk, blk, axis=2)
        vo = lax.dynamic_slice_in_dim(v, own * blk, blk, axis=2)
        kpos_o = own * blk + jnp.arange(blk)
        dist_o = (tpos[:, None] - kpos_o[None, :]).astype(jnp.float32)
        logit_o = jnp.einsum('bhqd,bhld->bhql', qc, ko) * scale - slopes[None, :, None, None] * dist_o
        logit_o = jnp.where(dist_o >= 0, logit_o, -jnp.inf)
        if n_sel == 0:
            p = jax.nn.softmax(logit_o, axis=-1)
            return jnp.einsum('bhql,bhld->bhqd', p, vo)
        gate = jnp.einsum('bhqd,bhnd->bhqn', qc, k_mean)
        gate = jnp.where(jnp.arange(nb) < own, gate, -jnp.inf)
        _, idx = lax.top_k(gate, n_sel)
        valid = jnp.arange(n_sel) < own
        kg = k_blk[bi, hi, idx]
        vg = v_blk[bi, hi, idx]
        kpos_g = idx[..., None] * blk + jnp.arange(blk)
        dist_g = (tpos[None, None, :, None, None] - kpos_g).astype(jnp.float32)
        logit_g = jnp.einsum('bhqd,bhqjld->bhqjl', qc, kg) * scale - slopes[None, :, None, None, None] * dist_g
        logit_g = jnp.where(valid[:, None], logit_g, -jnp.inf)
        logits = jnp.concatenate([logit_g.reshape(bsz, h, MOBA_Q_CHUNK, n_sel * blk), logit_o], axis=-1)
        p = jax.nn.softmax(logits, axis=-1)
        pg = p[..., :n_sel * blk].reshape(bsz, h, MOBA_Q_CHUNK, n_sel, blk)
        po = p[..., n_sel * blk:]
        return jnp.einsum('bhqjl,bhqjld->bhqd', pg, vg) + jnp.einsum('bhql,bhld->bhqd', po, vo)

    out = lax.map(chunk, jnp.arange(sp // MOBA_Q_CHUNK))
    out = jnp.transpose(out, (1, 2, 0, 3, 4)).reshape(bsz, h, sp, d)
    return out[:, :, :s]


def hybrid_layer(x, pre_mix_norm, w_in, conv_w, conv_b, dt_bias, a_log, d_skip, ssd_norm, sb_norm,
                 moba_norm, w_out, post_mix_norm, pre_ffn_norm, w_gate, w_up, w_down, post_ffn_norm):
    h = rms_norm(x, pre_mix_norm)
    proj = h @ w_in
    o1 = SSD_WIDTH
    o2 = o1 + CONV_CH
    o3 = o2 + SSD_HEADS
    o4 = o3 + 3 * SB_WIDTH
    z, xbc, dt_raw, sb_qkv, moba_qkv = jnp.split(proj, [o1, o2, o3, o4], axis=-1)
    y_ssd = ssd_mixer(z, xbc, dt_raw, conv_w, conv_b, dt_bias, a_log, d_skip, ssd_norm)
    sq, sk, sv = jnp.split(sb_qkv, 3, axis=-1)
    y_sb = _merge_heads(stick_breaking_attention(_split_heads(sq, SB_HEADS), _split_heads(sk, SB_HEADS),
                                                 _split_heads(sv, SB_HEADS)))
    y_sb = (_rms(y_sb) * sb_norm.astype(jnp.float32)).astype(x.dtype)
    mq, mk, mv = jnp.split(moba_qkv, 3, axis=-1)
    y_moba = _merge_heads(moba_attention(_split_heads(mq, MOBA_HEADS), _split_heads(mk, MOBA_HEADS),
                                         _split_heads(mv, MOBA_HEADS), alibi_slopes(MOBA_HEADS)))
    y_moba = (_rms(y_moba) * moba_norm.astype(jnp.float32)).astype(x.dtype)
    mix = jnp.concatenate([y_ssd, y_sb, y_moba], axis=-1) @ w_out
    x = x + rms_norm(mix, post_mix_norm)
    h2 = rms_norm(x, pre_ffn_norm)
    f = (jax.nn.silu(h2 @ w_gate) * (h2 @ w_up)) @ w_down
    return x + rms_norm(f, post_ffn_norm)


def setup_inputs(seed: int = 0) -> dict:
    key = jax.random.key(seed)
    ks = jax.random.split(key, 20)
    f32 = jnp.float32

    def nrm(k, shape, scale):
        return jax.random.normal(k, shape, f32) * scale

    def gain(k, shape):
        return 1.0 + 0.02 * jax.random.normal(k, shape, f32)

    dt0 = jnp.exp(jax.random.uniform(ks[5], (DEPTH, SSD_HEADS), f32, math.log(1e-3), math.log(1e-1)))
    return {
        'x': jax.random.normal(ks[0], (BATCH, SEQ, D_MODEL), f32),
        'pre_mix_norm': gain(ks[1], (DEPTH, D_MODEL)),
        'w_in': nrm(ks[2], (DEPTH, D_MODEL, IN_COLS), D_MODEL ** -0.5),
        'conv_w': nrm(ks[3], (DEPTH, SSD_CONV, CONV_CH), SSD_CONV ** -0.5),
        'conv_b': nrm(ks[4], (DEPTH, CONV_CH), 0.01),
        'dt_bias': dt0 + jnp.log(-jnp.expm1(-dt0)),
        'a_log': jnp.log(jax.random.uniform(ks[6], (DEPTH, SSD_HEADS), f32, 1.0, 16.0)),
        'd_skip': 1.0 + 0.1 * jax.random.normal(ks[7], (DEPTH, SSD_HEADS), f32),
        'ssd_norm': gain(ks[8], (DEPTH, SSD_WIDTH)),
        'sb_norm': gain(ks[9], (DEPTH, SB_WIDTH)),
        'moba_norm': gain(ks[10], (DEPTH, MOBA_WIDTH)),
        'w_out': nrm(ks[11], (DEPTH, MIX_WIDTH, D_MODEL), MIX_WIDTH ** -0.5),
        'post_mix_norm': gain(ks[12], (DEPTH, D_MODEL)),
        'pre_ffn_norm': gain(ks[13], (DEPTH, D_MODEL)),
        'w_gate': nrm(ks[14], (DEPTH, D_MODEL, FFN_HIDDEN), D_MODEL ** -0.5),
        'w_up': nrm(ks[15], (DEPTH, D_MODEL, FFN_HIDDEN), D_MODEL ** -0.5),
        'w_down': nrm(ks[16], (DEPTH, FFN_HIDDEN, D_MODEL), FFN_HIDDEN ** -0.5),
        'post_ffn_norm': gain(ks[17], (DEPTH, D_MODEL)),
    }


def reference(x, pre_mix_norm, w_in, conv_w, conv_b, dt_bias, a_log, d_skip, ssd_norm, sb_norm, moba_norm,
              w_out, post_mix_norm, pre_ffn_norm, w_gate, w_up, w_down, post_ffn_norm):
    for l in range(DEPTH):
        x = hybrid_layer(x, pre_mix_norm[l], w_in[l], conv_w[l], conv_b[l], dt_bias[l], a_log[l], d_skip[l],
                         ssd_norm[l], sb_norm[l], moba_norm[l], w_out[l], post_mix_norm[l], pre_ffn_norm[l],
                         w_gate[l], w_up[l], w_down[l], post_ffn_norm[l])
    return x
```

```python
import contextlib
import numpy as np
import ml_dtypes
import concourse.bass as bass
import concourse.mybir as mybir
from concourse.bass_utils import run_bass_kernel_spmd

F32 = mybir.dt.float32
BF16 = mybir.dt.bfloat16
AF = mybir.ActivationFunctionType
ALU = mybir.AluOpType
AX = mybir.AxisListType

D = 1024
INC = 3080
FH = 2816
NHC = 22
EPS = 1e-6
BIG = 30000.0
ENGS = ["pe", "act", "dve", "pool", "sp"]
EPOCH = 12000
NDMA = 12


class Prog:
    def __init__(self, nc):
        self.nc = nc
        self.ins = {e: [] for e in ENGS}
        self.lastw = {}
        self.readers = {}
        self.dma_slot = {e: 0 for e in ENGS}
        self.dma_prev = {}

    def add(self, eng, fn, reads=(), writes=(), dma=False):
        lst = self.ins[eng]
        me = dict(eng=eng, idx=len(lst), fn=fn, deps=set(), dma=dma, sig=False)
        deps = me["deps"]
        for r in reads:
            w = self.lastw.get(r)
            if w is not None:
                deps.add((w["eng"], w["idx"]))
        for wr in writes:
            w = self.lastw.get(wr)
            if w is not None and (w["eng"] != eng or w["dma"] or dma or eng != "pe"):
                deps.add((w["eng"], w["idx"]))
            for rd in self.readers.get(wr, ()):
                if rd["eng"] != eng or rd["dma"] or dma or eng != "pe":
                    deps.add((rd["eng"], rd["idx"]))
        if dma:
            slot = self.dma_slot[eng]
            self.dma_slot[eng] = (slot + 1) % NDMA
            me["slot"] = slot
            prev = self.dma_prev.get((eng, slot))
            if prev is not None:
                deps.add((prev["eng"], prev["idx"]))
            self.dma_prev[(eng, slot)] = me
        deps.discard((eng, me["idx"]))
        for r in reads:
            self.readers.setdefault(r, []).append(me)
        for wr in writes:
            self.lastw[wr] = me
            self.readers[wr] = []
        lst.append(me)
        return me

    def emit(self, final_waits=()):
        nc = self.nc
        for e in ENGS:
            for me in self.ins[e]:
                for (pe, pi) in me["deps"]:
                    self.ins[pe][pi]["sig"] = True
        for me in final_waits:
            me["sig"] = True
        nep = {}
        for e in ENGS:
            c = 0
            for me in self.ins[e]:
                if me["dma"]:
                    continue
                if me["sig"]:
                    c += 1
                    me["cnt"] = c
            nep[e] = c // EPOCH + 1
        dcount = {}
        for e in ENGS:
            for me in self.ins[e]:
                if me["dma"]:
                    k = (e, me["slot"])
                    dcount[k] = dcount.get(k, 0) + 16
                    me["cnt"] = dcount[k]
        with contextlib.ExitStack() as st:
            esem = {e: [st.enter_context(nc.semaphore(f"s_{e}_{i}")) for i in range(nep[e])] for e in ENGS}
            dsem = {}
            for k in dcount:
                dsem[k] = st.enter_context(nc.semaphore(f"d_{k[0]}_{k[1]}"))
            block = st.enter_context(nc.Block())

            def target(p):
                if p["dma"]:
                    return dsem[(p["eng"], p["slot"])], p["cnt"]
                c = p["cnt"]
                ep = (c - 1) // EPOCH
                return esem[p["eng"]][ep], c - ep * EPOCH

            def run(e, engobj):
                waited = {}
                for me in self.ins[e]:
                    best = {}
                    for (pe, pi) in me["deps"]:
                        p = self.ins[pe][pi]
                        key = ("d", pe, p["slot"]) if p["dma"] else ("e", pe)
                        if key not in best or best[key]["idx"] < p["idx"]:
                            best[key] = p
                    for key, p in best.items():
                        if waited.get(key, 0) >= p["cnt"]:
                            continue
                        waited[key] = p["cnt"]
                        sem, val = target(p)
                        engobj.wait_ge(sem, val)
                    r = me["fn"](engobj)
                    if me["dma"]:
                        r.then_inc(dsem[(e, me["slot"])], 16)
                    elif me["sig"]:
                        sem, _ = target(me)
                        r.then_inc(sem, 1)
                if e == "sp":
                    for p in final_waits:
                        sem, val = target(p)
                        engobj.wait_ge(sem, val)

            block.tensor(lambda eng: run("pe", eng))
            block.scalar(lambda eng: run("act", eng))
            block.vector(lambda eng: run("dve", eng))
            block.gpsimd(lambda eng: run("pool", eng))
            block.sync(lambda eng: run("sp", eng))


def host_consts(S):
    NBLK = S // 256
    p = np.arange(128)
    c = {}
    c["ident_f"] = np.eye(128, dtype=np.float32)
    c["tri_incl"] = (p[:, None] <= p[None, :]).astype(np.float32)
    c["tri_strict"] = (p[:, None] < p[None, :]).astype(np.float32)
    c["ones_f"] = np.ones((128, 128), np.float32)
    bf = ml_dtypes.bfloat16
    c["ut_b"] = (p[:, None] >= p[None, :]).astype(bf)
    c["ones_b"] = np.ones((128, 512), bf)
    c["zeros_b"] = np.zeros((128, 128), bf)
    nm = np.where(p[None, :] < p[:, None], -BIG, 0.0).astype(np.float32)
    c["negmask_b"] = np.tile(nm[:, None, :], (1, 8, 1)).reshape(128, 1024).astype(bf)
    own = np.arange(NBLK + 1)
    n = np.arange(NBLK)
    vm = np.where(n[None, :] < own[:, None], 0.0, -1e30).astype(np.float32)
    om = (n[None, :] == own[:, None]).astype(np.float32)
    c["vmask"] = np.tile(vm.reshape(1, -1), (128, 1))
    c["ownmask"] = np.tile(om.reshape(1, -1), (128, 1))
    s = np.arange(S)
    kaug = np.zeros((NBLK + 1, S), np.float32)
    kaug[s // 256, s] = 1.0
    kaug[NBLK, :] = 1.0
    c["kaug"] = kaug.astype(bf)
    slopes = 2.0 ** (-8.0 * np.arange(1, 5) / 4)
    c["alibi_row"] = (-slopes[:, None] * np.arange(512)[None, :]).astype(bf)
    rel = np.arange(32)
    ab = slopes[None, :, None] * (p[:, None, None] + 384 - 128 * rel[None, None, :])
    c["alibi_kb"] = ab.astype(np.float32)
    return c


import os as _os
KSTOP = int(_os.environ.get('KSTOP', '0'))


def build(S, NSEQ, DEPTH, dbg=False, passes=(1, 2, 3, 4, 5), ng_limit=None):
    nc = bass.Bass("TRN2", target_bir_lowering=False)
    NG, NT, NBLK = S // 512, S // 128, S // 256
    if ng_limit:
        NG = ng_limit
    KA = 64 + NBLK + 1
    hc = host_consts(S)

    def din(name, shape, dt=F32):
        return nc.dram_tensor(name, list(shape), dt, kind="ExternalInput").ap()

    def dscr(name, shape, dt):
        return nc.dram_tensor(name, list(shape), dt, kind=("ExternalOutput" if dbg else "Internal")).ap()

    x_in = din("x", [NSEQ, S, D])
    w_in = din("w_in", [DEPTH, D, INC])
    w_out = din("w_out", [DEPTH, D, D])
    w_gate = din("w_gate", [DEPTH, D, FH])
    w_up = din("w_up", [DEPTH, D, FH])
    w_down = din("w_down", [DEPTH, FH, D])
    vec1024 = din("vec1024", [DEPTH, 4, D])
    convw = din("convw", [DEPTH, 128, 8, 4])
    convb = din("convb", [DEPTH, 128, 8])
    hv = din("hv", [DEPTH, 3, 8])
    ssdn = din("ssdn", [DEPTH, 512])
    sbn = din("sbn", [DEPTH, 64, 4])
    mon = din("mon", [DEPTH, 64, 4])
    cd = {}
    for k, v in hc.items():
        cd[k] = din("c_" + k, v.shape, BF16 if v.dtype == ml_dtypes.bfloat16 else F32)
    out = nc.dram_tensor("out", [NSEQ, S, D], F32, kind="ExternalOutput").ap()

    d_ssd = [dscr(f"d_ssd{b}", [512, S], BF16) for b in range(NSEQ)]
    d_sbq = [dscr(f"d_sbq{b}", [256, S], BF16) for b in range(NSEQ)]
    d_sbk = [dscr(f"d_sbk{b}", [256, S], BF16) for b in range(NSEQ)]
    d_sbv = [dscr(f"d_sbv{b}", [S, 256], BF16) for b in range(NSEQ)]
    d_moq = [dscr(f"d_moq{b}", [256, S], BF16) for b in range(NSEQ)]
    d_mok = [dscr(f"d_mok{b}", [256, S], BF16) for b in range(NSEQ)]
    d_mov = [dscr(f"d_mov{b}", [S, 256], BF16) for b in range(NSEQ)]
    d_sel = [dscr(f"d_sel{b}", [4 * NBLK, S], BF16) for b in range(NSEQ)]
    d_msb = [dscr(f"d_msb{b}", [4, 64, S], BF16) for b in range(NSEQ)]
    d_mmo = [dscr(f"d_mmo{b}", [4, 64, S], BF16) for b in range(NSEQ)]
    d_x1 = [dscr(f"d_x1{b}", [S, D], F32) for b in range(NSEQ)]
    d_x2 = [dscr(f"d_x2{b}", [S, D], F32) for b in range(NSEQ)]

    dbg_t = dscr("dbg_t", [128, 256], F32)
    P = Prog(nc)
    sb_ = nc.alloc_sbuf_tensor
    _n = [0]
    ARENA_BYTES = 192 * 1024
    arena_state = {"on": False, "off": 0, "t": None}

    def SB(shape, dt, name=None):
        _n[0] += 1
        if not arena_state["on"]:
            return sb_(name or f"t{_n[0]}", list(shape), dt)
        free = int(np.prod(shape[1:]))
        nb = free * (2 if dt == BF16 else 4)
        nb = (nb + 63) // 64 * 64
        off = arena_state["off"]
        assert off + nb <= ARENA_BYTES, (name, off, nb)
        ap = arena_state["t"][0:shape[0], off // 4:(off + nb) // 4]
        if dt == BF16:
            ap = ap.bitcast(BF16)
        ap = ap[:, 0:free]
        if len(shape) == 3:
            ap = ap.rearrange("p (a b) -> p a b", b=shape[2])
        elif len(shape) == 4:
            ap = ap.rearrange("p (a b c) -> p a b c", b=shape[2], c=shape[3])
        arena_state["off"] = off + nb
        return ap

    def DMA(out_, in_, reads, writes, eng="sp"):
        return P.add(eng, lambda e: e.dma_start(out=out_, in_=in_), reads, writes, dma=True)

    def MM(out_, lhsT, rhs, start, stop, reads, writes):
        return P.add("pe", lambda e: e.matmul(out_, lhsT=lhsT, rhs=rhs, start=start, stop=stop), reads, writes)

    def TR(out_, in_, ident, reads, writes):
        return P.add("pe", lambda e: e.transpose(out=out_, in_=in_, identity=ident), reads, writes)

    def ACT(out_, in_, func, reads, writes, **kw):
        return P.add("act", lambda e: e.activation(out=out_, in_=in_, func=func, **kw), reads, writes)

    def TT(eng, out_, in0, in1, op, reads, writes):
        return P.add(eng, lambda e: e.tensor_tensor(out=out_, in0=in0, in1=in1, op=op), reads, writes)

    def TS(eng, out_, in0, s1, s2, op0, op1, reads, writes):
        if s2 is None:
            return P.add(eng, lambda e: e.tensor_scalar(out=out_, in0=in0, scalar1=s1, scalar2=None, op0=op0), reads, writes)
        return P.add(eng, lambda e: e.tensor_scalar(out=out_, in0=in0, scalar1=s1, scalar2=s2, op0=op0, op1=op1), reads, writes)

    def STT(eng, out_, in0, scalar, in1, op0, op1, reads, writes):
        return P.add(eng, lambda e: e.scalar_tensor_tensor(out=out_, in0=in0, scalar=scalar, in1=in1, op0=op0, op1=op1), reads, writes)

    def CP(eng, out_, in_, reads, writes):
        if eng == "act":
            return ACT(out_, in_, AF.Copy, reads, writes)
        return P.add(eng, lambda e: e.tensor_copy(out=out_, in_=in_), reads, writes)

    def MS(eng, ap, val, writes):
        return P.add(eng, lambda e: e.memset(ap, val), (), writes)

    psb = [nc.alloc_psum_tensor(f"ps{i}", [128, 512], F32) for i in range(8)]

    def psbf(i):
        return psb[i][:].bitcast(BF16)

    ident_f = SB([128, 128], F32, "ident_f")
    ident_b = SB([128, 128], BF16, "ident_b")
    tri_incl = SB([128, 128], F32, "tri_incl")
    tri_strict = SB([128, 128], F32, "tri_strict")
    ones_f = SB([128, 128], F32, "ones_f")
    ut_b = SB([128, 128], BF16, "ut_b")
    ones_b = SB([128, 512], BF16, "ones_b")
    zeros_b = SB([128, 128], BF16, "zeros_b")
    negmask_b = SB([128, 1024], BF16, "negmask_b")
    vmask = SB([128, (NBLK + 1) * NBLK], F32, "vmask")
    ownmask = SB([128, (NBLK + 1) * NBLK], F32, "ownmask")
    alibi_kb = SB([128, 4, 32], F32, "alibi_kb")
    for nm_, t in [("ident_f", ident_f), ("tri_incl", tri_incl), ("tri_strict", tri_strict), ("ones_f", ones_f),
                   ("ut_b", ut_b), ("ones_b", ones_b), ("zeros_b", zeros_b), ("negmask_b", negmask_b),
                   ("vmask", vmask), ("ownmask", ownmask), ("alibi_kb", alibi_kb)]:
        DMA(t[:], cd[nm_], (), ["C_" + nm_])
    CP("dve", ident_b[:], ident_f[:], ["C_ident_f"], ["C_ident_b"])
    CONST = ["C_ident_f", "C_ident_b", "C_tri_incl", "C_tri_strict", "C_ones_f", "C_ut_b", "C_ones_b", "C_zeros_b",
             "C_negmask_b", "C_vmask", "C_ownmask", "C_alibi_kb"]

    bar_sb = {e: SB([128, 8], F32, f"bar_{e}") for e in ("act", "dve", "pool")}
    bar_dram = nc.dram_tensor("bar_dram", [2, 64], F32).ap()

    def barrier():
        keys = []
        P.add("pe", lambda e: e.matmul(psb[7][:, 0:8], lhsT=zeros_b[:, 0:128], rhs=zeros_b[:, 0:8], start=True, stop=True),
              ["C_zeros_b"], ["ps7", "bar_pe"])
        for en in ("act", "dve", "pool"):
            CP(en, bar_sb[en][:], ones_f[:, 0:8], ["C_ones_f"], [f"bar_{en}"])
        allk = ["bar_pe", "bar_act", "bar_dve", "bar_pool"]
        dmas = {(d["eng"], d["idx"]) for d in P.dma_prev.values()}
        m = P.add("pe", lambda e: e.matmul(psb[7][:, 0:8], lhsT=zeros_b[:, 0:128], rhs=zeros_b[:, 0:8], start=True, stop=True),
                  ["C_zeros_b"] + allk, ["ps7", "bar2_pe"])
        m["deps"] |= dmas
        for en in ("act", "dve", "pool"):
            m = CP(en, bar_sb[en][:], ones_f[:, 0:8], ["C_ones_f"] + allk, [f"bar2_{en}"])
            m["deps"] |= dmas
        m = DMA(bar_dram[1:2, :], cd["ones_f"][0:1, 0:64], allk, ["bar2_sp"])
        m["deps"] |= {d for d in dmas if d != (m["eng"], m["idx"])}

    arena_state["t"] = sb_("arena", [128, ARENA_BYTES // 4], F32)
    arena_state["on"] = True
    gain = [SB([128, D], F32, f"gain{i}") for i in range(2)]
    xt = [SB([128, D], F32, f"xt{i}") for i in range(4)]
    junks = [SB([128, D], BF16, f"junk{i}") for i in range(4)]
    _jr = [0]

    def nextjunk():
        _jr[0] = (_jr[0] + 1) % 4
        return junks[_jr[0]], f"junk{_jr[0]}"
    ss4 = SB([128, 8], F32, "ss4")
    rs4 = SB([128, 8], F32, "rs4")
    hb = [SB([128, D], BF16, f"hb{i}") for i in range(2)]

    def load_gain(slot, l, which):
        DMA(gain[slot][:], vec1024[l, which].partition_broadcast(128), (), [f"gain{slot}"])

    base_off = arena_state["off"]

    win = SB([128, 8, INC], BF16, "win")
    hT = SB([128, 8, 512], BF16, "hT")
    zs = SB([128, 4, 512], F32, "zs")
    ubuf = [SB([128, 515], F32, f"u{i}") for i in range(2)]
    halo = SB([128, 8, 3], F32, "halo")
    cta = [SB([128, 512], F32, f"cta{i}") for i in range(2)]
    ctb = [SB([128, 512], F32, f"ctb{i}") for i in range(2)]
    xsT = SB([128, 4, 512], F32, "xsT")
    xs_tok = [SB([128, 512], F32, f"xstok{i}") for i in range(4)]
    BT = SB([128, 2, 512], BF16, "BT")
    CT = SB([128, 2, 512], BF16, "CT")
    Btok = SB([128, 4, 2, 128], BF16, "Btok")
    cw = SB([128, 8, 4], F32, "cw")
    cb = SB([128, 8], F32, "cb")
    hv_bc = SB([128, 3, 8], F32, "hv_bc")
    a_bc = SB([128, 8], F32, "a_bc")
    dsk_bc = SB([128, 8, 64], F32, "dsk_bc")
    ssdn_bc = SB([128, 512], F32, "ssdn_bc")
    dtx = SB([128, 4, 8], F32, "dtx")
    dt_all = SB([128, 4, 8], F32, "dt_all")
    adt = SB([128, 4, 8], F32, "adt")
    sbq_st = SB([128, 2, 512], BF16, "sbq_st")
    sbk_st = SB([128, 2, 512], BF16, "sbk_st")
    sbv_st = SB([128, 4, 256], BF16, "sbv_st")
    moq32 = SB([128, 2, 512], F32, "moq32")
    mok32 = SB([128, 2, 512], F32, "mok32")
    moq_st = SB([128, 2, 512], BF16, "moq_st")
    mok_st = SB([128, 2, 512], BF16, "mok_st")
    mov_st = SB([128, 4, 256], BF16, "mov_st")
    kmean = SB([128, 2, 2, NBLK], F32, "kmean")
    gm = SB([128, 4, NBLK], F32, "gm")
    max8 = SB([128, 4, 8], F32, "max8")
    thr = SB([128, 4], F32, "thr")
    sel = SB([128, 4, NBLK], F32, "sel")
    selneg = SB([128, 4 * NBLK], BF16, "selneg")
    selT_st = SB([64, 512], BF16, "selT_st")
    rhs_cum = SB([128, 8, 128], F32, "rhs_cum")
    negac = SB([128, 8], F32, "negac")
    ea = SB([128, 8], F32, "ea")
    cdb = SB([128, 8], F32, "cdb")
    Eb = SB([128, 8, 128], F32, "Eb")
    MT = SB([128, 8, 128], BF16, "MT")
    dtd = SB([128, 8], F32, "dtd")
    xdt = SB([128, 8, 64], BF16, "xdt")
    xdt2 = SB([128, 8, 64], BF16, "xdt2")
    S32 = SB([128, 512], F32, "S32")
    Sb = SB([128, 512], BF16, "Sb")
    y1 = SB([128, 512], F32, "y1")
    y2 = SB([128, 512], F32, "y2")
    y3 = SB([128, 512], F32, "y3")
    ssq = SB([128, 2], F32, "ssq")
    rsq = SB([128, 2], F32, "rsq")
    yn = SB([128, 512], BF16, "yn")
    mixT_ssd = SB([128, 4, 512], BF16, "mixT_ssd")

    rot01 = [0]

    def nextbank():
        rot01[0] ^= 1
        return rot01[0]

    def load_layer_p1(l):
        for (c0, c1) in [(0, 512), (512, 1536), (1536, 2312), (2312, INC)]:
            DMA(win[:, :, c0:c1], w_in[l, :, c0:c1].rearrange("(c p) n -> p c n", p=128), (), ["win"], eng="pool")
        DMA(cw[:], convw[l], (), ["cw"])
        DMA(cb[:], convb[l], (), ["cb"])
        for i in range(3):
            DMA(hv_bc[:, i, :], hv[l, i].partition_broadcast(128), (), ["hv_bc"])
        DMA(ssdn_bc[:], ssdn[l].partition_broadcast(128), (), ["ssdn_bc"])
        ACT(a_bc[:], hv_bc[:, 1, :], AF.Exp, ["hv_bc"], ["a_bc"])
        TS("dve", a_bc[:], a_bc[:], -1.0, None, ALU.mult, None, ["a_bc"], ["a_bc"])
        CP("dve", dsk_bc[:], hv_bc[:, 2, :].unsqueeze(2).to_broadcast([128, 8, 64]), ["hv_bc"], ["dsk_bc"])

    def pass1(l, b, xsrc):
        load_layer_p1(l)
        load_gain(0, l, 0)
        MS("pool", S32[:], 0.0, ["S32"])
        MS("pool", Sb[:], 0.0, ["Sb"])
        MS("pool", halo[:], 0.0, ["halo"])
        MS("pool", kmean[:], 0.0, ["kmean"])
        for g in range(NG):
            t0 = g * 512
            for tt in range(4):
                DMA(xt[tt][:], xsrc[t0 + tt * 128:t0 + (tt + 1) * 128, :], (), [f"xt{tt}"])
            for tt in range(4):
                jb, jk = nextjunk()
                ACT(jb[:], xt[tt][:], AF.Square, [f"xt{tt}"], [jk, f"ss4_{tt}"], scale=1.0 / 32, accum_out=ss4[:, tt:tt + 1])
            ACT(rs4[:, 0:4], ss4[:, 0:4], AF.Ln, [f"ss4_{i}" for i in range(4)], ["rs4"], bias=EPS)
            ACT(rs4[:, 0:4], rs4[:, 0:4], AF.Exp, ["rs4"], ["rs4"], scale=-0.5)
            for tt in range(4):
                hbk = f"hb{tt % 2}"
                STT("dve", hb[tt % 2][:], xt[tt][:], rs4[:, tt:tt + 1], gain[0][:], ALU.mult, ALU.mult,
                    [f"xt{tt}", "rs4", "gain0"], [hbk])
                bk = nextbank()
                pT = psbf(bk)
                for c in range(8):
                    TR(pT[:, c * 128:(c + 1) * 128], hb[tt % 2][:, c * 128:(c + 1) * 128], ident_b[:], [hbk, "C_ident_b"], [f"ps{bk}"])
                CP("act" if tt % 2 else "dve", hT[:, :, tt * 128:(tt + 1) * 128], pT.rearrange("p (c k) -> p c k", k=128),
                   [f"ps{bk}"], ["hT"])

            def proj_fm(col0, ncols=128):
                bk = nextbank()
                for c in range(8):
                    MM(psb[bk][0:ncols, :], win[:, c, col0:col0 + ncols], hT[:, c, :], c == 0, c == 7, ["win", "hT"], [f"ps{bk}"])
                return bk

            def proj_tm(tt, col0, ncols, bk, off):
                for c in range(8):
                    MM(psb[bk][:, off:off + ncols], hT[:, c, tt * 128:(tt + 1) * 128], win[:, c, col0:col0 + ncols], c == 0, c == 7,
                       ["win", "hT"], [f"ps{bk}"])

            if KSTOP == 1:
                return
            for tt in range(4):
                bk = nextbank()
                proj_tm(tt, 0, 512, bk, 0)
                ACT(zs[:, tt, :], psb[bk][:], AF.Silu, [f"ps{bk}"], ["zs"])
            if KSTOP == 2:
                return
            for tt in range(4):
                for c in range(8):
                    MM(psb[4][:, 400 + tt * 8:408 + tt * 8], hT[:, c, tt * 128:(tt + 1) * 128], win[:, c, 1536:1544], c == 0, c == 7,
                       ["win", "hT"], ["ps4"])
            TT("dve", dtx[:], psb[4][:, 400:432].rearrange("p (t h) -> p t h", h=8),
               hv_bc[:, 0, :].unsqueeze(1).to_broadcast([128, 4, 8]), ALU.add, ["ps4", "hv_bc"], ["dtx"])
            ACT(dtx[:], dtx[:], AF.Exp, ["dtx"], ["dtx"])
            ACT(dt_all[:], dtx[:], AF.Ln, ["dtx"], ["dt_all"], bias=1.0)
            TT("dve", adt[:], dt_all[:], a_bc[:].unsqueeze(1).to_broadcast([128, 4, 8]), ALU.mult, ["dt_all", "a_bc"], ["adt"])
            if KSTOP == 3:
                return
            for j in range(8):
                bk = proj_fm(512 + 128 * j)
                u = ubuf[j % 2]
                uk = f"u{j % 2}"
                CP("dve", u[:, 0:3], halo[:, j, :], ["halo"], [uk])
                CP("dve", u[:, 3:515], psb[bk][:], [f"ps{bk}"], [uk])
                CP("pool", halo[:, j, :], u[:, 512:515], [uk], ["halo"])
                ta = cta[j % 2]
                tk = f"cta{j % 2}"
                tb = ctb[j % 2]
                tbk = f"ctb{j % 2}"
                TS("pool", ta[:], u[:, 0:512], cw[:, j, 0:1], None, ALU.mult, None, [uk, "cw"], [tk])
                TS("pool", tb[:], u[:, 2:514], cw[:, j, 2:3], None, ALU.mult, None, [uk, "cw"], [tbk])
                STT("dve", ta[:], u[:, 1:513], cw[:, j, 1:2], ta[:], ALU.mult, ALU.add, [uk, "cw", tk], [tk])
                STT("dve", tb[:], u[:, 3:515], cw[:, j, 3:4], tb[:], ALU.mult, ALU.add, [uk, "cw", tbk], [tbk])
                TT("pool", ta[:], ta[:], tb[:], ALU.add, [tk, tbk], [tk])
                if j < 4:
                    dst, dk = xsT[:, j, :], "xsT"
                elif j < 6:
                    dst, dk = BT[:, j - 4, :], "BT"
                else:
                    dst, dk = CT[:, j - 6, :], "CT"
                ACT(dst, ta[:], AF.Silu, [tk, "cb"], [dk], bias=cb[:, j:j + 1])
            if KSTOP == 4:
                return
            for pr in range(2):
                bk = proj_fm(1544 + 128 * pr)
                ACT(sbq_st[:, pr, :], psb[bk][:], AF.Copy, [f"ps{bk}"], ["sbq_st"], scale=0.125)
                bk = proj_fm(1800 + 128 * pr)
                CP("dve", sbk_st[:, pr, :], psb[bk][:], [f"ps{bk}"], ["sbk_st"])
            for t2 in range(2):
                bk = nextbank()
                for q in range(2):
                    proj_tm(2 * t2 + q, 2056, 256, bk, q * 256)
                CP("act", sbv_st[:, 2 * t2:2 * t2 + 2, :], psb[bk][:].rearrange("p (t c) -> p t c", c=256), [f"ps{bk}"], ["sbv_st"])
            DMA(d_sbq[b].rearrange("(r p) s -> p r s", p=128)[:, :, t0:t0 + 512], sbq_st[:], ["sbq_st"], [f"d_sbq{b}_{g}"])
            DMA(d_sbk[b].rearrange("(r p) s -> p r s", p=128)[:, :, t0:t0 + 512], sbk_st[:], ["sbk_st"], [f"d_sbk{b}_{g}"])
            DMA(d_sbv[b][t0:t0 + 512, :].rearrange("(t p) c -> p t c", p=128), sbv_st[:], ["sbv_st"], [f"d_sbv{b}_{g}"])
            if KSTOP == 5:
                return
            for pr in range(2):
                bk = proj_fm(2312 + 128 * pr)
                CP("dve", moq32[:, pr, :], psb[bk][:], [f"ps{bk}"], ["moq32"])
                TS("pool", moq_st[:, pr, :], moq32[:, pr, :], 0.125, None, ALU.mult, None, ["moq32"], ["moq_st"])
                bk = proj_fm(2568 + 128 * pr)
                CP("act", mok32[:, pr, :], psb[bk][:], [f"ps{bk}"], ["mok32"])
                CP("pool", mok_st[:, pr, :], mok32[:, pr, :], ["mok32"], ["mok_st"])
                for e_ in range(2):
                    rows = slice(e_ * 64, e_ * 64 + 64)
                    P.add("dve", lambda e, pr=pr, g=g, e_=e_, rows=rows: e.tensor_reduce(
                        out=kmean[rows, pr, e_, 2 * g:2 * g + 2], in_=mok32[rows, pr, :].rearrange("p (b k) -> p b k", k=256),
                        axis=AX.X, op=ALU.add), ["mok32"], ["kmean"])
            for t2 in range(2):
                bk = nextbank()
                for q in range(2):
                    proj_tm(2 * t2 + q, 2824, 256, bk, q * 256)
                CP("dve", mov_st[:, 2 * t2:2 * t2 + 2, :], psb[bk][:].rearrange("p (t c) -> p t c", c=256), [f"ps{bk}"], ["mov_st"])
            DMA(d_moq[b].rearrange("(r p) s -> p r s", p=128)[:, :, t0:t0 + 512], moq_st[:], ["moq_st"], [f"d_moq{b}_{g}"])
            DMA(d_mok[b].rearrange("(r p) s -> p r s", p=128)[:, :, t0:t0 + 512], mok_st[:], ["mok_st"], [f"d_mok{b}_{g}"])
            DMA(d_mov[b][t0:t0 + 512, :].rearrange("(t p) c -> p t c", p=128), mov_st[:], ["mov_st"], [f"d_mov{b}_{g}"])
            if KSTOP == 6:
                return
            gps = psb[5][:, 0:16 * NBLK].rearrange("p (t h n) -> p t h n", t=4, h=4)
            for tt in range(4):
                for pr in range(2):
                    MM(gps[:, tt, 2 * pr:2 * pr + 2, :], moq32[:, pr, tt * 128:(tt + 1) * 128],
                       kmean[:, pr, :, :], True, True, ["moq32", "kmean"], ["ps5"])
            if KSTOP == 61:
                return
            for tt in range(4):
                own = 2 * g + tt // 2
                TT("dve", gm[:], gps[:, tt], vmask[:, own * NBLK:(own + 1) * NBLK].unsqueeze(1).to_broadcast([128, 4, NBLK]),
                   ALU.add, ["ps5", "C_vmask"], ["gm"])
                for h in range(4):
                    P.add("dve", lambda e, h=h: e.max(out=max8[:, h, :], in_=gm[:, h, :]), ["gm"], ["max8"])
                TS("dve", thr[:], max8[:, :, 2], -1e20, None, ALU.max, None, ["max8"], ["thr"])
                TT("dve", sel[:], gm[:], thr[:].unsqueeze(2).to_broadcast([128, 4, NBLK]), ALU.is_ge, ["gm", "thr"], ["sel"])
                TT("dve", sel[:], sel[:], ownmask[:, own * NBLK:(own + 1) * NBLK].unsqueeze(1).to_broadcast([128, 4, NBLK]),
                   ALU.add, ["sel", "C_ownmask"], ["sel"])
                TS("dve", selneg[:], sel[:].rearrange("p h n -> p (h n)"), -1.0, BIG, ALU.add, ALU.mult, ["sel"], ["selneg"])
                if dbg and g == NG - 1 and tt == 3:
                    DMA(dbg_t[:, 0:4 * NBLK], gm[:].rearrange("p h n -> p (h n)"), ["gm"], ["dbg_t"])
                    DMA(dbg_t[:, 64:96], max8[:].rearrange("p h n -> p (h n)"), ["max8"], ["dbg_t"])
                    DMA(dbg_t[:, 96:100], thr[:], ["thr"], ["dbg_t"])
                    DMA(dbg_t[:, 128:128 + 4 * NBLK], sel[:].rearrange("p h n -> p (h n)"), ["sel"], ["dbg_t"])
                if KSTOP == 62:
                    continue
                bk = nextbank()
                TR(psbf(bk)[0:4 * NBLK, 0:128], selneg[:], ident_b[:], ["selneg", "C_ident_b"], [f"ps{bk}"])
                CP("act", selT_st[0:4 * NBLK, tt * 128:(tt + 1) * 128], psbf(bk)[0:4 * NBLK, 0:128], [f"ps{bk}"], ["selT_st"])
            if KSTOP == 62:
                return
            DMA(d_sel[b][:, t0:t0 + 512], selT_st[0:4 * NBLK, :], ["selT_st"], [f"d_sel{b}_{g}"])
            if KSTOP == 7:
                return
            for tt in range(4):
                bk = nextbank()
                for j in range(4):
                    TR(psb[bk][:, j * 128:(j + 1) * 128], xsT[:, j, tt * 128:(tt + 1) * 128], ident_f[:], ["xsT", "C_ident_f"], [f"ps{bk}"])
                CP("act" if tt % 2 else "dve", xs_tok[tt][:], psb[bk][:], [f"ps{bk}"], [f"xstok{tt}"])
                bk = nextbank()
                for gi in range(2):
                    TR(psbf(bk)[:, gi * 128:(gi + 1) * 128], BT[:, gi, tt * 128:(tt + 1) * 128], ident_b[:], ["BT", "C_ident_b"], [f"ps{bk}"])
                CP("dve", Btok[:, tt, :, :], psbf(bk)[:, 0:256].rearrange("p (g n) -> p g n", n=128), [f"ps{bk}"], ["Btok"])
            if KSTOP == 8:
                return
            acb = None
            for tt in range(4):
                tsl = slice(tt * 128, (tt + 1) * 128)
                xs3 = xs_tok[tt][:].rearrange("p (h d) -> p h d", d=64)
                xk = f"xstok{tt}"
                TT("dve", rhs_cum[:], tri_incl[:].unsqueeze(1).to_broadcast([128, 8, 128]),
                   adt[:, tt, :].unsqueeze(2).to_broadcast([128, 8, 128]), ALU.mult, ["C_tri_incl", "adt"], ["rhs_cum"])
                rc = rhs_cum[:].rearrange("p h l -> p (h l)")
                for half in range(2):
                    pb = psb[2 + half]
                    MM(pb[:], ones_f[:], rc[:, half * 512:(half + 1) * 512], True, False, ["C_ones_f", "rhs_cum"], [f"ps{2 + half}"])
                    MM(pb[:], ident_b[:], negmask_b[:, half * 512:(half + 1) * 512], False, True, ["C_ident_b", "C_negmask_b"], [f"ps{2 + half}"])
                MM(psb[4][:, 0:8], tri_incl[:], adt[:, tt, :], True, True, ["C_tri_incl", "adt"], ["ps4"])
                ACT(negac[:], psb[4][:, 0:8], AF.Copy, ["ps4"], ["negac"], scale=-1.0)
                ACT(ea[:], psb[4][:, 0:8], AF.Exp, ["ps4"], ["ea"])
                for half in range(2):
                    ACT(cdb[:, half * 4:(half + 1) * 4], psb[2 + half][:].rearrange("p (h l) -> p h l", l=128)[:, :, 127], AF.Exp,
                        [f"ps{2 + half}"], ["cdb"])
                for h in range(8):
                    ACT(Eb[:, h, :], psb[2 + h // 4][:, (h % 4) * 128:(h % 4 + 1) * 128], AF.Exp, [f"ps{2 + h // 4}", "negac"], ["Eb"],
                        bias=negac[:, h:h + 1])
                for gi in range(2):
                    MM(psb[4][:, 128 + gi * 128:256 + gi * 128], BT[:, gi, tsl], CT[:, gi, tsl], True, True, ["BT", "CT"], ["ps4"])
                for gi in range(2):
                    TT("dve", MT[:, gi * 4:(gi + 1) * 4, :], Eb[:, gi * 4:(gi + 1) * 4, :],
                       psb[4][:, 128 + gi * 128:256 + gi * 128].unsqueeze(1).to_broadcast([128, 4, 128]), ALU.mult,
                       ["Eb", "ps4"], ["MT"])
                TT("dve", dtd[:], dt_all[:, tt, :], Eb[:, :, 127], ALU.mult, ["dt_all", "Eb"], ["dtd"])
                TT("dve", xdt[:], xs3, dt_all[:, tt, :].unsqueeze(2).to_broadcast([128, 8, 64]), ALU.mult, [xk, "dt_all"], ["xdt"])
                TT("pool", xdt2[:], xs3, dtd[:].unsqueeze(2).to_broadcast([128, 8, 64]), ALU.mult, [xk, "dtd"], ["xdt2"])
                x2f = xdt2[:].rearrange("p h d -> p (h d)")
                x1f = xdt[:].rearrange("p h d -> p (h d)")
                for gi in range(2):
                    MM(psb[5][:, gi * 256:(gi + 1) * 256], Btok[:, tt, gi, :], x2f[:, gi * 256:(gi + 1) * 256], True, True,
                       ["Btok", "xdt2"], ["ps5"])
                for gi in range(2):
                    MM(psb[6][:, gi * 256:(gi + 1) * 256], CT[:, gi, tsl], Sb[:, gi * 256:(gi + 1) * 256], True, True,
                       ["CT", "Sb"], ["ps6"])
                for h in range(8):
                    MM(psb[7][:, h * 64:(h + 1) * 64], MT[:, h, :], x1f[:, h * 64:(h + 1) * 64], True, True, ["MT", "xdt"], ["ps7"])
                TT("dve", S32[:].rearrange("p (h d) -> p h d", d=64), S32[:].rearrange("p (h d) -> p h d", d=64),
                   cdb[:].unsqueeze(2).to_broadcast([128, 8, 64]), ALU.mult, ["S32", "cdb"], ["S32"])
                TT("dve", S32[:], S32[:], psb[5][:], ALU.add, ["S32", "ps5"], ["S32"])
                CP("pool", Sb[:], S32[:], ["S32"], ["Sb"])
                TT("dve", y1[:].rearrange("p (h d) -> p h d", d=64), psb[6][:].rearrange("p (h d) -> p h d", d=64),
                   ea[:].unsqueeze(2).to_broadcast([128, 8, 64]), ALU.mult, ["ps6", "ea"], ["y1"])
                TT("dve", y1[:], y1[:], psb[7][:], ALU.add, ["y1", "ps7"], ["y1"])
                TT("pool", y2[:].rearrange("p (h d) -> p h d", d=64), xs3, dsk_bc[:], ALU.mult, [xk, "dsk_bc"], ["y2"])
                TT("pool", y2[:], y2[:], y1[:], ALU.add, ["y2", "y1"], ["y2"])
                TT("dve", y3[:], y2[:], zs[:, tt, :], ALU.mult, ["y2", "zs"], ["y3"])
                for gi in range(2):
                    jb, jk = nextjunk()
                    ACT(jb[:, 0:256], y3[:, gi * 256:(gi + 1) * 256], AF.Square, ["y3"], [jk, f"ssq{gi}"], scale=1.0 / 16,
                        accum_out=ssq[:, gi:gi + 1])
                ACT(rsq[:], ssq[:], AF.Ln, ["ssq0", "ssq1"], ["rsq"], bias=EPS)
                ACT(rsq[:], rsq[:], AF.Exp, ["rsq"], ["rsq"], scale=-0.5)
                for gi in range(2):
                    STT("dve", yn[:, gi * 256:(gi + 1) * 256], y3[:, gi * 256:(gi + 1) * 256], rsq[:, gi:gi + 1],
                        ssdn_bc[:, gi * 256:(gi + 1) * 256], ALU.mult, ALU.mult, ["y3", "rsq", "ssdn_bc"], ["yn"])
                bk = nextbank()
                for j in range(4):
                    TR(psbf(bk)[:, j * 128:(j + 1) * 128], yn[:, j * 128:(j + 1) * 128], ident_b[:], ["yn", "C_ident_b"], [f"ps{bk}"])
                CP("act", mixT_ssd[:, :, tsl], psbf(bk)[:, 0:512].rearrange("p (c k) -> p c k", k=128), [f"ps{bk}"], ["mixT_ssd"])
            DMA(d_ssd[b].rearrange("(c p) s -> p c s", p=128)[:, :, t0:t0 + 512], mixT_ssd[:], ["mixT_ssd"], [f"d_ssd{b}_{g}"])

    arena_state["off"] = base_off
    p2_kT = SB([128, 2, S], BF16, "p2_kT")
    p2_v = SB([128, NT, 256], BF16, "p2_v")
    p2_q = [SB([128, 2, 512], BF16, f"p2_q{i}") for i in range(2)]
    p2_nq = [SB([128, 2, 512], BF16, f"p2_nq{i}") for i in range(2)]
    p2_e = [SB([128, 512], F32, f"p2_e{i}") for i in range(2)]
    p2_sp = [SB([128, 512], BF16, f"p2_sp{i}") for i in range(2)]
    p2_S = SB([128, 512], BF16, "p2_S")
    p2_w = [SB([128, 512], BF16, f"p2_w{i}") for i in range(2)]
    p23_y = SB([64, 4, 512], F32, "p23_y")
    p23_sq = SB([64, 4, 512], BF16, "p23_sq")
    p23_rs = SB([64, 512], F32, "p23_rs")
    p23_mix = SB([64, 4, 512], BF16, "p23_mix")
    p23_nw = SB([64, 4], F32, "p23_nw")

    def head_norm_store(l, b, g, dst):
        t0 = g * 512
        for h in range(4):
            ACT(p23_sq[:, h, :], p23_y[:, h, :], AF.Square, ["p23_y"], ["p23_sq"])
        for h in range(4):
            MM(psb[6][0:64, :], ones_b[0:64, 0:64], p23_sq[:, h, :], h == 0, h == 3, ["C_ones_b", "p23_sq"], ["ps6"])
        ACT(p23_rs[:], psb[6][0:64, :], AF.Ln, ["ps6"], ["p23_rs"], scale=1.0 / 256, bias=EPS)
        ACT(p23_rs[:], p23_rs[:], AF.Exp, ["p23_rs"], ["p23_rs"], scale=-0.5)
        for h in range(4):
            STT("dve", p23_mix[:, h, :], p23_y[:, h, :], p23_nw[:, h:h + 1], p23_rs[:], ALU.mult, ALU.mult,
                ["p23_y", "p23_nw", "p23_rs"], ["p23_mix"])
        DMA(dst.rearrange("h d s -> d h s")[:, :, t0:t0 + 512], p23_mix[:], ["p23_mix"], [f"{dst.tensor.name}_{g}"])

    def pass2(l, b):
        DMA(p23_nw[:], sbn[l], (), ["p23_nw"])
        for g in range(NG):
            t0 = g * 512
            DMA(p2_kT[:, :, t0:t0 + 512], d_sbk[b].rearrange("(r p) s -> p r s", p=128)[:, :, t0:t0 + 512],
                [f"d_sbk{b}_{g}"], [f"p2_kT_{g}"])
            DMA(p2_v[:, 4 * g:4 * g + 4, :], d_sbv[b][t0:t0 + 512, :].rearrange("(t p) c -> p t c", p=128),
                [f"d_sbv{b}_{g}"], [f"p2_v_{g}"])
            q, nq = p2_q[g % 2], p2_nq[g % 2]
            qk, nqk = f"p2_q{g % 2}", f"p2_nq{g % 2}"
            DMA(q[:], d_sbq[b].rearrange("(r p) s -> p r s", p=128)[:, :, t0:t0 + 512], [f"d_sbq{b}_{g}"], [qk])
            TS("pool", nq[:], q[:], -1.0, None, ALU.mult, None, [qk], [nqk])
            last = 4 * g + 3
            it = 0
            for h in range(4):
                pr, e_ = h // 2, h % 2
                rows = slice(e_ * 64, e_ * 64 + 64)
                yb = 4 + (h % 2)
                MS("pool", p2_S[:], 0.0, ["p2_S"])
                MM(psb[yb][0:64, :], zeros_b[:, 0:64], ones_b[:, 0:512], True, False, ["C_zeros_b", "C_ones_b"], [f"ps{yb}"])
                for kb in range(last, -1, -1):
                    gk = kb // 4
                    qoff = max(0, kb - 4 * g) * 128
                    W = 512 - qoff
                    diag = kb >= 4 * g
                    i2 = it % 2
                    it += 1
                    b1, b2 = i2, 2 + i2
                    ksl = slice(kb * 128, (kb + 1) * 128)
                    MM(psb[b1][:, 0:W], p2_kT[rows, pr, ksl], q[rows, pr, qoff:512], True, True, [f"p2_kT_{gk}", qk], [f"ps{b1}"])
                    ACT(p2_e[i2][:, 0:W], psb[b1][:, 0:W], AF.Exp, [f"ps{b1}"], [f"p2_e{i2}"])
                    ACT(p2_sp[i2][:, 0:W], p2_e[i2][:, 0:W], AF.Ln, [f"p2_e{i2}"], [f"p2_sp{i2}"], bias=1.0)
                    if diag:
                        TT("pool", p2_sp[i2][:, 0:128], p2_sp[i2][:, 0:128], tri_strict[:], ALU.mult, [f"p2_sp{i2}", "C_tri_strict"], [f"p2_sp{i2}"])
                    MM(psb[b2][:, 0:W], ut_b[:], p2_sp[i2][:, 0:W], True, False, ["C_ut_b", f"p2_sp{i2}"], [f"ps{b2}"])
                    if kb < last:
                        MM(psb[b2][:, 0:W], ones_b[:, 0:128], p2_S[:, qoff:512], False, False, ["C_ones_b", "p2_S"], [f"ps{b2}"])
                    MM(psb[b2][:, 0:W], p2_kT[rows, pr, ksl], nq[rows, pr, qoff:512], False, True, [f"p2_kT_{gk}", nqk], [f"ps{b2}"])
                    if kb > 0:
                        TT("pool", p2_S[:, qoff:512], p2_S[:, qoff:512], p2_sp[i2][:, 0:W], ALU.add, ["p2_S", f"p2_sp{i2}"], ["p2_S"])
                    ACT(p2_w[i2][:, 0:W], psb[b2][:, 0:W], AF.Exp, [f"ps{b2}"], [f"p2_w{i2}"], scale=-1.0)
                    if diag:
                        TT("dve", p2_w[i2][:, 0:128], p2_w[i2][:, 0:128], tri_strict[:], ALU.mult, [f"p2_w{i2}", "C_tri_strict"], [f"p2_w{i2}"])
                    MM(psb[yb][0:64, qoff:512], p2_v[:, kb, h * 64:(h + 1) * 64], p2_w[i2][:, 0:W], False, kb == 0,
                       [f"p2_v_{gk}", f"p2_w{i2}"], [f"ps{yb}"])
                CP("dve", p23_y[:, h, :], psb[yb][0:64, :], [f"ps{yb}"], ["p23_y"])
            head_norm_store(l, b, g, d_msb[b])

    arena_state["off"] = base_off
    p3_kT = [SB([96, S], BF16, f"p3_kT{h}") for h in range(4)]
    p3_v = SB([128, NT, 256], BF16, "p3_v")
    p3_q = [[SB([96, 512], BF16, f"p3_q{i}_{h}") for h in range(4)] for i in range(2)]
    p3_p = [SB([128, 512], BF16, f"p3_p{i}") for i in range(2)]
    arena_state["off"] = max(arena_state["off"], 0)
    p3_y = SB([64, 4, 512], F32, "p3_y")
    p3_sq = SB([64, 4, 512], BF16, "p3_sq")
    p3_rs = SB([64, 512], F32, "p3_rs")
    p3_mix = SB([64, 4, 512], BF16, "p3_mix")
    p3_nw = SB([64, 4], F32, "p3_nw")

    def pass3(l, b):
        nonlocal p23_y, p23_sq, p23_rs, p23_mix, p23_nw
        p23_y, p23_sq, p23_rs, p23_mix, p23_nw = p3_y, p3_sq, p3_rs, p3_mix, p3_nw
        DMA(p23_nw[:], mon[l], (), ["p23_nw"])
        for h in range(4):
            MS("pool", p3_kT[h][:], 0.0, [f"p3_kT{h}_c"])
            DMA(p3_kT[h][64:64 + NBLK + 1, :], cd["kaug"], [], [f"p3_kT{h}_c"])
            for i in range(2):
                MS("pool", p3_q[i][h][:], 0.0, [f"p3_q{i}_{h}"])
                DMA(p3_q[i][h][64 + NBLK:64 + NBLK + 1, :], cd["alibi_row"][h:h + 1, :], [], [f"p3_q{i}_{h}"])
        for g in range(NG):
            t0 = g * 512
            for h in range(4):
                DMA(p3_kT[h][0:64, t0:t0 + 512], d_mok[b][h * 64:(h + 1) * 64, t0:t0 + 512], [f"d_mok{b}_{g}", f"p3_kT{h}_c"], [f"p3_kT{h}_{g}"])
            DMA(p3_v[:, 4 * g:4 * g + 4, :], d_mov[b][t0:t0 + 512, :].rearrange("(t p) c -> p t c", p=128),
                [f"d_mov{b}_{g}"], [f"p3_v_{g}"])
            i_ = g % 2
            for h in range(4):
                qk = f"p3_q{i_}_{h}"
                DMA(p3_q[i_][h][0:64, :], d_moq[b][h * 64:(h + 1) * 64, t0:t0 + 512], [f"d_moq{b}_{g}"], [qk])
                DMA(p3_q[i_][h][64:64 + NBLK, :], d_sel[b][h * NBLK:(h + 1) * NBLK, t0:t0 + 512], [f"d_sel{b}_{g}"], [qk])
            it = 0
            for h in range(4):
                qk = f"p3_q{i_}_{h}"
                qa = p3_q[i_][h]
                yb = 4 + (h % 2)
                MM(psb[yb][0:64, :], zeros_b[:, 0:64], ones_b[:, 0:512], True, False, ["C_zeros_b", "C_ones_b"], [f"ps{yb}"])
                MM(psb[7][0:64, :], zeros_b[:, 0:64], ones_b[:, 0:512], True, False, ["C_zeros_b", "C_ones_b"], ["ps7"])
                for kb in range(0, 4 * g + 4):
                    gk = kb // 4
                    qoff = max(0, kb - 4 * g) * 128
                    W = 512 - qoff
                    diag = kb >= 4 * g
                    rel = 4 * g + 3 - kb
                    i2 = it % 2
                    it += 1
                    b1 = i2
                    lastk = kb == 4 * g + 3
                    MM(psb[b1][:, 0:W], p3_kT[h][:, kb * 128:(kb + 1) * 128], qa[:, qoff:512], True, True,
                       [f"p3_kT{h}_{gk}", f"p3_kT{h}_c", qk], [f"ps{b1}"])
                    ACT(p3_p[i2][:, 0:W], psb[b1][:, 0:W], AF.Exp, [f"ps{b1}", "C_alibi_kb"], [f"p3_p{i2}"], bias=alibi_kb[:, h, rel:rel + 1])
                    if diag:
                        TT("dve", p3_p[i2][:, 0:128], p3_p[i2][:, 0:128], tri_incl[:], ALU.mult, [f"p3_p{i2}", "C_tri_incl"], [f"p3_p{i2}"])
                    MM(psb[yb][0:64, qoff:512], p3_v[:, kb, h * 64:(h + 1) * 64], p3_p[i2][:, 0:W], False, lastk,
                       [f"p3_v_{gk}", f"p3_p{i2}"], [f"ps{yb}"])
                    MM(psb[7][0:64, qoff:512], ones_b[:, 0:64], p3_p[i2][:, 0:W], False, lastk, ["C_ones_b", f"p3_p{i2}"], ["ps7"])
                CP("act", p23_y[:, h, :], psb[yb][0:64, :], [f"ps{yb}"], ["p23_y"])
                P.add("dve", lambda e: e.reciprocal(out=p23_rs[:], in_=psb[7][0:64, :]), ["ps7"], ["p23_rs"])
                TT("dve", p23_y[:, h, :], p23_y[:, h, :], p23_rs[:], ALU.mult, ["p23_y", "p23_rs"], ["p23_y"])
            head_norm_store(l, b, g, d_mmo[b])

    arena_state["off"] = base_off
    p45_tmp = [SB([128, D], F32, f"p45_tmp{i}") for i in range(2)]
    p45_xo = [SB([128, D], F32, f"p45_xo{i}") for i in range(2)]
    p45_ss = SB([128, 4], F32, "p45_ss")
    base45 = arena_state["off"]
    p4_woa = SB([128, 4, D], BF16, "p4_woa")
    p4_wob = SB([64, 8, D], BF16, "p4_wob")
    p4_ms = [SB([128, 4, 512], BF16, f"p4_ms{i}") for i in range(2)]
    p4_mh = [SB([64, 8, 512], BF16, f"p4_mh{i}") for i in range(2)]

    def norm_resid_store(pb, xtile, xk, i2, dst_ap, dkey):
        for nh in range(2):
            jb, jk = nextjunk()
            ACT(jb[:, 0:512], psb[pb + nh][:], AF.Square, [f"ps{pb + nh}"], [jk, f"p45_ss{nh}"], scale=1.0 / 32,
                accum_out=p45_ss[:, nh:nh + 1])
        TT("dve", p45_ss[:, 2:3], p45_ss[:, 0:1], p45_ss[:, 1:2], ALU.add, ["p45_ss0", "p45_ss1"], ["p45_ss2"])
        ACT(p45_ss[:, 3:4], p45_ss[:, 2:3], AF.Ln, ["p45_ss2"], ["p45_ss3"], bias=EPS)
        ACT(p45_ss[:, 3:4], p45_ss[:, 3:4], AF.Exp, ["p45_ss3"], ["p45_ss3"], scale=-0.5)
        tmp, tk = p45_tmp[i2], f"p45_tmp{i2}"
        for nh in range(2):
            STT("dve", tmp[:, nh * 512:(nh + 1) * 512], psb[pb + nh][:], p45_ss[:, 3:4], gain[1][:, nh * 512:(nh + 1) * 512],
                ALU.mult, ALU.mult, [f"ps{pb + nh}", "p45_ss3", "gain1"], [tk])
        xo, xok = p45_xo[i2], f"p45_xo{i2}"
        TT("pool", xo[:], tmp[:], xtile, ALU.add, [tk, xk], [xok])
        DMA(dst_ap, xo[:], [xok], [dkey])

    def pass4(l, b, xsrc, srckey):
        load_gain(1, l, 1)
        DMA(p4_woa[:], w_out[l, 0:512, :].rearrange("(c p) n -> p c n", p=128), (), ["p4_woa"], eng="pool")
        DMA(p4_wob[:], w_out[l, 512:1024, :].rearrange("(j p) n -> p j n", p=64), (), ["p4_wob"], eng="pool")
        n = 0
        for g in range(NG):
            t0 = g * 512
            ms, mh = p4_ms[g % 2], p4_mh[g % 2]
            msk, mhk = f"p4_ms{g % 2}", f"p4_mh{g % 2}"
            DMA(ms[:], d_ssd[b].rearrange("(c p) s -> p c s", p=128)[:, :, t0:t0 + 512], [f"d_ssd{b}_{g}"], [msk])
            DMA(mh[:, 0:4, :], d_msb[b].rearrange("h d s -> d h s")[:, :, t0:t0 + 512], [f"d_msb{b}_{g}"], [mhk])
            DMA(mh[:, 4:8, :], d_mmo[b].rearrange("h d s -> d h s")[:, :, t0:t0 + 512], [f"d_mmo{b}_{g}"], [mhk])
            for tt in range(4):
                tok = slice(t0 + tt * 128, t0 + (tt + 1) * 128)
                xi = n % 4
                DMA(xt[xi][:], xsrc[tok, :], [f"{srckey}_{(t0 + tt * 128) // 1024}"], [f"xt{xi}"])
                pb = 4 + 2 * (n % 2)
                tsl = slice(tt * 128, (tt + 1) * 128)
                for nh in range(2):
                    cs = slice(nh * 512, (nh + 1) * 512)
                    for c in range(4):
                        MM(psb[pb + nh][:], ms[:, c, tsl], p4_woa[:, c, cs], c == 0, False, [msk, "p4_woa"], [f"ps{pb + nh}"])
                    for j in range(8):
                        MM(psb[pb + nh][:], mh[:, j, tsl], p4_wob[:, j, cs], False, j == 7, [mhk, "p4_wob"], [f"ps{pb + nh}"])
                norm_resid_store(pb, xt[xi][:], f"xt{xi}", n % 2, d_x1[b][tok, :], f"d_x1{b}_{(t0 + tt * 128) // 1024}")
                n += 1

    arena_state["off"] = base45
    p5_hT = SB([128, 8, 1024], BF16, "p5_hT")
    p5_act = SB([128, NHC, 1024], BF16, "p5_act")
    p5_wd = SB([128, NHC, D], BF16, "p5_wd")
    p5_wg = [SB([128, 8, 256], BF16, f"p5_wg{i}") for i in range(2)]
    p5_wu = [SB([128, 8, 256], BF16, f"p5_wu{i}") for i in range(2)]
    p5_sg = [SB([128, 512], F32, f"p5_sg{i}") for i in range(2)]

    def pass5(l, b, xdst, dstkey):
        load_gain(0, l, 2)
        load_gain(1, l, 3)
        for (c0, c1) in [(0, 11), (11, 22)]:
            DMA(p5_wd[:, c0:c1, :], w_down[l, c0 * 128:c1 * 128, :].rearrange("(c p) n -> p c n", p=128), (), ["p5_wd"], eng="pool")
        n = 0
        wi = 0
        for G in range(S // 1024 if not ng_limit else max(1, NG // 2)):
            T0 = G * 1024
            for tt in range(8):
                xi = n % 4
                n += 1
                tok = slice(T0 + tt * 128, T0 + (tt + 1) * 128)
                DMA(xt[xi][:], d_x1[b][tok, :], [f"d_x1{b}_{G}"], [f"xt{xi}"])
                jb, jk = nextjunk()
                ACT(jb[:], xt[xi][:], AF.Square, [f"xt{xi}"], [jk, "p5_ssa"], scale=1.0 / 32, accum_out=ss4[:, 0:1])
                ACT(rs4[:, 0:1], ss4[:, 0:1], AF.Ln, ["p5_ssa"], ["p5_rsa"], bias=EPS)
                ACT(rs4[:, 0:1], rs4[:, 0:1], AF.Exp, ["p5_rsa"], ["p5_rsa"], scale=-0.5)
                hbk = f"hb{tt % 2}"
                STT("dve", hb[tt % 2][:], xt[xi][:], rs4[:, 0:1], gain[0][:], ALU.mult, ALU.mult, [f"xt{xi}", "p5_rsa", "gain0"], [hbk])
                bk = 4 + (tt % 4)
                pT = psbf(bk)
                for c in range(8):
                    TR(pT[:, c * 128:(c + 1) * 128], hb[tt % 2][:, c * 128:(c + 1) * 128], ident_b[:], [hbk, "C_ident_b"], [f"ps{bk}"])
                CP("act" if tt % 2 else "dve", p5_hT[:, :, tt * 128:(tt + 1) * 128], pT.rearrange("p (c k) -> p c k", k=128),
                   [f"ps{bk}"], ["p5_hT"])
            it = 0
            for hq in range(0, NHC, 2):
                nq_ = min(2, NHC - hq)
                wg, wu = p5_wg[wi % 2], p5_wu[wi % 2]
                wgk, wuk = f"p5_wg{wi % 2}", f"p5_wu{wi % 2}"
                wi += 1
                cols = slice(hq * 128, (hq + nq_) * 128)
                DMA(wg[:, :, 0:nq_ * 128], w_gate[l, :, cols].rearrange("(c p) n -> p c n", p=128), (), [wgk], eng="pool")
                DMA(wu[:, :, 0:nq_ * 128], w_up[l, :, cols].rearrange("(c p) n -> p c n", p=128), (), [wuk], eng="pool")
                for hl in range(nq_):
                    hcx = hq + hl
                    for half in range(2):
                        i2 = it % 2
                        it += 1
                        ba, bb = 2 * i2, 2 * i2 + 1
                        hs = slice(half * 512, (half + 1) * 512)
                        for c in range(8):
                            MM(psb[ba][:], wg[:, c, hl * 128:(hl + 1) * 128], p5_hT[:, c, hs], c == 0, c == 7, [wgk, "p5_hT"], [f"ps{ba}"])
                        for c in range(8):
                            MM(psb[bb][:], wu[:, c, hl * 128:(hl + 1) * 128], p5_hT[:, c, hs], c == 0, c == 7, [wuk, "p5_hT"], [f"ps{bb}"])
                        ACT(p5_sg[i2][:], psb[ba][:], AF.Silu, [f"ps{ba}"], [f"p5_sg{i2}"])
                        TT("dve", p5_act[:, hcx, hs], p5_sg[i2][:], psb[bb][:], ALU.mult, [f"p5_sg{i2}", f"ps{bb}"], ["p5_act"])
            for tt in range(8):
                tok = slice(T0 + tt * 128, T0 + (tt + 1) * 128)
                xi = n % 4
                n += 1
                DMA(xt[xi][:], d_x1[b][tok, :], [f"d_x1{b}_{G}"], [f"xt{xi}"])
                pb = 4 + 2 * (tt % 2)
                tsl = slice(tt * 128, (tt + 1) * 128)
                for nh in range(2):
                    for hcx in range(NHC):
                        MM(psb[pb + nh][:], p5_act[:, hcx, tsl], p5_wd[:, hcx, nh * 512:(nh + 1) * 512], hcx == 0, hcx == NHC - 1,
                           ["p5_act", "p5_wd"], [f"ps{pb + nh}"])
                norm_resid_store(pb, xt[xi][:], f"xt{xi}", tt % 2, xdst[tok, :], f"{dstkey}_{G}")

    for l in range(DEPTH):
        for b in range(NSEQ):
            xsrc = x_in[b] if l == 0 else d_x2[b]
            srckey = f"xin{b}" if l == 0 else f"d_x2{b}"
            if 1 in passes:
                barrier()
                pass1(l, b, xsrc)
            if 2 in passes:
                barrier()
                pass2(l, b)
            if 3 in passes:
                barrier()
                pass3(l, b)
            if 4 in passes:
                barrier()
                pass4(l, b, xsrc, srckey)
            if 5 in passes:
                barrier()
                if l == DEPTH - 1:
                    pass5(l, b, out[b], f"out{b}")
                else:
                    pass5(l, b, d_x2[b], f"d_x2{b}")
    finals = list(P.dma_prev.values())
    P.emit(final_waits=finals)
    return nc, hc


def prep_inputs(inputs, DEPTH):
    f = lambda a: np.ascontiguousarray(np.asarray(a, dtype=np.float32))
    d = {}
    for k in ["w_in", "w_out", "w_gate", "w_up", "w_down"]:
        d[k] = f(inputs[k])
    d["vec1024"] = f(np.stack([inputs["pre_mix_norm"], inputs["post_mix_norm"], inputs["pre_ffn_norm"], inputs["post_ffn_norm"]], axis=1))
    cwv = np.asarray(inputs["conv_w"], np.float32)
    d["convw"] = f(cwv.reshape(DEPTH, 4, 8, 128).transpose(0, 3, 2, 1))
    d["convb"] = f(np.asarray(inputs["conv_b"], np.float32).reshape(DEPTH, 8, 128).transpose(0, 2, 1))
    d["hv"] = f(np.stack([inputs["dt_bias"], inputs["a_log"], inputs["d_skip"]], axis=1))
    d["ssdn"] = f(inputs["ssd_norm"])
    d["sbn"] = f(np.asarray(inputs["sb_norm"], np.float32).reshape(DEPTH, 4, 64).transpose(0, 2, 1))
    d["mon"] = f(np.asarray(inputs["moba_norm"], np.float32).reshape(DEPTH, 4, 64).transpose(0, 2, 1))
    return d


def kernel(**inputs):
    x = np.asarray(inputs["x"], np.float32)
    B, S, _ = x.shape
    DEPTH = inputs["w_in"].shape[0]
    NCORE = 8
    NSEQ = B // NCORE
    nc, hc = build(S, NSEQ, DEPTH)
    shared = prep_inputs(inputs, DEPTH)
    for k, v in hc.items():
        shared["c_" + k] = v
    in_maps = []
    for c in range(NCORE):
        m = dict(shared)
        m["x"] = np.ascontiguousarray(x[c * NSEQ:(c + 1) * NSEQ])
        in_maps.append(m)
    res = run_bass_kernel_spmd(nc, in_maps, core_ids=list(range(NCORE)))
    return np.concatenate([r["out"] for r in res.results], axis=0).astype(np.float32)
```

```python
import contextlib
import numpy as np
import ml_dtypes
import concourse.bass as bass
import concourse.mybir as mybir
from concourse.bass_utils import run_bass_kernel_spmd

F32 = mybir.dt.float32
BF16 = mybir.dt.bfloat16
AF = mybir.ActivationFunctionType
ALU = mybir.AluOpType
AX = mybir.AxisListType

D = 1024
INC = 3080
FH = 2816
NHC = 22
EPS = 1e-6
BIG = 30000.0
ENGS = ["pe", "act", "dve", "pool", "sp"]
EPOCH = 12000
NDMA = 12


class Prog:
    def __init__(self, nc):
        self.nc = nc
        self.ins = {e: [] for e in ENGS}
        self.lastw = {}
        self.readers = {}
        self.dma_slot = {e: 0 for e in ENGS}
        self.dma_prev = {}

    def add(self, eng, fn, reads=(), writes=(), dma=False):
        lst = self.ins[eng]
        me = dict(eng=eng, idx=len(lst), fn=fn, deps=set(), dma=dma, sig=False)
        deps = me["deps"]
        for r in reads:
            w = self.lastw.get(r)
            if w is not None:
                deps.add((w["eng"], w["idx"]))
        for wr in writes:
            w = self.lastw.get(wr)
            if w is not None and (w["eng"] != eng or w["dma"] or dma or eng != "pe"):
                deps.add((w["eng"], w["idx"]))
            for rd in self.readers.get(wr, ()):
                if rd["eng"] != eng or rd["dma"] or dma or eng != "pe":
                    deps.add((rd["eng"], rd["idx"]))
        if dma:
            slot = self.dma_slot[eng]
            self.dma_slot[eng] = (slot + 1) % NDMA
            me["slot"] = slot
            prev = self.dma_prev.get((eng, slot))
            if prev is not None:
                deps.add((prev["eng"], prev["idx"]))
            self.dma_prev[(eng, slot)] = me
        deps.discard((eng, me["idx"]))
        for r in reads:
            self.readers.setdefault(r, []).append(me)
        for wr in writes:
            self.lastw[wr] = me
            self.readers[wr] = []
        lst.append(me)
        return me

    def emit(self, final_waits=()):
        nc = self.nc
        for e in ENGS:
            for me in self.ins[e]:
                for (pe, pi) in me["deps"]:
                    self.ins[pe][pi]["sig"] = True
        for me in final_waits:
            me["sig"] = True
        nep = {}
        for e in ENGS:
            c = 0
            for me in self.ins[e]:
                if me["dma"]:
                    continue
                if me["sig"]:
                    c += 1
                    me["cnt"] = c
            nep[e] = c // EPOCH + 1
        dcount = {}
        for e in ENGS:
            for me in self.ins[e]:
                if me["dma"]:
                    k = (e, me["slot"])
                    dcount[k] = dcount.get(k, 0) + 16
                    me["cnt"] = dcount[k]
        with contextlib.ExitStack() as st:
            esem = {e: [st.enter_context(nc.semaphore(f"s_{e}_{i}")) for i in range(nep[e])] for e in ENGS}
            dsem = {}
            for k in dcount:
                dsem[k] = st.enter_context(nc.semaphore(f"d_{k[0]}_{k[1]}"))
            block = st.enter_context(nc.Block())

            def target(p):
                if p["dma"]:
                    return dsem[(p["eng"], p["slot"])], p["cnt"]
                c = p["cnt"]
                ep = (c - 1) // EPOCH
                return esem[p["eng"]][ep], c - ep * EPOCH

            def run(e, engobj):
                waited = {}
                for me in self.ins[e]:
                    best = {}
                    for (pe, pi) in me["deps"]:
                        p = self.ins[pe][pi]
                        key = ("d", pe, p["slot"]) if p["dma"] else ("e", pe)
                        if key not in best or best[key]["idx"] < p["idx"]:
                            best[key] = p
                    for key, p in best.items():
                        if waited.get(key, 0) >= p["cnt"]:
                            continue
                        waited[key] = p["cnt"]
                        sem, val = target(p)
                        engobj.wait_ge(sem, val)
                    r = me["fn"](engobj)
                    if me["dma"]:
                        r.then_inc(dsem[(e, me["slot"])], 16)
                    elif me["sig"]:
                        sem, _ = target(me)
                        r.then_inc(sem, 1)
                if e == "sp":
                    for p in final_waits:
                        sem, val = target(p)
                        engobj.wait_ge(sem, val)

            block.tensor(lambda eng: run("pe", eng))
            block.scalar(lambda eng: run("act", eng))
            block.vector(lambda eng: run("dve", eng))
            block.gpsimd(lambda eng: run("pool", eng))
            block.sync(lambda eng: run("sp", eng))


def host_consts(S):
    NBLK = S // 256
    p = np.arange(128)
    c = {}
    c["ident_f"] = np.eye(128, dtype=np.float32)
    c["tri_incl"] = (p[:, None] <= p[None, :]).astype(np.float32)
    c["tri_strict"] = (p[:, None] < p[None, :]).astype(np.float32)
    c["ones_f"] = np.ones((128, 128), np.float32)
    bf = ml_dtypes.bfloat16
    c["ut_b"] = (p[:, None] >= p[None, :]).astype(bf)
    c["ones_b"] = np.ones((128, 512), bf)
    c["zeros_b"] = np.zeros((128, 128), bf)
    nm = np.where(p[None, :] < p[:, None], -BIG, 0.0).astype(np.float32)
    c["negmask_b"] = np.tile(nm[:, None, :], (1, 8, 1)).reshape(128, 1024).astype(bf)
    own = np.arange(NBLK + 1)
    n = np.arange(NBLK)
    vm = np.where(n[None, :] < own[:, None], 0.0, -1e30).astype(np.float32)
    om = (n[None, :] == own[:, None]).astype(np.float32)
    c["vmask"] = np.tile(vm.reshape(1, -1), (128, 1))
    c["ownmask"] = np.tile(om.reshape(1, -1), (128, 1))
    s = np.arange(S)
    kaug = np.zeros((NBLK + 1, S), np.float32)
    kaug[s // 256, s] = 1.0
    kaug[NBLK, :] = 1.0
    c["kaug"] = kaug.astype(bf)
    slopes = 2.0 ** (-8.0 * np.arange(1, 5) / 4)
    c["alibi_row"] = (-slopes[:, None] * np.arange(512)[None, :]).astype(bf)
    rel = np.arange(32)
    ab = slopes[None, :, None] * (p[:, None, None] + 384 - 128 * rel[None, None, :])
    c["alibi_kb"] = ab.astype(np.float32)
    return c


import os as _os
KSTOP = int(_os.environ.get('KSTOP', '0'))


def build(S, NSEQ, DEPTH, dbg=False, passes=(1, 2, 3, 4, 5), ng_limit=None):
    nc = bass.Bass("TRN2", target_bir_lowering=False)
    NG, NT, NBLK = S // 512, S // 128, S // 256
    if ng_limit:
        NG = ng_limit
    KA = 64 + NBLK + 1
    hc = host_consts(S)

    def din(name, shape, dt=F32):
        return nc.dram_tensor(name, list(shape), dt, kind="ExternalInput").ap()

    def dscr(name, shape, dt):
        return nc.dram_tensor(name, list(shape), dt, kind=("ExternalOutput" if dbg else "Internal")).ap()

    x_in = din("x", [NSEQ, S, D])
    w_in = din("w_in", [DEPTH, D, INC])
    w_out = din("w_out", [DEPTH, D, D])
    w_gate = din("w_gate", [DEPTH, D, FH])
    w_up = din("w_up", [DEPTH, D, FH])
    w_down = din("w_down", [DEPTH, FH, D])
    vec1024 = din("vec1024", [DEPTH, 4, D])
    convw = din("convw", [DEPTH, 128, 8, 4])
    convb = din("convb", [DEPTH, 128, 8])
    hv = din("hv", [DEPTH, 3, 8])
    ssdn = din("ssdn", [DEPTH, 512])
    sbn = din("sbn", [DEPTH, 64, 4])
    mon = din("mon", [DEPTH, 64, 4])
    cd = {}
    for k, v in hc.items():
        cd[k] = din("c_" + k, v.shape, BF16 if v.dtype == ml_dtypes.bfloat16 else F32)
    out = nc.dram_tensor("out", [NSEQ, S, D], F32, kind="ExternalOutput").ap()

    d_ssd = [dscr(f"d_ssd{b}", [512, S], BF16) for b in range(NSEQ)]
    d_sbq = [dscr(f"d_sbq{b}", [256, S], BF16) for b in range(NSEQ)]
    d_sbk = [dscr(f"d_sbk{b}", [256, S], BF16) for b in range(NSEQ)]
    d_sbv = [dscr(f"d_sbv{b}", [S, 256], BF16) for b in range(NSEQ)]
    d_moq = [dscr(f"d_moq{b}", [256, S], BF16) for b in range(NSEQ)]
    d_mok = [dscr(f"d_mok{b}", [256, S], BF16) for b in range(NSEQ)]
    d_mov = [dscr(f"d_mov{b}", [S, 256], BF16) for b in range(NSEQ)]
    d_sel = [dscr(f"d_sel{b}", [4 * NBLK, S], BF16) for b in range(NSEQ)]
    d_msb = [dscr(f"d_msb{b}", [4, 64, S], BF16) for b in range(NSEQ)]
    d_mmo = [dscr(f"d_mmo{b}", [4, 64, S], BF16) for b in range(NSEQ)]
    d_x1 = [dscr(f"d_x1{b}", [S, D], F32) for b in range(NSEQ)]
    d_x2 = [dscr(f"d_x2{b}", [S, D], F32) for b in range(NSEQ)]

    dbg_t = dscr("dbg_t", [128, 256], F32)
    P = Prog(nc)
    sb_ = nc.alloc_sbuf_tensor
    _n = [0]
    ARENA_BYTES = 192 * 1024
    arena_state = {"on": False, "off": 0, "t": None}

    def SB(shape, dt, name=None):
        _n[0] += 1
        if not arena_state["on"]:
            return sb_(name or f"t{_n[0]}", list(shape), dt)
        free = int(np.prod(shape[1:]))
        nb = free * (2 if dt == BF16 else 4)
        nb = (nb + 63) // 64 * 64
        off = arena_state["off"]
        assert off + nb <= ARENA_BYTES, (name, off, nb)
        ap = arena_state["t"][0:shape[0], off // 4:(off + nb) // 4]
        if dt == BF16:
            ap = ap.bitcast(BF16)
        ap = ap[:, 0:free]
        if len(shape) == 3:
            ap = ap.rearrange("p (a b) -> p a b", b=shape[2])
        elif len(shape) == 4:
            ap = ap.rearrange("p (a b c) -> p a b c", b=shape[2], c=shape[3])
        arena_state["off"] = off + nb
        return ap

    def DMA(out_, in_, reads, writes, eng="sp"):
        return P.add(eng, lambda e: e.dma_start(out=out_, in_=in_), reads, writes, dma=True)

    def MM(out_, lhsT, rhs, start, stop, reads, writes):
        return P.add("pe", lambda e: e.matmul(out_, lhsT=lhsT, rhs=rhs, start=start, stop=stop), reads, writes)

    def TR(out_, in_, ident, reads, writes):
        return P.add("pe", lambda e: e.transpose(out=out_, in_=in_, identity=ident), reads, writes)

    def ACT(out_, in_, func, reads, writes, **kw):
        return P.add("act", lambda e: e.activation(out=out_, in_=in_, func=func, **kw), reads, writes)

    def TT(eng, out_, in0, in1, op, reads, writes):
        return P.add(eng, lambda e: e.tensor_tensor(out=out_, in0=in0, in1=in1, op=op), reads, writes)

    def TS(eng, out_, in0, s1, s2, op0, op1, reads, writes):
        if s2 is None:
            return P.add(eng, lambda e: e.tensor_scalar(out=out_, in0=in0, scalar1=s1, scalar2=None, op0=op0), reads, writes)
        return P.add(eng, lambda e: e.tensor_scalar(out=out_, in0=in0, scalar1=s1, scalar2=s2, op0=op0, op1=op1), reads, writes)

    def STT(eng, out_, in0, scalar, in1, op0, op1, reads, writes):
        return P.add(eng, lambda e: e.scalar_tensor_tensor(out=out_, in0=in0, scalar=scalar, in1=in1, op0=op0, op1=op1), reads, writes)

    def CP(eng, out_, in_, reads, writes):
        if eng == "act":
            return ACT(out_, in_, AF.Copy, reads, writes)
        return P.add(eng, lambda e: e.tensor_copy(out=out_, in_=in_), reads, writes)

    def MS(eng, ap, val, writes):
        return P.add(eng, lambda e: e.memset(ap, val), (), writes)

    psb = [nc.alloc_psum_tensor(f"ps{i}", [128, 512], F32) for i in range(8)]

    def psbf(i):
        return psb[i][:].bitcast(BF16)

    ident_f = SB([128, 128], F32, "ident_f")
    ident_b = SB([128, 128], BF16, "ident_b")
    tri_incl = SB([128, 128], F32, "tri_incl")
    tri_strict = SB([128, 128], F32, "tri_strict")
    ones_f = SB([128, 128], F32, "ones_f")
    ut_b = SB([128, 128], BF16, "ut_b")
    ones_b = SB([128, 512], BF16, "ones_b")
    zeros_b = SB([128, 128], BF16, "zeros_b")
    negmask_b = SB([128, 1024], BF16, "negmask_b")
    vmask = SB([128, (NBLK + 1) * NBLK], F32, "vmask")
    ownmask = SB([128, (NBLK + 1) * NBLK], F32, "ownmask")
    alibi_kb = SB([128, 4, 32], F32, "alibi_kb")
    for nm_, t in [("ident_f", ident_f), ("tri_incl", tri_incl), ("tri_strict", tri_strict), ("ones_f", ones_f),
                   ("ut_b", ut_b), ("ones_b", ones_b), ("zeros_b", zeros_b), ("negmask_b", negmask_b),
                   ("vmask", vmask), ("ownmask", ownmask), ("alibi_kb", alibi_kb)]:
        DMA(t[:], cd[nm_], (), ["C_" + nm_])
    CP("dve", ident_b[:], ident_f[:], ["C_ident_f"], ["C_ident_b"])
    CONST = ["C_ident_f", "C_ident_b", "C_tri_incl", "C_tri_strict", "C_ones_f", "C_ut_b", "C_ones_b", "C_zeros_b",
             "C_negmask_b", "C_vmask", "C_ownmask", "C_alibi_kb"]

    bar_sb = {e: SB([128, 8], F32, f"bar_{e}") for e in ("act", "dve", "pool")}
    bar_dram = nc.dram_tensor("bar_dram", [2, 64], F32).ap()

    def barrier():
        keys = []
        P.add("pe", lambda e: e.matmul(psb[7][:, 0:8], lhsT=zeros_b[:, 0:128], rhs=zeros_b[:, 0:8], start=True, stop=True),
              ["C_zeros_b"], ["ps7", "bar_pe"])
        for en in ("act", "dve", "pool"):
            CP(en, bar_sb[en][:], ones_f[:, 0:8], ["C_ones_f"], [f"bar_{en}"])
        allk = ["bar_pe", "bar_act", "bar_dve", "bar_pool"]
        dmas = {(d["eng"], d["idx"]) for d in P.dma_prev.values()}
        m = P.add("pe", lambda e: e.matmul(psb[7][:, 0:8], lhsT=zeros_b[:, 0:128], rhs=zeros_b[:, 0:8], start=True, stop=True),
                  ["C_zeros_b"] + allk, ["ps7", "bar2_pe"])
        m["deps"] |= dmas
        for en in ("act", "dve", "pool"):
            m = CP(en, bar_sb[en][:], ones_f[:, 0:8], ["C_ones_f"] + allk, [f"bar2_{en}"])
            m["deps"] |= dmas
        m = DMA(bar_dram[1:2, :], cd["ones_f"][0:1, 0:64], allk, ["bar2_sp"])
        m["deps"] |= {d for d in dmas if d != (m["eng"], m["idx"])}

    arena_state["t"] = sb_("arena", [128, ARENA_BYTES // 4], F32)
    arena_state["on"] = True
    gain = [SB([128, D], F32, f"gain{i}") for i in range(2)]
    xt = [SB([128, D], F32, f"xt{i}") for i in range(4)]
    junks = [SB([128, D], BF16, f"junk{i}") for i in range(4)]
    _jr = [0]

    def nextjunk():
        _jr[0] = (_jr[0] + 1) % 4
        return junks[_jr[0]], f"junk{_jr[0]}"
    ss4 = SB([128, 8], F32, "ss4")
    rs4 = SB([128, 8], F32, "rs4")
    hb = [SB([128, D], BF16, f"hb{i}") for i in range(2)]

    def load_gain(slot, l, which):
        DMA(gain[slot][:], vec1024[l, which].partition_broadcast(128), (), [f"gain{slot}"])

    base_off = arena_state["off"]

    win = SB([128, 8, INC], BF16, "win")
    hT = SB([128, 8, 512], BF16, "hT")
    zs = SB([128, 4, 512], F32, "zs")
    ubuf = [SB([128, 515], F32, f"u{i}") for i in range(2)]
    halo = SB([128, 8, 3], F32, "halo")
    cta = [SB([128, 512], F32, f"cta{i}") for i in range(2)]
    ctb = [SB([128, 512], F32, f"ctb{i}") for i in range(2)]
    xsT = SB([128, 4, 512], F32, "xsT")
    xs_tok = [SB([128, 512], F32, f"xstok{i}") for i in range(4)]
    BT = SB([128, 2, 512], BF16, "BT")
    CT = SB([128, 2, 512], BF16, "CT")
    Btok = SB([128, 4, 2, 128], BF16, "Btok")
    cw = SB([128, 8, 4], F32, "cw")
    cb = SB([128, 8], F32, "cb")
    hv_bc = SB([128, 3, 8], F32, "hv_bc")
    a_bc = SB([128, 8], F32, "a_bc")
    dsk_bc = SB([128, 8, 64], F32, "dsk_bc")
    ssdn_bc = SB([128, 512], F32, "ssdn_bc")
    dtx = SB([128, 4, 8], F32, "dtx")
    dt_all = SB([128, 4, 8], F32, "dt_all")
    adt = SB([128, 4, 8], F32, "adt")
    sbq_st = SB([128, 2, 512], BF16, "sbq_st")
    sbk_st = SB([128, 2, 512], BF16, "sbk_st")
    sbv_st = SB([128, 4, 256], BF16, "sbv_st")
    moq32 = SB([128, 2, 512], F32, "moq32")
    mok32 = SB([128, 2, 512], F32, "mok32")
    moq_st = SB([128, 2, 512], BF16, "moq_st")
    mok_st = SB([128, 2, 512], BF16, "mok_st")
    mov_st = SB([128, 4, 256], BF16, "mov_st")
    kmean = SB([128, 2, 2, NBLK], F32, "kmean")
    gm = SB([128, 4, NBLK], F32, "gm")
    max8 = SB([128, 4, 8], F32, "max8")
    thr = SB([128, 4], F32, "thr")
    sel = SB([128, 4, NBLK], F32, "sel")
    selneg = SB([128, 4 * NBLK], BF16, "selneg")
    selT_st = SB([64, 512], BF16, "selT_st")
    rhs_cum = SB([128, 8, 128], F32, "rhs_cum")
    negac = SB([128, 8], F32, "negac")
    ea = SB([128, 8], F32, "ea")
    cdb = SB([128, 8], F32, "cdb")
    Eb = SB([128, 8, 128], F32, "Eb")
    MT = SB([128, 8, 128], BF16, "MT")
    dtd = SB([128, 8], F32, "dtd")
    xdt = SB([128, 8, 64], BF16, "xdt")
    xdt2 = SB([128, 8, 64], BF16, "xdt2")
    S32 = SB([128, 512], F32, "S32")
    Sb = SB([128, 512], BF16, "Sb")
    y1 = SB([128, 512], F32, "y1")
    y2 = SB([128, 512], F32, "y2")
    y3 = SB([128, 512], F32, "y3")
    ssq = SB([128, 2], F32, "ssq")
    rsq = SB([128, 2], F32, "rsq")
    yn = SB([128, 512], BF16, "yn")
    mixT_ssd = SB([128, 4, 512], BF16, "mixT_ssd")

    rot01 = [0]

    def nextbank():
        rot01[0] ^= 1
        return rot01[0]

    def load_layer_p1(l):
        for (c0, c1) in [(0, 512), (512, 1536), (1536, 2312), (2312, INC)]:
            DMA(win[:, :, c0:c1], w_in[l, :, c0:c1].rearrange("(c p) n -> p c n", p=128), (), ["win"], eng="pool")
        DMA(cw[:], convw[l], (), ["cw"])
        DMA(cb[:], convb[l], (), ["cb"])
        for i in range(3):
            DMA(hv_bc[:, i, :], hv[l, i].partition_broadcast(128), (), ["hv_bc"])
        DMA(ssdn_bc[:], ssdn[l].partition_broadcast(128), (), ["ssdn_bc"])
        ACT(a_bc[:], hv_bc[:, 1, :], AF.Exp, ["hv_bc"], ["a_bc"])
        TS("dve", a_bc[:], a_bc[:], -1.0, None, ALU.mult, None, ["a_bc"], ["a_bc"])
        CP("dve", dsk_bc[:], hv_bc[:, 2, :].unsqueeze(2).to_broadcast([128, 8, 64]), ["hv_bc"], ["dsk_bc"])

    def pass1(l, b, xsrc):
        load_layer_p1(l)
        load_gain(0, l, 0)
        MS("pool", S32[:], 0.0, ["S32"])
        MS("pool", Sb[:], 0.0, ["Sb"])
        MS("pool", halo[:], 0.0, ["halo"])
        MS("pool", kmean[:], 0.0, ["kmean"])
        for g in range(NG):
            t0 = g * 512
            for tt in range(4):
                DMA(xt[tt][:], xsrc[t0 + tt * 128:t0 + (tt + 1) * 128, :], (), [f"xt{tt}"])
            for tt in range(4):
                jb, jk = nextjunk()
                ACT(jb[:], xt[tt][:], AF.Square, [f"xt{tt}"], [jk, f"ss4_{tt}"], scale=1.0 / 32, accum_out=ss4[:, tt:tt + 1])
            ACT(rs4[:, 0:4], ss4[:, 0:4], AF.Ln, [f"ss4_{i}" for i in range(4)], ["rs4"], bias=EPS)
            ACT(rs4[:, 0:4], rs4[:, 0:4], AF.Exp, ["rs4"], ["rs4"], scale=-0.5)
            for tt in range(4):
                hbk = f"hb{tt % 2}"
                STT("dve", hb[tt % 2][:], xt[tt][:], rs4[:, tt:tt + 1], gain[0][:], ALU.mult, ALU.mult,
                    [f"xt{tt}", "rs4", "gain0"], [hbk])
                bk = nextbank()
                pT = psbf(bk)
                for c in range(8):
                    TR(pT[:, c * 128:(c + 1) * 128], hb[tt % 2][:, c * 128:(c + 1) * 128], ident_b[:], [hbk, "C_ident_b"], [f"ps{bk}"])
                CP("act" if tt % 2 else "dve", hT[:, :, tt * 128:(tt + 1) * 128], pT.rearrange("p (c k) -> p c k", k=128),
                   [f"ps{bk}"], ["hT"])

            def proj_fm(col0, ncols=128):
                bk = nextbank()
                for c in range(8):
                    MM(psb[bk][0:ncols, :], win[:, c, col0:col0 + ncols], hT[:, c, :], c == 0, c == 7, ["win", "hT"], [f"ps{bk}"])
                return bk

            def proj_tm(tt, col0, ncols, bk, off):
                for c in range(8):
                    MM(psb[bk][:, off:off + ncols], hT[:, c, tt * 128:(tt + 1) * 128], win[:, c, col0:col0 + ncols], c == 0, c == 7,
                       ["win", "hT"], [f"ps{bk}"])

            if KSTOP == 1:
                return
            for tt in range(4):
                bk = nextbank()
                proj_tm(tt, 0, 512, bk, 0)
                ACT(zs[:, tt, :], psb[bk][:], AF.Silu, [f"ps{bk}"], ["zs"])
            if KSTOP == 2:
                return
            for tt in range(4):
                for c in range(8):
                    MM(psb[4][:, 400 + tt * 8:408 + tt * 8], hT[:, c, tt * 128:(tt + 1) * 128], win[:, c, 1536:1544], c == 0, c == 7,
                       ["win", "hT"], ["ps4"])
            TT("dve", dtx[:], psb[4][:, 400:432].rearrange("p (t h) -> p t h", h=8),
               hv_bc[:, 0, :].unsqueeze(1).to_broadcast([128, 4, 8]), ALU.add, ["ps4", "hv_bc"], ["dtx"])
            ACT(dtx[:], dtx[:], AF.Exp, ["dtx"], ["dtx"])
            ACT(dt_all[:], dtx[:], AF.Ln, ["dtx"], ["dt_all"], bias=1.0)
            TT("dve", adt[:], dt_all[:], a_bc[:].unsqueeze(1).to_broadcast([128, 4, 8]), ALU.mult, ["dt_all", "a_bc"], ["adt"])
            if KSTOP == 3:
                return
            for j in range(8):
                bk = proj_fm(512 + 128 * j)
                u = ubuf[j % 2]
                uk = f"u{j % 2}"
                CP("dve", u[:, 0:3], halo[:, j, :], ["halo"], [uk])
                CP("dve", u[:, 3:515], psb[bk][:], [f"ps{bk}"], [uk])
                CP("pool", halo[:, j, :], u[:, 512:515], [uk], ["halo"])
                ta = cta[j % 2]
                tk = f"cta{j % 2}"
                ACT(ta[:], u[:, 0:512], AF.Copy, [uk, "cw"], [tk], scale=cw[:, j, 0:1])
                for k_ in range(1, 4):
                    STT("dve", ta[:], u[:, k_:k_ + 512], cw[:, j, k_:k_ + 1], ta[:], ALU.mult, ALU.add, [uk, "cw", tk], [tk])
                if j < 4:
                    dst, dk = xsT[:, j, :], "xsT"
                elif j < 6:
                    dst, dk = BT[:, j - 4, :], "BT"
                else:
                    dst, dk = CT[:, j - 6, :], "CT"
                ACT(dst, ta[:], AF.Silu, [tk, "cb"], [dk], bias=cb[:, j:j + 1])
            if KSTOP == 4:
                return
            for pr in range(2):
                bk = proj_fm(1544 + 128 * pr)
                ACT(sbq_st[:, pr, :], psb[bk][:], AF.Copy, [f"ps{bk}"], ["sbq_st"], scale=0.125)
                bk = proj_fm(1800 + 128 * pr)
                CP("dve", sbk_st[:, pr, :], psb[bk][:], [f"ps{bk}"], ["sbk_st"])
            for t2 in range(2):
                bk = nextbank()
                for q in range(2):
                    proj_tm(2 * t2 + q, 2056, 256, bk, q * 256)
                CP("act", sbv_st[:, 2 * t2:2 * t2 + 2, :], psb[bk][:].rearrange("p (t c) -> p t c", c=256), [f"ps{bk}"], ["sbv_st"])
            DMA(d_sbq[b].rearrange("(r p) s -> p r s", p=128)[:, :, t0:t0 + 512], sbq_st[:], ["sbq_st"], [f"d_sbq{b}_{g}"])
            DMA(d_sbk[b].rearrange("(r p) s -> p r s", p=128)[:, :, t0:t0 + 512], sbk_st[:], ["sbk_st"], [f"d_sbk{b}_{g}"])
            DMA(d_sbv[b][t0:t0 + 512, :].rearrange("(t p) c -> p t c", p=128), sbv_st[:], ["sbv_st"], [f"d_sbv{b}_{g}"])
            if KSTOP == 5:
                return
            for pr in range(2):
                bk = proj_fm(2312 + 128 * pr)
                CP("dve", moq32[:, pr, :], psb[bk][:], [f"ps{bk}"], ["moq32"])
                TS("pool", moq_st[:, pr, :], moq32[:, pr, :], 0.125, None, ALU.mult, None, ["moq32"], ["moq_st"])
                bk = proj_fm(2568 + 128 * pr)
                CP("act", mok32[:, pr, :], psb[bk][:], [f"ps{bk}"], ["mok32"])
                CP("pool", mok_st[:, pr, :], mok32[:, pr, :], ["mok32"], ["mok_st"])
                for e_ in range(2):
                    rows = slice(e_ * 64, e_ * 64 + 64)
                    P.add("dve", lambda e, pr=pr, g=g, e_=e_, rows=rows: e.tensor_reduce(
                        out=kmean[rows, pr, e_, 2 * g:2 * g + 2], in_=mok32[rows, pr, :].rearrange("p (b k) -> p b k", k=256),
                        axis=AX.X, op=ALU.add), ["mok32"], ["kmean"])
            for t2 in range(2):
                bk = nextbank()
                for q in range(2):
                    proj_tm(2 * t2 + q, 2824, 256, bk, q * 256)
                CP("dve", mov_st[:, 2 * t2:2 * t2 + 2, :], psb[bk][:].rearrange("p (t c) -> p t c", c=256), [f"ps{bk}"], ["mov_st"])
            DMA(d_moq[b].rearrange("(r p) s -> p r s", p=128)[:, :, t0:t0 + 512], moq_st[:], ["moq_st"], [f"d_moq{b}_{g}"])
            DMA(d_mok[b].rearrange("(r p) s -> p r s", p=128)[:, :, t0:t0 + 512], mok_st[:], ["mok_st"], [f"d_mok{b}_{g}"])
            DMA(d_mov[b][t0:t0 + 512, :].rearrange("(t p) c -> p t c", p=128), mov_st[:], ["mov_st"], [f"d_mov{b}_{g}"])
            if KSTOP == 6:
                return
            gps = psb[5][:, 0:16 * NBLK].rearrange("p (t h n) -> p t h n", t=4, h=4)
            for tt in range(4):
                for pr in range(2):
                    MM(gps[:, tt, 2 * pr:2 * pr + 2, :], moq32[:, pr, tt * 128:(tt + 1) * 128],
                       kmean[:, pr, :, :], True, True, ["moq32", "kmean"], ["ps5"])
            if KSTOP == 61:
                return
            for tt in range(4):
                own = 2 * g + tt // 2
                TT("dve", gm[:], gps[:, tt], vmask[:, own * NBLK:(own + 1) * NBLK].unsqueeze(1).to_broadcast([128, 4, NBLK]),
                   ALU.add, ["ps5", "C_vmask"], ["gm"])
                for h in range(4):
                    P.add("dve", lambda e, h=h: e.max(out=max8[:, h, :], in_=gm[:, h, :]), ["gm"], ["max8"])
                TS("dve", thr[:], max8[:, :, 2], -1e20, None, ALU.max, None, ["max8"], ["thr"])
                TT("dve", sel[:], gm[:], thr[:].unsqueeze(2).to_broadcast([128, 4, NBLK]), ALU.is_ge, ["gm", "thr"], ["sel"])
                TT("dve", sel[:], sel[:], ownmask[:, own * NBLK:(own + 1) * NBLK].unsqueeze(1).to_broadcast([128, 4, NBLK]),
                   ALU.add, ["sel", "C_ownmask"], ["sel"])
                TS("dve", selneg[:], sel[:].rearrange("p h n -> p (h n)"), -1.0, BIG, ALU.add, ALU.mult, ["sel"], ["selneg"])
                if dbg and g == NG - 1 and tt == 3:
                    DMA(dbg_t[:, 0:4 * NBLK], gm[:].rearrange("p h n -> p (h n)"), ["gm"], ["dbg_t"])
                    DMA(dbg_t[:, 64:96], max8[:].rearrange("p h n -> p (h n)"), ["max8"], ["dbg_t"])
                    DMA(dbg_t[:, 96:100], thr[:], ["thr"], ["dbg_t"])
                    DMA(dbg_t[:, 128:128 + 4 * NBLK], sel[:].rearrange("p h n -> p (h n)"), ["sel"], ["dbg_t"])
                if KSTOP == 62:
                    continue
                bk = nextbank()
                TR(psbf(bk)[0:4 * NBLK, 0:128], selneg[:], ident_b[:], ["selneg", "C_ident_b"], [f"ps{bk}"])
                CP("act", selT_st[0:4 * NBLK, tt * 128:(tt + 1) * 128], psbf(bk)[0:4 * NBLK, 0:128], [f"ps{bk}"], ["selT_st"])
            if KSTOP == 62:
                return
            DMA(d_sel[b][:, t0:t0 + 512], selT_st[0:4 * NBLK, :], ["selT_st"], [f"d_sel{b}_{g}"])
            if KSTOP == 7:
                return
            for tt in range(4):
                bk = nextbank()
                for j in range(4):
                    TR(psb[bk][:, j * 128:(j + 1) * 128], xsT[:, j, tt * 128:(tt + 1) * 128], ident_f[:], ["xsT", "C_ident_f"], [f"ps{bk}"])
                CP("act" if tt % 2 else "dve", xs_tok[tt][:], psb[bk][:], [f"ps{bk}"], [f"xstok{tt}"])
                bk = nextbank()
                for gi in range(2):
                    TR(psbf(bk)[:, gi * 128:(gi + 1) * 128], BT[:, gi, tt * 128:(tt + 1) * 128], ident_b[:], ["BT", "C_ident_b"], [f"ps{bk}"])
                CP("dve", Btok[:, tt, :, :], psbf(bk)[:, 0:256].rearrange("p (g n) -> p g n", n=128), [f"ps{bk}"], ["Btok"])
            if KSTOP == 8:
                return
            acb = None
            for tt in range(4):
                tsl = slice(tt * 128, (tt + 1) * 128)
                xs3 = xs_tok[tt][:].rearrange("p (h d) -> p h d", d=64)
                xk = f"xstok{tt}"
                TT("dve", rhs_cum[:], tri_incl[:].unsqueeze(1).to_broadcast([128, 8, 128]),
                   adt[:, tt, :].unsqueeze(2).to_broadcast([128, 8, 128]), ALU.mult, ["C_tri_incl", "adt"], ["rhs_cum"])
                rc = rhs_cum[:].rearrange("p h l -> p (h l)")
                for half in range(2):
                    pb = psb[2 + half]
                    MM(pb[:], ones_f[:], rc[:, half * 512:(half + 1) * 512], True, False, ["C_ones_f", "rhs_cum"], [f"ps{2 + half}"])
                    MM(pb[:], ident_b[:], negmask_b[:, half * 512:(half + 1) * 512], False, True, ["C_ident_b", "C_negmask_b"], [f"ps{2 + half}"])
                MM(psb[4][:, 0:8], tri_incl[:], adt[:, tt, :], True, True, ["C_tri_incl", "adt"], ["ps4"])
                ACT(negac[:], psb[4][:, 0:8], AF.Copy, ["ps4"], ["negac"], scale=-1.0)
                ACT(ea[:], psb[4][:, 0:8], AF.Exp, ["ps4"], ["ea"])
                for half in range(2):
                    ACT(cdb[:, half * 4:(half + 1) * 4], psb[2 + half][:].rearrange("p (h l) -> p h l", l=128)[:, :, 127], AF.Exp,
                        [f"ps{2 + half}"], ["cdb"])
                for h in range(8):
                    ACT(Eb[:, h, :], psb[2 + h // 4][:, (h % 4) * 128:(h % 4 + 1) * 128], AF.Exp, [f"ps{2 + h // 4}", "negac"], ["Eb"],
                        bias=negac[:, h:h + 1])
                for gi in range(2):
                    MM(psb[4][:, 128 + gi * 128:256 + gi * 128], BT[:, gi, tsl], CT[:, gi, tsl], True, True, ["BT", "CT"], ["ps4"])
                for gi in range(2):
                    TT("dve", MT[:, gi * 4:(gi + 1) * 4, :], Eb[:, gi * 4:(gi + 1) * 4, :],
                       psb[4][:, 128 + gi * 128:256 + gi * 128].unsqueeze(1).to_broadcast([128, 4, 128]), ALU.mult,
                       ["Eb", "ps4"], ["MT"])
                TT("dve", dtd[:], dt_all[:, tt, :], Eb[:, :, 127], ALU.mult, ["dt_all", "Eb"], ["dtd"])
                TT("dve", xdt[:], xs3, dt_all[:, tt, :].unsqueeze(2).to_broadcast([128, 8, 64]), ALU.mult, [xk, "dt_all"], ["xdt"])
                TT("pool", xdt2[:], xs3, dtd[:].unsqueeze(2).to_broadcast([128, 8, 64]), ALU.mult, [xk, "dtd"], ["xdt2"])
                x2f = xdt2[:].rearrange("p h d -> p (h d)")
                x1f = xdt[:].rearrange("p h d -> p (h d)")
                for gi in range(2):
                    MM(psb[5][:, gi * 256:(gi + 1) * 256], Btok[:, tt, gi, :], x2f[:, gi * 256:(gi + 1) * 256], True, True,
                       ["Btok", "xdt2"], ["ps5"])
                for gi in range(2):
                    MM(psb[6][:, gi * 256:(gi + 1) * 256], CT[:, gi, tsl], Sb[:, gi * 256:(gi + 1) * 256], True, True,
                       ["CT", "Sb"], ["ps6"])
                for h in range(8):
                    MM(psb[7][:, h * 64:(h + 1) * 64], MT[:, h, :], x1f[:, h * 64:(h + 1) * 64], True, True, ["MT", "xdt"], ["ps7"])
                TT("dve", S32[:].rearrange("p (h d) -> p h d", d=64), S32[:].rearrange("p (h d) -> p h d", d=64),
                   cdb[:].unsqueeze(2).to_broadcast([128, 8, 64]), ALU.mult, ["S32", "cdb"], ["S32"])
                TT("dve", S32[:], S32[:], psb[5][:], ALU.add, ["S32", "ps5"], ["S32"])
                CP("pool", Sb[:], S32[:], ["S32"], ["Sb"])
                TT("dve", y1[:].rearrange("p (h d) -> p h d", d=64), psb[6][:].rearrange("p (h d) -> p h d", d=64),
                   ea[:].unsqueeze(2).to_broadcast([128, 8, 64]), ALU.mult, ["ps6", "ea"], ["y1"])
                TT("dve", y1[:], y1[:], psb[7][:], ALU.add, ["y1", "ps7"], ["y1"])
                TT("pool", y2[:].rearrange("p (h d) -> p h d", d=64), xs3, dsk_bc[:], ALU.mult, [xk, "dsk_bc"], ["y2"])
                TT("pool", y2[:], y2[:], y1[:], ALU.add, ["y2", "y1"], ["y2"])
                TT("dve", y3[:], y2[:], zs[:, tt, :], ALU.mult, ["y2", "zs"], ["y3"])
                for gi in range(2):
                    jb, jk = nextjunk()
                    ACT(jb[:, 0:256], y3[:, gi * 256:(gi + 1) * 256], AF.Square, ["y3"], [jk, f"ssq{gi}"], scale=1.0 / 16,
                        accum_out=ssq[:, gi:gi + 1])
                ACT(rsq[:], ssq[:], AF.Ln, ["ssq0", "ssq1"], ["rsq"], bias=EPS)
                ACT(rsq[:], rsq[:], AF.Exp, ["rsq"], ["rsq"], scale=-0.5)
                for gi in range(2):
                    STT("dve", yn[:, gi * 256:(gi + 1) * 256], y3[:, gi * 256:(gi + 1) * 256], rsq[:, gi:gi + 1],
                        ssdn_bc[:, gi * 256:(gi + 1) * 256], ALU.mult, ALU.mult, ["y3", "rsq", "ssdn_bc"], ["yn"])
                bk = nextbank()
                for j in range(4):
                    TR(psbf(bk)[:, j * 128:(j + 1) * 128], yn[:, j * 128:(j + 1) * 128], ident_b[:], ["yn", "C_ident_b"], [f"ps{bk}"])
                CP("act", mixT_ssd[:, :, tsl], psbf(bk)[:, 0:512].rearrange("p (c k) -> p c k", k=128), [f"ps{bk}"], ["mixT_ssd"])
            DMA(d_ssd[b].rearrange("(c p) s -> p c s", p=128)[:, :, t0:t0 + 512], mixT_ssd[:], ["mixT_ssd"], [f"d_ssd{b}_{g}"])

    arena_state["off"] = base_off
    p2_kT = SB([128, 2, S], BF16, "p2_kT")
    p2_v = SB([128, NT, 256], BF16, "p2_v")
    p2_q = [SB([128, 2, 512], BF16, f"p2_q{i}") for i in range(3)]
    p2_nq = [SB([128, 2, 512], BF16, f"p2_nq{i}") for i in range(3)]
    p2_e = [SB([128, 512], F32, f"p2_e{i}") for i in range(2)]
    p2_sp = [SB([128, 512], BF16, f"p2_sp{i}") for i in range(2)]
    p2_S = SB([128, 512], BF16, "p2_S")
    p2_w = [SB([128, 512], BF16, f"p2_w{i}") for i in range(2)]
    p23_y = SB([64, 4, 512], F32, "p23_y")
    p23_sq = SB([64, 4, 512], BF16, "p23_sq")
    p23_rs = SB([64, 512], F32, "p23_rs")
    p23_mix = SB([64, 4, 512], BF16, "p23_mix")
    p23_nw = SB([64, 4], F32, "p23_nw")

    def head_norm_store(l, b, g, dst, nb=6):
        t0 = g * 512
        for h in range(4):
            ACT(p23_sq[:, h, :], p23_y[:, h, :], AF.Square, ["p23_y"], ["p23_sq"])
        for h in range(4):
            MM(psb[nb][0:64, :], ones_b[0:64, 0:64], p23_sq[:, h, :], h == 0, h == 3, ["C_ones_b", "p23_sq"], [f"ps{nb}"])
        ACT(p23_rs[:], psb[nb][0:64, :], AF.Ln, [f"ps{nb}"], ["p23_rs"], scale=1.0 / 256, bias=EPS)
        ACT(p23_rs[:], p23_rs[:], AF.Exp, ["p23_rs"], ["p23_rs"], scale=-0.5)
        for h in range(4):
            STT("dve", p23_mix[:, h, :], p23_y[:, h, :], p23_nw[:, h:h + 1], p23_rs[:], ALU.mult, ALU.mult,
                ["p23_y", "p23_nw", "p23_rs"], ["p23_mix"])
        DMA(dst.rearrange("h d s -> d h s")[:, :, t0:t0 + 512], p23_mix[:], ["p23_mix"], [f"{dst.tensor.name}_{g}"])

    def run_pipeline(gens):
        active = []
        gi = iter(gens)
        more = True
        while True:
            if more:
                try:
                    active.append(next(gi))
                except StopIteration:
                    more = False
            if not active:
                break
            for gen in list(active):
                try:
                    next(gen)
                except StopIteration:
                    active.remove(gen)

    NB2 = 3
    p2_e3 = [SB([128, 512], F32, f"p2_e3{i}") for i in range(NB2)]
    p2_sp3 = [SB([128, 512], BF16, f"p2_sp3{i}") for i in range(NB2)]
    p2_w3 = [SB([128, 512], BF16, f"p2_w3{i}") for i in range(NB2)]
    p2_S2 = [SB([128, 512], BF16, f"p2_S2{i}") for i in range(4)]

    p2set = (p23_y, p23_sq, p23_rs, p23_mix, p23_nw)

    def pass2(l, b):
        nonlocal p23_y, p23_sq, p23_rs, p23_mix, p23_nw
        p23_y, p23_sq, p23_rs, p23_mix, p23_nw = p2set
        DMA(p23_nw[:], sbn[l], (), ["p23_nw"])

        def loads(g):
            t0 = g * 512
            DMA(p2_kT[:, :, t0:t0 + 512], d_sbk[b].rearrange("(r p) s -> p r s", p=128)[:, :, t0:t0 + 512],
                [f"d_sbk{b}_{g}"], [f"p2_kT_{g}"])
            DMA(p2_v[:, 4 * g:4 * g + 4, :], d_sbv[b][t0:t0 + 512, :].rearrange("(t p) c -> p t c", p=128),
                [f"d_sbv{b}_{g}"], [f"p2_v_{g}"])
            q, nq = p2_q[g % 3], p2_nq[g % 3]
            DMA(q[:], d_sbq[b].rearrange("(r p) s -> p r s", p=128)[:, :, t0:t0 + 512], [f"d_sbq{b}_{g}"], [f"p2_q{g % 3}"])
            ACT(nq[:], q[:], AF.Copy, [f"p2_q{g % 3}"], [f"p2_nq{g % 3}"], scale=-1.0)

        def it_gen(g, h, kb, n):
            q, nq = p2_q[g % 3], p2_nq[g % 3]
            qk, nqk = f"p2_q{g % 3}", f"p2_nq{g % 3}"
            pr, e_ = h // 2, h % 2
            rows = slice(e_ * 64, e_ * 64 + 64)
            yb = 4 + (h % 2)
            last = 4 * g + 3
            gk = kb // 4
            qoff = max(0, kb - 4 * g) * 128
            W = 512 - qoff
            diag = kb >= 4 * g
            i3 = n % NB2
            b1, b2 = n % 2, 2 + n % 2
            ksl = slice(kb * 128, (kb + 1) * 128)
            e3, sp3, w3 = p2_e3[i3], p2_sp3[i3], p2_w3[i3]
            ek, spk, wk = f"p2_e3{i3}", f"p2_sp3{i3}", f"p2_w3{i3}"
            par = (last - kb) % 2
            S_in, S_out = p2_S2[2 * e_ + par], p2_S2[2 * e_ + 1 - par]
            Sik, Sok = f"p2_S2{2 * e_ + par}", f"p2_S2{2 * e_ + 1 - par}"
            if kb == last:
                if h == 0 and g + 1 < NG:
                    loads(g + 1)
                MM(psb[yb][0:64, :], zeros_b[:, 0:64], ones_b[:, 0:512], True, False, ["C_zeros_b", "C_ones_b"], [f"ps{yb}"])
            MM(psb[b1][:, 0:W], p2_kT[rows, pr, ksl], q[rows, pr, qoff:512], True, True, [f"p2_kT_{gk}", qk], [f"ps{b1}"])
            yield
            ACT(e3[:, 0:W], psb[b1][:, 0:W], AF.Exp, [f"ps{b1}"], [ek])
            ACT(sp3[:, 0:W], e3[:, 0:W], AF.Ln, [ek], [spk], bias=1.0)
            if diag:
                TT("pool", sp3[:, 0:128], sp3[:, 0:128], tri_strict[:], ALU.mult, [spk, "C_tri_strict"], [spk])
            yield
            MM(psb[b2][:, 0:W], ut_b[:], sp3[:, 0:W], True, False, ["C_ut_b", spk], [f"ps{b2}"])
            if kb < last:
                MM(psb[b2][:, 0:W], ones_b[:, 0:128], S_in[:, qoff:512], False, False, ["C_ones_b", Sik], [f"ps{b2}"])
            MM(psb[b2][:, 0:W], p2_kT[rows, pr, ksl], nq[rows, pr, qoff:512], False, True, [f"p2_kT_{gk}", nqk], [f"ps{b2}"])
            if kb > 0:
                if kb == last:
                    if qoff > 0:
                        MS("pool", S_out[:, 0:qoff], 0.0, [Sok])
                    CP("dve", S_out[:, qoff:512], sp3[:, 0:W], [spk], [Sok])
                else:
                    if qoff > 0:
                        CP("pool", S_out[:, 0:qoff], S_in[:, 0:qoff], [Sik], [Sok])
                    TT("dve", S_out[:, qoff:512], S_in[:, qoff:512], sp3[:, 0:W], ALU.add, [Sik, spk], [Sok])
            yield
            ACT(w3[:, 0:W], psb[b2][:, 0:W], AF.Exp, [f"ps{b2}"], [wk], scale=-1.0)
            if diag:
                TT("pool", w3[:, 0:128], w3[:, 0:128], tri_strict[:], ALU.mult, [wk, "C_tri_strict"], [wk])
            yield
            MM(psb[yb][0:64, qoff:512], p2_v[:, kb, h * 64:(h + 1) * 64], w3[:, 0:W], False, kb == 0,
               [f"p2_v_{gk}", wk], [f"ps{yb}"])
            if kb == 0:
                CP("dve", p23_y[:, h, :], psb[yb][0:64, :], [f"ps{yb}"], ["p23_y"])
                if h == 3:
                    head_norm_store(l, b, g, d_msb[b])

        def all_iters():
            n = 0
            for g in range(NG):
                for h in range(4):
                    for kb in range(4 * g + 3, -1, -1):
                        yield it_gen(g, h, kb, n)
                        n += 1

        loads(0)
        run_pipeline(all_iters())

    arena_state["off"] = base_off
    p3_kT = [SB([96, S], BF16, f"p3_kT{h}") for h in range(4)]
    p3_v = SB([128, NT, 256], BF16, "p3_v")
    p3_q = [[SB([96, 512], BF16, f"p3_q{i}_{h}") for h in range(4)] for i in range(3)]
    p3_p = [SB([128, 512], BF16, f"p3_p{i}") for i in range(3)]
    arena_state["off"] = max(arena_state["off"], 0)
    p3_y = SB([64, 4, 512], F32, "p3_y")
    p3_sq = SB([64, 4, 512], BF16, "p3_sq")
    p3_rs = SB([64, 512], F32, "p3_rs")
    p3_mix = SB([64, 4, 512], BF16, "p3_mix")
    p3_nw = SB([64, 4], F32, "p3_nw")

    def pass3(l, b):
        nonlocal p23_y, p23_sq, p23_rs, p23_mix, p23_nw
        p23_y, p23_sq, p23_rs, p23_mix, p23_nw = p3_y, p3_sq, p3_rs, p3_mix, p3_nw
        DMA(p23_nw[:], mon[l], (), ["p23_nw"])
        for h in range(4):
            MS("pool", p3_kT[h][:], 0.0, [f"p3_kT{h}_c"])
            DMA(p3_kT[h][64:64 + NBLK + 1, :], cd["kaug"], [], [f"p3_kT{h}_c"])
            for i in range(3):
                MS("pool", p3_q[i][h][:], 0.0, [f"p3_q{i}_{h}"])
                DMA(p3_q[i][h][64 + NBLK:64 + NBLK + 1, :], cd["alibi_row"][h:h + 1, :], [], [f"p3_q{i}_{h}"])

        def loads(g):
            t0 = g * 512
            for h in range(4):
                DMA(p3_kT[h][0:64, t0:t0 + 512], d_mok[b][h * 64:(h + 1) * 64, t0:t0 + 512], [f"d_mok{b}_{g}", f"p3_kT{h}_c"], [f"p3_kT{h}_{g}"])
            DMA(p3_v[:, 4 * g:4 * g + 4, :], d_mov[b][t0:t0 + 512, :].rearrange("(t p) c -> p t c", p=128),
                [f"d_mov{b}_{g}"], [f"p3_v_{g}"])
            i_ = g % 3
            for h in range(4):
                qk = f"p3_q{i_}_{h}"
                DMA(p3_q[i_][h][0:64, :], d_moq[b][h * 64:(h + 1) * 64, t0:t0 + 512], [f"d_moq{b}_{g}"], [qk])
                DMA(p3_q[i_][h][64:64 + NBLK, :], d_sel[b][h * NBLK:(h + 1) * NBLK, t0:t0 + 512], [f"d_sel{b}_{g}"], [qk])

        def it_gen(g, h, kb, n):
            i_ = g % 3
            qk = f"p3_q{i_}_{h}"
            qa = p3_q[i_][h]
            yb = 4 + (h % 2)
            db = 6 + (h % 2)
            gk = kb // 4
            qoff = max(0, kb - 4 * g) * 128
            W = 512 - qoff
            diag = kb >= 4 * g
            rel = 4 * g + 3 - kb
            i3 = n % 3
            b1 = n % 3
            lastk = kb == 4 * g + 3
            pp, pk = p3_p[i3], f"p3_p{i3}"
            if kb == 0:
                if h == 0 and g + 1 < NG:
                    loads(g + 1)
                MM(psb[yb][0:64, :], zeros_b[:, 0:64], ones_b[:, 0:512], True, False, ["C_zeros_b", "C_ones_b"], [f"ps{yb}"])
                MM(psb[db][0:64, :], zeros_b[:, 0:64], ones_b[:, 0:512], True, False, ["C_zeros_b", "C_ones_b"], [f"ps{db}"])
            MM(psb[b1][:, 0:W], p3_kT[h][:, kb * 128:(kb + 1) * 128], qa[:, qoff:512], True, True,
               [f"p3_kT{h}_{gk}", f"p3_kT{h}_c", qk], [f"ps{b1}"])
            yield
            ACT(pp[:, 0:W], psb[b1][:, 0:W], AF.Exp, [f"ps{b1}", "C_alibi_kb"], [pk], bias=alibi_kb[:, h, rel:rel + 1])
            if diag:
                TT("dve", pp[:, 0:128], pp[:, 0:128], tri_incl[:], ALU.mult, [pk, "C_tri_incl"], [pk])
            yield
            MM(psb[yb][0:64, qoff:512], p3_v[:, kb, h * 64:(h + 1) * 64], pp[:, 0:W], False, lastk, [f"p3_v_{gk}", pk], [f"ps{yb}"])
            MM(psb[db][0:64, qoff:512], ones_b[:, 0:64], pp[:, 0:W], False, lastk, ["C_ones_b", pk], [f"ps{db}"])
            if lastk:
                CP("act", p23_y[:, h, :], psb[yb][0:64, :], [f"ps{yb}"], ["p23_y"])
                P.add("dve", lambda e: e.reciprocal(out=p23_rs[:], in_=psb[db][0:64, :]), [f"ps{db}"], ["p23_rs"])
                TT("dve", p23_y[:, h, :], p23_y[:, h, :], p23_rs[:], ALU.mult, ["p23_y", "p23_rs"], ["p23_y"])
                if h == 3:
                    head_norm_store(l, b, g, d_mmo[b], nb=3)

        def all_iters():
            n = 0
            for g in range(NG):
                for h in range(4):
                    for kb in range(0, 4 * g + 4):
                        yield it_gen(g, h, kb, n)
                        n += 1

        loads(0)
        run_pipeline(all_iters())

    arena_state["off"] = base_off
    p45_tmp = [SB([128, D], F32, f"p45_tmp{i}") for i in range(2)]
    p45_xo = [SB([128, D], F32, f"p45_xo{i}") for i in range(2)]
    p45_ss = SB([128, 4], F32, "p45_ss")
    base45 = arena_state["off"]
    p4_woa = SB([128, 4, D], BF16, "p4_woa")
    p4_wob = SB([64, 8, D], BF16, "p4_wob")
    p4_ms = [SB([128, 4, 512], BF16, f"p4_ms{i}") for i in range(2)]
    p4_mh = [SB([64, 8, 512], BF16, f"p4_mh{i}") for i in range(2)]

    def norm_resid_store(pb, xtile, xk, i2, dst_ap, dkey):
        for nh in range(2):
            jb, jk = nextjunk()
            ACT(jb[:, 0:512], psb[pb + nh][:], AF.Square, [f"ps{pb + nh}"], [jk, f"p45_ss{nh}"], scale=1.0 / 32,
                accum_out=p45_ss[:, nh:nh + 1])
        TT("dve", p45_ss[:, 2:3], p45_ss[:, 0:1], p45_ss[:, 1:2], ALU.add, ["p45_ss0", "p45_ss1"], ["p45_ss2"])
        ACT(p45_ss[:, 3:4], p45_ss[:, 2:3], AF.Ln, ["p45_ss2"], ["p45_ss3"], bias=EPS)
        ACT(p45_ss[:, 3:4], p45_ss[:, 3:4], AF.Exp, ["p45_ss3"], ["p45_ss3"], scale=-0.5)
        tmp, tk = p45_tmp[i2], f"p45_tmp{i2}"
        for nh in range(2):
            STT("dve", tmp[:, nh * 512:(nh + 1) * 512], psb[pb + nh][:], p45_ss[:, 3:4], gain[1][:, nh * 512:(nh + 1) * 512],
                ALU.mult, ALU.mult, [f"ps{pb + nh}", "p45_ss3", "gain1"], [tk])
        xo, xok = p45_xo[i2], f"p45_xo{i2}"
        TT("pool", xo[:], tmp[:], xtile, ALU.add, [tk, xk], [xok])
        DMA(dst_ap, xo[:], [xok], [dkey])

    def pass4(l, b, xsrc, srckey):
        load_gain(1, l, 1)
        DMA(p4_woa[:], w_out[l, 0:512, :].rearrange("(c p) n -> p c n", p=128), (), ["p4_woa"], eng="pool")
        DMA(p4_wob[:], w_out[l, 512:1024, :].rearrange("(j p) n -> p j n", p=64), (), ["p4_wob"], eng="pool")
        n = 0
        for g in range(NG):
            t0 = g * 512
            ms, mh = p4_ms[g % 2], p4_mh[g % 2]
            msk, mhk = f"p4_ms{g % 2}", f"p4_mh{g % 2}"
            DMA(ms[:], d_ssd[b].rearrange("(c p) s -> p c s", p=128)[:, :, t0:t0 + 512], [f"d_ssd{b}_{g}"], [msk])
            DMA(mh[:, 0:4, :], d_msb[b].rearrange("h d s -> d h s")[:, :, t0:t0 + 512], [f"d_msb{b}_{g}"], [mhk])
            DMA(mh[:, 4:8, :], d_mmo[b].rearrange("h d s -> d h s")[:, :, t0:t0 + 512], [f"d_mmo{b}_{g}"], [mhk])
            for tt in range(4):
                tok = slice(t0 + tt * 128, t0 + (tt + 1) * 128)
                xi = n % 4
                DMA(xt[xi][:], xsrc[tok, :], [f"{srckey}_{(t0 + tt * 128) // 1024}"], [f"xt{xi}"])
                pb = 4 + 2 * (n % 2)
                tsl = slice(tt * 128, (tt + 1) * 128)
                for nh in range(2):
                    cs = slice(nh * 512, (nh + 1) * 512)
                    for c in range(4):
                        MM(psb[pb + nh][:], ms[:, c, tsl], p4_woa[:, c, cs], c == 0, False, [msk, "p4_woa"], [f"ps{pb + nh}"])
                    for j in range(8):
                        MM(psb[pb + nh][:], mh[:, j, tsl], p4_wob[:, j, cs], False, j == 7, [mhk, "p4_wob"], [f"ps{pb + nh}"])
                norm_resid_store(pb, xt[xi][:], f"xt{xi}", n % 2, d_x1[b][tok, :], f"d_x1{b}_{(t0 + tt * 128) // 1024}")
                n += 1

    arena_state["off"] = base45
    p5_hT = SB([128, 8, 1024], BF16, "p5_hT")
    p5_act = SB([128, NHC, 1024], BF16, "p5_act")
    p5_wd = SB([128, NHC, D], BF16, "p5_wd")
    p5_wg = [SB([128, 8, 256], BF16, f"p5_wg{i}") for i in range(2)]
    p5_wu = [SB([128, 8, 256], BF16, f"p5_wu{i}") for i in range(2)]
    p5_sg = [SB([128, 512], F32, f"p5_sg{i}") for i in range(2)]

    def pass5(l, b, xdst, dstkey):
        load_gain(0, l, 2)
        load_gain(1, l, 3)
        for (c0, c1) in [(0, 11), (11, 22)]:
            DMA(p5_wd[:, c0:c1, :], w_down[l, c0 * 128:c1 * 128, :].rearrange("(c p) n -> p c n", p=128), (), ["p5_wd"], eng="pool")
        n = 0
        wi = 0
        for G in range(S // 1024 if not ng_limit else max(1, NG // 2)):
            T0 = G * 1024
            for tt in range(8):
                xi = n % 4
                n += 1
                tok = slice(T0 + tt * 128, T0 + (tt + 1) * 128)
                DMA(xt[xi][:], d_x1[b][tok, :], [f"d_x1{b}_{G}"], [f"xt{xi}"])
                jb, jk = nextjunk()
                ACT(jb[:], xt[xi][:], AF.Square, [f"xt{xi}"], [jk, "p5_ssa"], scale=1.0 / 32, accum_out=ss4[:, 0:1])
                ACT(rs4[:, 0:1], ss4[:, 0:1], AF.Ln, ["p5_ssa"], ["p5_rsa"], bias=EPS)
                ACT(rs4[:, 0:1], rs4[:, 0:1], AF.Exp, ["p5_rsa"], ["p5_rsa"], scale=-0.5)
                hbk = f"hb{tt % 2}"
                STT("dve", hb[tt % 2][:], xt[xi][:], rs4[:, 0:1], gain[0][:], ALU.mult, ALU.mult, [f"xt{xi}", "p5_rsa", "gain0"], [hbk])
                bk = 4 + (tt % 4)
                pT = psbf(bk)
                for c in range(8):
                    TR(pT[:, c * 128:(c + 1) * 128], hb[tt % 2][:, c * 128:(c + 1) * 128], ident_b[:], [hbk, "C_ident_b"], [f"ps{bk}"])
                CP("act" if tt % 2 else "dve", p5_hT[:, :, tt * 128:(tt + 1) * 128], pT.rearrange("p (c k) -> p c k", k=128),
                   [f"ps{bk}"], ["p5_hT"])
            it = 0
            for hq in range(0, NHC, 2):
                nq_ = min(2, NHC - hq)
                wg, wu = p5_wg[wi % 2], p5_wu[wi % 2]
                wgk, wuk = f"p5_wg{wi % 2}", f"p5_wu{wi % 2}"
                wi += 1
                cols = slice(hq * 128, (hq + nq_) * 128)
                DMA(wg[:, :, 0:nq_ * 128], w_gate[l, :, cols].rearrange("(c p) n -> p c n", p=128), (), [wgk], eng="pool")
                DMA(wu[:, :, 0:nq_ * 128], w_up[l, :, cols].rearrange("(c p) n -> p c n", p=128), (), [wuk], eng="pool")
                for hl in range(nq_):
                    hcx = hq + hl
                    for half in range(2):
                        i2 = it % 2
                        it += 1
                        ba, bb = 2 * i2, 2 * i2 + 1
                        hs = slice(half * 512, (half + 1) * 512)
                        for c in range(8):
                            MM(psb[ba][:], wg[:, c, hl * 128:(hl + 1) * 128], p5_hT[:, c, hs], c == 0, c == 7, [wgk, "p5_hT"], [f"ps{ba}"])
                        for c in range(8):
                            MM(psb[bb][:], wu[:, c, hl * 128:(hl + 1) * 128], p5_hT[:, c, hs], c == 0, c == 7, [wuk, "p5_hT"], [f"ps{bb}"])
                        ACT(p5_sg[i2][:], psb[ba][:], AF.Silu, [f"ps{ba}"], [f"p5_sg{i2}"])
                        TT("dve", p5_act[:, hcx, hs], p5_sg[i2][:], psb[bb][:], ALU.mult, [f"p5_sg{i2}", f"ps{bb}"], ["p5_act"])
            for tt in range(8):
                tok = slice(T0 + tt * 128, T0 + (tt + 1) * 128)
                xi = n % 4
                n += 1
                DMA(xt[xi][:], d_x1[b][tok, :], [f"d_x1{b}_{G}"], [f"xt{xi}"])
                pb = 4 + 2 * (tt % 2)
                tsl = slice(tt * 128, (tt + 1) * 128)
                for nh in range(2):
                    for hcx in range(NHC):
                        MM(psb[pb + nh][:], p5_act[:, hcx, tsl], p5_wd[:, hcx, nh * 512:(nh + 1) * 512], hcx == 0, hcx == NHC - 1,
                           ["p5_act", "p5_wd"], [f"ps{pb + nh}"])
                norm_resid_store(pb, xt[xi][:], f"xt{xi}", tt % 2, xdst[tok, :], f"{dstkey}_{G}")

    for l in range(DEPTH):
        for b in range(NSEQ):
            xsrc = x_in[b] if l == 0 else d_x2[b]
            srckey = f"xin{b}" if l == 0 else f"d_x2{b}"
            if 1 in passes:
                barrier()
                pass1(l, b, xsrc)
            if 2 in passes:
                barrier()
                pass2(l, b)
            if 3 in passes:
                barrier()
                pass3(l, b)
            if 4 in passes:
                barrier()
                pass4(l, b, xsrc, srckey)
            if 5 in passes:
                barrier()
                if l == DEPTH - 1:
                    pass5(l, b, out[b], f"out{b}")
                else:
                    pass5(l, b, d_x2[b], f"d_x2{b}")
    finals = list(P.dma_prev.values())
    P.emit(final_waits=finals)
    return nc, hc


def prep_inputs(inputs, DEPTH):
    f = lambda a: np.ascontiguousarray(np.asarray(a, dtype=np.float32))
    d = {}
    for k in ["w_in", "w_out", "w_gate", "w_up", "w_down"]:
        d[k] = f(inputs[k])
    d["vec1024"] = f(np.stack([inputs["pre_mix_norm"], inputs["post_mix_norm"], inputs["pre_ffn_norm"], inputs["post_ffn_norm"]], axis=1))
    cwv = np.asarray(inputs["conv_w"], np.float32)
    d["convw"] = f(cwv.reshape(DEPTH, 4, 8, 128).transpose(0, 3, 2, 1))
    d["convb"] = f(np.asarray(inputs["conv_b"], np.float32).reshape(DEPTH, 8, 128).transpose(0, 2, 1))
    d["hv"] = f(np.stack([inputs["dt_bias"], inputs["a_log"], inputs["d_skip"]], axis=1))
    d["ssdn"] = f(inputs["ssd_norm"])
    d["sbn"] = f(np.asarray(inputs["sb_norm"], np.float32).reshape(DEPTH, 4, 64).transpose(0, 2, 1))
    d["mon"] = f(np.asarray(inputs["moba_norm"], np.float32).reshape(DEPTH, 4, 64).transpose(0, 2, 1))
    return d


def kernel(**inputs):
    x = np.asarray(inputs["x"], np.float32)
    B, S, _ = x.shape
    DEPTH = inputs["w_in"].shape[0]
    NCORE = 8
    NSEQ = B // NCORE
    nc, hc = build(S, NSEQ, DEPTH)
    shared = prep_inputs(inputs, DEPTH)
    for k, v in hc.items():
        shared["c_" + k] = v
    in_maps = []
    for c in range(NCORE):
        m = dict(shared)
        m["x"] = np.ascontiguousarray(x[c * NSEQ:(c + 1) * NSEQ])
        in_maps.append(m)
    res = run_bass_kernel_spmd(nc, in_maps, core_ids=list(range(NCORE)))
    return np.concatenate([r["out"] for r in res.results], axis=0).astype(np.float32)
```

```python
import contextlib
import numpy as np
import ml_dtypes
import concourse.bass as bass
import concourse.mybir as mybir
from concourse.bass_utils import run_bass_kernel_spmd

F32 = mybir.dt.float32
BF16 = mybir.dt.bfloat16
AF = mybir.ActivationFunctionType
ALU = mybir.AluOpType
AX = mybir.AxisListType

D = 1024
INC = 3080
FH = 2816
NHC = 22
EPS = 1e-6
BIG = 30000.0
ENGS = ["pe", "act", "dve", "pool", "sp"]
EPOCH = 12000
NDMA = 12


class Prog:
    def __init__(self, nc):
        self.nc = nc
        self.ins = {e: [] for e in ENGS}
        self.lastw = {}
        self.readers = {}
        self.dma_slot = {e: 0 for e in ENGS}
        self.dma_prev = {}

    def add(self, eng, fn, reads=(), writes=(), dma=False):
        lst = self.ins[eng]
        me = dict(eng=eng, idx=len(lst), fn=fn, deps=set(), dma=dma, sig=False)
        deps = me["deps"]
        for r in reads:
            w = self.lastw.get(r)
            if w is not None:
                deps.add((w["eng"], w["idx"]))
        for wr in writes:
            w = self.lastw.get(wr)
            if w is not None and (w["eng"] != eng or w["dma"] or dma or eng != "pe"):
                deps.add((w["eng"], w["idx"]))
            for rd in self.readers.get(wr, ()):
                if rd["eng"] != eng or rd["dma"] or dma or eng != "pe":
                    deps.add((rd["eng"], rd["idx"]))
        if dma:
            slot = self.dma_slot[eng]
            self.dma_slot[eng] = (slot + 1) % NDMA
            me["slot"] = slot
            prev = self.dma_prev.get((eng, slot))
            if prev is not None:
                deps.add((prev["eng"], prev["idx"]))
            self.dma_prev[(eng, slot)] = me
        deps.discard((eng, me["idx"]))
        for r in reads:
            self.readers.setdefault(r, []).append(me)
        for wr in writes:
            self.lastw[wr] = me
            self.readers[wr] = []
        lst.append(me)
        return me

    def emit(self, final_waits=()):
        nc = self.nc
        for e in ENGS:
            for me in self.ins[e]:
                for (pe, pi) in me["deps"]:
                    self.ins[pe][pi]["sig"] = True
        for me in final_waits:
            me["sig"] = True
        nep = {}
        for e in ENGS:
            c = 0
            for me in self.ins[e]:
                if me["dma"]:
                    continue
                if me["sig"]:
                    c += 1
                    me["cnt"] = c
            nep[e] = c // EPOCH + 1
        dcount = {}
        for e in ENGS:
            for me in self.ins[e]:
                if me["dma"]:
                    k = (e, me["slot"])
                    dcount[k] = dcount.get(k, 0) + 16
                    me["cnt"] = dcount[k]
        with contextlib.ExitStack() as st:
            esem = {e: [st.enter_context(nc.semaphore(f"s_{e}_{i}")) for i in range(nep[e])] for e in ENGS}
            dsem = {}
            for k in dcount:
                dsem[k] = st.enter_context(nc.semaphore(f"d_{k[0]}_{k[1]}"))
            block = st.enter_context(nc.Block())

            def target(p):
                if p["dma"]:
                    return dsem[(p["eng"], p["slot"])], p["cnt"]
                c = p["cnt"]
                ep = (c - 1) // EPOCH
                return esem[p["eng"]][ep], c - ep * EPOCH

            def run(e, engobj):
                waited = {}
                for me in self.ins[e]:
                    best = {}
                    for (pe, pi) in me["deps"]:
                        p = self.ins[pe][pi]
                        key = ("d", pe, p["slot"]) if p["dma"] else ("e", pe)
                        if key not in best or best[key]["idx"] < p["idx"]:
                            best[key] = p
                    for key, p in best.items():
                        if waited.get(key, 0) >= p["cnt"]:
                            continue
                        waited[key] = p["cnt"]
                        sem, val = target(p)
                        engobj.wait_ge(sem, val)
                    r = me["fn"](engobj)
                    if me["dma"]:
                        r.then_inc(dsem[(e, me["slot"])], 16)
                    elif me["sig"]:
                        sem, _ = target(me)
                        r.then_inc(sem, 1)
                if e == "sp":
                    for p in final_waits:
                        sem, val = target(p)
                        engobj.wait_ge(sem, val)

            block.tensor(lambda eng: run("pe", eng))
            block.scalar(lambda eng: run("act", eng))
            block.vector(lambda eng: run("dve", eng))
            block.gpsimd(lambda eng: run("pool", eng))
            block.sync(lambda eng: run("sp", eng))


def host_consts(S):
    NBLK = S // 256
    p = np.arange(128)
    c = {}
    c["ident_f"] = np.eye(128, dtype=np.float32)
    c["tri_incl"] = (p[:, None] <= p[None, :]).astype(np.float32)
    c["tri_strict"] = (p[:, None] < p[None, :]).astype(np.float32)
    c["ones_f"] = np.ones((128, 128), np.float32)
    bf = ml_dtypes.bfloat16
    c["ut_b"] = (p[:, None] >= p[None, :]).astype(bf)
    c["ones_b"] = np.ones((128, 512), bf)
    c["zeros_b"] = np.zeros((128, 128), bf)
    nm = np.where(p[None, :] < p[:, None], -BIG, 0.0).astype(np.float32)
    c["negmask_b"] = np.tile(nm[:, None, :], (1, 8, 1)).reshape(128, 1024).astype(bf)
    own = np.arange(NBLK + 1)
    n = np.arange(NBLK)
    vm = np.where(n[None, :] < own[:, None], 0.0, -1e30).astype(np.float32)
    om = (n[None, :] == own[:, None]).astype(np.float32)
    c["vmask"] = np.tile(vm.reshape(1, -1), (128, 1))
    c["ownmask"] = np.tile(om.reshape(1, -1), (128, 1))
    s = np.arange(S)
    kaug = np.zeros((NBLK + 1, S), np.float32)
    kaug[s // 256, s] = 1.0
    kaug[NBLK, :] = 1.0
    c["kaug"] = kaug.astype(bf)
    slopes = 2.0 ** (-8.0 * np.arange(1, 5) / 4)
    c["alibi_row"] = (-slopes[:, None] * np.arange(512)[None, :]).astype(bf)
    rel = np.arange(32)
    ab = slopes[None, :, None] * (p[:, None, None] + 384 - 128 * rel[None, None, :])
    c["alibi_kb"] = ab.astype(np.float32)
    return c


import os as _os
KSTOP = int(_os.environ.get('KSTOP', '0'))


def build(S, NSEQ, DEPTH, dbg=False, passes=(1, 2, 3, 4, 5), ng_limit=None):
    nc = bass.Bass("TRN2", target_bir_lowering=False)
    NG, NT, NBLK = S // 512, S // 128, S // 256
    if ng_limit:
        NG = ng_limit
    KA = 64 + NBLK + 1
    hc = host_consts(S)

    def din(name, shape, dt=F32):
        return nc.dram_tensor(name, list(shape), dt, kind="ExternalInput").ap()

    def dscr(name, shape, dt):
        return nc.dram_tensor(name, list(shape), dt, kind=("ExternalOutput" if dbg else "Internal")).ap()

    x_in = din("x", [NSEQ, S, D])
    w_in = din("w_in", [DEPTH, D, INC])
    w_out = din("w_out", [DEPTH, D, D])
    w_gate = din("w_gate", [DEPTH, D, FH])
    w_up = din("w_up", [DEPTH, D, FH])
    w_down = din("w_down", [DEPTH, FH, D])
    vec1024 = din("vec1024", [DEPTH, 4, D])
    convw = din("convw", [DEPTH, 128, 8, 4])
    convb = din("convb", [DEPTH, 128, 8])
    hv = din("hv", [DEPTH, 3, 8])
    ssdn = din("ssdn", [DEPTH, 512])
    sbn = din("sbn", [DEPTH, 64, 4])
    mon = din("mon", [DEPTH, 64, 4])
    cd = {}
    for k, v in hc.items():
        cd[k] = din("c_" + k, v.shape, BF16 if v.dtype == ml_dtypes.bfloat16 else F32)
    out = nc.dram_tensor("out", [NSEQ, S, D], F32, kind="ExternalOutput").ap()

    d_ssd = [dscr(f"d_ssd{b}", [512, S], BF16) for b in range(NSEQ)]
    d_sbq = [dscr(f"d_sbq{b}", [256, S], BF16) for b in range(NSEQ)]
    d_sbk = [dscr(f"d_sbk{b}", [256, S], BF16) for b in range(NSEQ)]
    d_sbv = [dscr(f"d_sbv{b}", [S, 256], BF16) for b in range(NSEQ)]
    d_moq = [dscr(f"d_moq{b}", [256, S], BF16) for b in range(NSEQ)]
    d_mok = [dscr(f"d_mok{b}", [256, S], BF16) for b in range(NSEQ)]
    d_mov = [dscr(f"d_mov{b}", [S, 256], BF16) for b in range(NSEQ)]
    d_sel = [dscr(f"d_sel{b}", [4 * NBLK, S], BF16) for b in range(NSEQ)]
    d_msb = [dscr(f"d_msb{b}", [4, 64, S], BF16) for b in range(NSEQ)]
    d_mmo = [dscr(f"d_mmo{b}", [4, 64, S], BF16) for b in range(NSEQ)]
    d_x1 = [dscr(f"d_x1{b}", [S, D], F32) for b in range(NSEQ)]
    d_x2 = [dscr(f"d_x2{b}", [S, D], F32) for b in range(NSEQ)]

    dbg_t = dscr("dbg_t", [128, 256], F32)
    P = Prog(nc)
    sb_ = nc.alloc_sbuf_tensor
    _n = [0]
    ARENA_BYTES = 192 * 1024
    arena_state = {"on": False, "off": 0, "t": None}

    def SB(shape, dt, name=None):
        _n[0] += 1
        if not arena_state["on"]:
            return sb_(name or f"t{_n[0]}", list(shape), dt)
        free = int(np.prod(shape[1:]))
        nb = free * (2 if dt == BF16 else 4)
        nb = (nb + 63) // 64 * 64
        off = arena_state["off"]
        assert off + nb <= ARENA_BYTES, (name, off, nb)
        ap = arena_state["t"][0:shape[0], off // 4:(off + nb) // 4]
        if dt == BF16:
            ap = ap.bitcast(BF16)
        ap = ap[:, 0:free]
        if len(shape) == 3:
            ap = ap.rearrange("p (a b) -> p a b", b=shape[2])
        elif len(shape) == 4:
            ap = ap.rearrange("p (a b c) -> p a b c", b=shape[2], c=shape[3])
        arena_state["off"] = off + nb
        return ap

    def DMA(out_, in_, reads, writes, eng="sp"):
        return P.add(eng, lambda e: e.dma_start(out=out_, in_=in_), reads, writes, dma=True)

    def MM(out_, lhsT, rhs, start, stop, reads, writes):
        return P.add("pe", lambda e: e.matmul(out_, lhsT=lhsT, rhs=rhs, start=start, stop=stop), reads, writes)

    def TR(out_, in_, ident, reads, writes):
        return P.add("pe", lambda e: e.transpose(out=out_, in_=in_, identity=ident), reads, writes)

    def ACT(out_, in_, func, reads, writes, **kw):
        return P.add("act", lambda e: e.activation(out=out_, in_=in_, func=func, **kw), reads, writes)

    def TT(eng, out_, in0, in1, op, reads, writes):
        return P.add(eng, lambda e: e.tensor_tensor(out=out_, in0=in0, in1=in1, op=op), reads, writes)

    def TS(eng, out_, in0, s1, s2, op0, op1, reads, writes):
        if s2 is None:
            return P.add(eng, lambda e: e.tensor_scalar(out=out_, in0=in0, scalar1=s1, scalar2=None, op0=op0), reads, writes)
        return P.add(eng, lambda e: e.tensor_scalar(out=out_, in0=in0, scalar1=s1, scalar2=s2, op0=op0, op1=op1), reads, writes)

    def STT(eng, out_, in0, scalar, in1, op0, op1, reads, writes):
        return P.add(eng, lambda e: e.scalar_tensor_tensor(out=out_, in0=in0, scalar=scalar, in1=in1, op0=op0, op1=op1), reads, writes)

    def CP(eng, out_, in_, reads, writes):
        if eng == "act":
            return ACT(out_, in_, AF.Copy, reads, writes)
        return P.add(eng, lambda e: e.tensor_copy(out=out_, in_=in_), reads, writes)

    def MS(eng, ap, val, writes):
        return P.add(eng, lambda e: e.memset(ap, val), (), writes)

    psb = [nc.alloc_psum_tensor(f"ps{i}", [128, 512], F32) for i in range(8)]

    def psbf(i):
        return psb[i][:].bitcast(BF16)

    ident_f = SB([128, 128], F32, "ident_f")
    ident_b = SB([128, 128], BF16, "ident_b")
    tri_incl = SB([128, 128], F32, "tri_incl")
    tri_strict = SB([128, 128], F32, "tri_strict")
    ones_f = SB([128, 128], F32, "ones_f")
    ut_b = SB([128, 128], BF16, "ut_b")
    ones_b = SB([128, 512], BF16, "ones_b")
    zeros_b = SB([128, 128], BF16, "zeros_b")
    negmask_b = SB([128, 1024], BF16, "negmask_b")
    vmask = SB([128, (NBLK + 1) * NBLK], F32, "vmask")
    ownmask = SB([128, (NBLK + 1) * NBLK], F32, "ownmask")
    alibi_kb = SB([128, 4, 32], F32, "alibi_kb")
    for nm_, t in [("ident_f", ident_f), ("tri_incl", tri_incl), ("tri_strict", tri_strict), ("ones_f", ones_f),
                   ("ut_b", ut_b), ("ones_b", ones_b), ("zeros_b", zeros_b), ("negmask_b", negmask_b),
                   ("vmask", vmask), ("ownmask", ownmask), ("alibi_kb", alibi_kb)]:
        DMA(t[:], cd[nm_], (), ["C_" + nm_])
    CP("dve", ident_b[:], ident_f[:], ["C_ident_f"], ["C_ident_b"])
    CONST = ["C_ident_f", "C_ident_b", "C_tri_incl", "C_tri_strict", "C_ones_f", "C_ut_b", "C_ones_b", "C_zeros_b",
             "C_negmask_b", "C_vmask", "C_ownmask", "C_alibi_kb"]

    bar_sb = {e: SB([128, 8], F32, f"bar_{e}") for e in ("act", "dve", "pool")}
    bar_dram = nc.dram_tensor("bar_dram", [2, 64], F32).ap()

    def barrier():
        keys = []
        P.add("pe", lambda e: e.matmul(psb[7][:, 0:8], lhsT=zeros_b[:, 0:128], rhs=zeros_b[:, 0:8], start=True, stop=True),
              ["C_zeros_b"], ["ps7", "bar_pe"])
        for en in ("act", "dve", "pool"):
            CP(en, bar_sb[en][:], ones_f[:, 0:8], ["C_ones_f"], [f"bar_{en}"])
        allk = ["bar_pe", "bar_act", "bar_dve", "bar_pool"]
        dmas = {(d["eng"], d["idx"]) for d in P.dma_prev.values()}
        m = P.add("pe", lambda e: e.matmul(psb[7][:, 0:8], lhsT=zeros_b[:, 0:128], rhs=zeros_b[:, 0:8], start=True, stop=True),
                  ["C_zeros_b"] + allk, ["ps7", "bar2_pe"])
        m["deps"] |= dmas
        for en in ("act", "dve", "pool"):
            m = CP(en, bar_sb[en][:], ones_f[:, 0:8], ["C_ones_f"] + allk, [f"bar2_{en}"])
            m["deps"] |= dmas
        m = DMA(bar_dram[1:2, :], cd["ones_f"][0:1, 0:64], allk, ["bar2_sp"])
        m["deps"] |= {d for d in dmas if d != (m["eng"], m["idx"])}

    arena_state["t"] = sb_("arena", [128, ARENA_BYTES // 4], F32)
    arena_state["on"] = True
    gain = [SB([128, D], F32, f"gain{i}") for i in range(2)]
    xt = [SB([128, D], F32, f"xt{i}") for i in range(4)]
    junks = [SB([128, D], BF16, f"junk{i}") for i in range(4)]
    _jr = [0]

    def nextjunk():
        _jr[0] = (_jr[0] + 1) % 4
        return junks[_jr[0]], f"junk{_jr[0]}"
    ss4 = SB([128, 8], F32, "ss4")
    rs4 = SB([128, 8], F32, "rs4")
    hb = [SB([128, D], BF16, f"hb{i}") for i in range(2)]

    def load_gain(slot, l, which):
        DMA(gain[slot][:], vec1024[l, which].partition_broadcast(128), (), [f"gain{slot}"])

    base_off = arena_state["off"]

    win = SB([128, 8, INC], BF16, "win")
    hT = SB([128, 8, 512], BF16, "hT")
    zs = SB([128, 4, 512], F32, "zs")
    ubuf = [SB([128, 515], F32, f"u{i}") for i in range(2)]
    halo = SB([128, 8, 3], F32, "halo")
    cta = [SB([128, 512], F32, f"cta{i}") for i in range(2)]
    ctb = [SB([128, 512], F32, f"ctb{i}") for i in range(2)]
    xsT = SB([128, 4, 512], F32, "xsT")
    xs_tok = [SB([128, 512], F32, f"xstok{i}") for i in range(4)]
    BT = SB([128, 2, 512], BF16, "BT")
    CT = SB([128, 2, 512], BF16, "CT")
    Btok = SB([128, 4, 2, 128], BF16, "Btok")
    cw = SB([128, 8, 4], F32, "cw")
    cb = SB([128, 8], F32, "cb")
    hv_bc = SB([128, 3, 8], F32, "hv_bc")
    a_bc = SB([128, 8], F32, "a_bc")
    dsk_bc = SB([128, 8, 64], F32, "dsk_bc")
    ssdn_bc = SB([128, 512], F32, "ssdn_bc")
    dtx = SB([128, 4, 8], F32, "dtx")
    dt_all = SB([128, 4, 8], F32, "dt_all")
    adt = SB([128, 4, 8], F32, "adt")
    sbq_st = SB([128, 2, 512], BF16, "sbq_st")
    sbk_st = SB([128, 2, 512], BF16, "sbk_st")
    sbv_st = SB([128, 4, 256], BF16, "sbv_st")
    moq32 = SB([128, 2, 512], F32, "moq32")
    mok32 = SB([128, 2, 512], F32, "mok32")
    moq_st = SB([128, 2, 512], BF16, "moq_st")
    mok_st = SB([128, 2, 512], BF16, "mok_st")
    mov_st = SB([128, 4, 256], BF16, "mov_st")
    kmean = SB([128, 2, 2, NBLK], F32, "kmean")
    gm = SB([128, 4, NBLK], F32, "gm")
    max8 = SB([128, 4, 8], F32, "max8")
    thr = SB([128, 4], F32, "thr")
    sel = SB([128, 4, NBLK], F32, "sel")
    selneg = SB([128, 4 * NBLK], BF16, "selneg")
    selT_st = SB([64, 512], BF16, "selT_st")
    rhs_cum = SB([128, 8, 128], F32, "rhs_cum")
    negac4 = SB([128, 4, 8], F32, "negac4")
    ea4 = SB([128, 4, 8], F32, "ea4")
    cdb4 = SB([128, 4, 8], F32, "cdb4")
    dtd4 = SB([128, 4, 8], F32, "dtd4")
    negac = SB([128, 8], F32, "negac")
    ea = SB([128, 8], F32, "ea")
    cdb = SB([128, 8], F32, "cdb")
    Eb = SB([128, 8, 128], F32, "Eb")
    MT = SB([128, 8, 128], BF16, "MT")
    dtd = SB([128, 8], F32, "dtd")
    xdt = SB([128, 8, 64], BF16, "xdt")
    xdt2 = SB([128, 8, 64], BF16, "xdt2")
    S32 = SB([128, 512], F32, "S32")
    Sb = SB([128, 512], BF16, "Sb")
    y1 = SB([128, 512], F32, "y1")
    y2 = SB([128, 512], F32, "y2")
    y3 = SB([128, 512], F32, "y3")
    ssq = SB([128, 2], F32, "ssq")
    rsq = SB([128, 2], F32, "rsq")
    yn = SB([128, 512], BF16, "yn")
    mixT_ssd = SB([128, 4, 512], BF16, "mixT_ssd")

    rot01 = [0]

    def nextbank():
        rot01[0] ^= 1
        return rot01[0]

    def load_layer_p1(l):
        for (c0, c1) in [(0, 512), (512, 1536), (1536, 2312), (2312, INC)]:
            DMA(win[:, :, c0:c1], w_in[l, :, c0:c1].rearrange("(c p) n -> p c n", p=128), (), ["win"], eng="pool")
        DMA(cw[:], convw[l], (), ["cw"])
        DMA(cb[:], convb[l], (), ["cb"])
        for i in range(3):
            DMA(hv_bc[:, i, :], hv[l, i].partition_broadcast(128), (), ["hv_bc"])
        DMA(ssdn_bc[:], ssdn[l].partition_broadcast(128), (), ["ssdn_bc"])
        ACT(a_bc[:], hv_bc[:, 1, :], AF.Exp, ["hv_bc"], ["a_bc"])
        TS("dve", a_bc[:], a_bc[:], -1.0, None, ALU.mult, None, ["a_bc"], ["a_bc"])
        CP("dve", dsk_bc[:], hv_bc[:, 2, :].unsqueeze(2).to_broadcast([128, 8, 64]), ["hv_bc"], ["dsk_bc"])

    def pass1(l, b, xsrc):
        load_layer_p1(l)
        load_gain(0, l, 0)
        MS("pool", S32[:], 0.0, ["S32"])
        MS("pool", Sb[:], 0.0, ["Sb"])
        MS("pool", halo[:], 0.0, ["halo"])
        MS("pool", kmean[:], 0.0, ["kmean"])
        for g in range(NG):
            t0 = g * 512
            for tt in range(4):
                DMA(xt[tt][:], xsrc[t0 + tt * 128:t0 + (tt + 1) * 128, :], (), [f"xt{tt}"])
            for tt in range(4):
                jb, jk = nextjunk()
                ACT(jb[:], xt[tt][:], AF.Square, [f"xt{tt}"], [jk, f"ss4_{tt}"], scale=1.0 / 32, accum_out=ss4[:, tt:tt + 1])
            ACT(rs4[:, 0:4], ss4[:, 0:4], AF.Ln, [f"ss4_{i}" for i in range(4)], ["rs4"], bias=EPS)
            ACT(rs4[:, 0:4], rs4[:, 0:4], AF.Exp, ["rs4"], ["rs4"], scale=-0.5)
            for tt in range(4):
                hbk = f"hb{tt % 2}"
                STT("dve", hb[tt % 2][:], xt[tt][:], rs4[:, tt:tt + 1], gain[0][:], ALU.mult, ALU.mult,
                    [f"xt{tt}", "rs4", "gain0"], [hbk])
                bk = nextbank()
                pT = psbf(bk)
                for c in range(8):
                    TR(pT[:, c * 128:(c + 1) * 128], hb[tt % 2][:, c * 128:(c + 1) * 128], ident_b[:], [hbk, "C_ident_b"], [f"ps{bk}"])
                CP("act" if tt % 2 else "dve", hT[:, :, tt * 128:(tt + 1) * 128], pT.rearrange("p (c k) -> p c k", k=128),
                   [f"ps{bk}"], ["hT"])

            def proj_fm(col0, ncols=128):
                bk = nextbank()
                for c in range(8):
                    MM(psb[bk][0:ncols, :], win[:, c, col0:col0 + ncols], hT[:, c, :], c == 0, c == 7, ["win", "hT"], [f"ps{bk}"])
                return bk

            def proj_tm(tt, col0, ncols, bk, off):
                for c in range(8):
                    MM(psb[bk][:, off:off + ncols], hT[:, c, tt * 128:(tt + 1) * 128], win[:, c, col0:col0 + ncols], c == 0, c == 7,
                       ["win", "hT"], [f"ps{bk}"])

            if KSTOP == 1:
                return
            for tt in range(4):
                bk = nextbank()
                proj_tm(tt, 0, 512, bk, 0)
                ACT(zs[:, tt, :], psb[bk][:], AF.Silu, [f"ps{bk}"], ["zs"])
            if KSTOP == 2:
                return
            for tt in range(4):
                for c in range(8):
                    MM(psb[4][:, 400 + tt * 8:408 + tt * 8], hT[:, c, tt * 128:(tt + 1) * 128], win[:, c, 1536:1544], c == 0, c == 7,
                       ["win", "hT"], ["ps4"])
            TT("dve", dtx[:], psb[4][:, 400:432].rearrange("p (t h) -> p t h", h=8),
               hv_bc[:, 0, :].unsqueeze(1).to_broadcast([128, 4, 8]), ALU.add, ["ps4", "hv_bc"], ["dtx"])
            ACT(dtx[:], dtx[:], AF.Exp, ["dtx"], ["dtx"])
            ACT(dt_all[:], dtx[:], AF.Ln, ["dtx"], ["dt_all"], bias=1.0)
            TT("dve", adt[:], dt_all[:], a_bc[:].unsqueeze(1).to_broadcast([128, 4, 8]), ALU.mult, ["dt_all", "a_bc"], ["adt"])
            if KSTOP == 3:
                return
            for j in range(8):
                bk = proj_fm(512 + 128 * j)
                u = ubuf[j % 2]
                uk = f"u{j % 2}"
                CP("dve", u[:, 0:3], halo[:, j, :], ["halo"], [uk])
                CP("dve", u[:, 3:515], psb[bk][:], [f"ps{bk}"], [uk])
                CP("pool", halo[:, j, :], u[:, 512:515], [uk], ["halo"])
                ta = cta[j % 2]
                tk = f"cta{j % 2}"
                ACT(ta[:], u[:, 0:512], AF.Copy, [uk, "cw"], [tk], scale=cw[:, j, 0:1])
                for k_ in range(1, 4):
                    STT("dve", ta[:], u[:, k_:k_ + 512], cw[:, j, k_:k_ + 1], ta[:], ALU.mult, ALU.add, [uk, "cw", tk], [tk])
                if j < 4:
                    dst, dk = xsT[:, j, :], "xsT"
                elif j < 6:
                    dst, dk = BT[:, j - 4, :], "BT"
                else:
                    dst, dk = CT[:, j - 6, :], "CT"
                ACT(dst, ta[:], AF.Silu, [tk, "cb"], [dk], bias=cb[:, j:j + 1])
            if KSTOP == 4:
                return
            for pr in range(2):
                bk = proj_fm(1544 + 128 * pr)
                ACT(sbq_st[:, pr, :], psb[bk][:], AF.Copy, [f"ps{bk}"], ["sbq_st"], scale=0.125)
                bk = proj_fm(1800 + 128 * pr)
                CP("dve", sbk_st[:, pr, :], psb[bk][:], [f"ps{bk}"], ["sbk_st"])
            for t2 in range(2):
                bk = nextbank()
                for q in range(2):
                    proj_tm(2 * t2 + q, 2056, 256, bk, q * 256)
                CP("act", sbv_st[:, 2 * t2:2 * t2 + 2, :], psb[bk][:].rearrange("p (t c) -> p t c", c=256), [f"ps{bk}"], ["sbv_st"])
            DMA(d_sbq[b].rearrange("(r p) s -> p r s", p=128)[:, :, t0:t0 + 512], sbq_st[:], ["sbq_st"], [f"d_sbq{b}_{g}"])
            DMA(d_sbk[b].rearrange("(r p) s -> p r s", p=128)[:, :, t0:t0 + 512], sbk_st[:], ["sbk_st"], [f"d_sbk{b}_{g}"])
            DMA(d_sbv[b][t0:t0 + 512, :].rearrange("(t p) c -> p t c", p=128), sbv_st[:], ["sbv_st"], [f"d_sbv{b}_{g}"])
            if KSTOP == 5:
                return
            for pr in range(2):
                bk = proj_fm(2312 + 128 * pr)
                CP("dve", moq32[:, pr, :], psb[bk][:], [f"ps{bk}"], ["moq32"])
                TS("pool", moq_st[:, pr, :], moq32[:, pr, :], 0.125, None, ALU.mult, None, ["moq32"], ["moq_st"])
                bk = proj_fm(2568 + 128 * pr)
                CP("act", mok32[:, pr, :], psb[bk][:], [f"ps{bk}"], ["mok32"])
                CP("pool", mok_st[:, pr, :], mok32[:, pr, :], ["mok32"], ["mok_st"])
                for e_ in range(2):
                    rows = slice(e_ * 64, e_ * 64 + 64)
                    P.add("dve", lambda e, pr=pr, g=g, e_=e_, rows=rows: e.tensor_reduce(
                        out=kmean[rows, pr, e_, 2 * g:2 * g + 2], in_=mok32[rows, pr, :].rearrange("p (b k) -> p b k", k=256),
                        axis=AX.X, op=ALU.add), ["mok32"], ["kmean"])
            for t2 in range(2):
                bk = nextbank()
                for q in range(2):
                    proj_tm(2 * t2 + q, 2824, 256, bk, q * 256)
                CP("dve", mov_st[:, 2 * t2:2 * t2 + 2, :], psb[bk][:].rearrange("p (t c) -> p t c", c=256), [f"ps{bk}"], ["mov_st"])
            DMA(d_moq[b].rearrange("(r p) s -> p r s", p=128)[:, :, t0:t0 + 512], moq_st[:], ["moq_st"], [f"d_moq{b}_{g}"])
            DMA(d_mok[b].rearrange("(r p) s -> p r s", p=128)[:, :, t0:t0 + 512], mok_st[:], ["mok_st"], [f"d_mok{b}_{g}"])
            DMA(d_mov[b][t0:t0 + 512, :].rearrange("(t p) c -> p t c", p=128), mov_st[:], ["mov_st"], [f"d_mov{b}_{g}"])
            if KSTOP == 6:
                return
            gps = psb[5][:, 0:16 * NBLK].rearrange("p (t h n) -> p t h n", t=4, h=4)
            for tt in range(4):
                for pr in range(2):
                    MM(gps[:, tt, 2 * pr:2 * pr + 2, :], moq32[:, pr, tt * 128:(tt + 1) * 128],
                       kmean[:, pr, :, :], True, True, ["moq32", "kmean"], ["ps5"])
            if KSTOP == 61:
                return
            for tt in range(4):
                own = 2 * g + tt // 2
                TT("dve", gm[:], gps[:, tt], vmask[:, own * NBLK:(own + 1) * NBLK].unsqueeze(1).to_broadcast([128, 4, NBLK]),
                   ALU.add, ["ps5", "C_vmask"], ["gm"])
                for h in range(4):
                    P.add("dve", lambda e, h=h: e.max(out=max8[:, h, :], in_=gm[:, h, :]), ["gm"], ["max8"])
                TS("dve", thr[:], max8[:, :, 2], -1e20, None, ALU.max, None, ["max8"], ["thr"])
                TT("dve", sel[:], gm[:], thr[:].unsqueeze(2).to_broadcast([128, 4, NBLK]), ALU.is_ge, ["gm", "thr"], ["sel"])
                TT("dve", sel[:], sel[:], ownmask[:, own * NBLK:(own + 1) * NBLK].unsqueeze(1).to_broadcast([128, 4, NBLK]),
                   ALU.add, ["sel", "C_ownmask"], ["sel"])
                TS("dve", selneg[:], sel[:].rearrange("p h n -> p (h n)"), -1.0, BIG, ALU.add, ALU.mult, ["sel"], ["selneg"])
                if dbg and g == NG - 1 and tt == 3:
                    DMA(dbg_t[:, 0:4 * NBLK], gm[:].rearrange("p h n -> p (h n)"), ["gm"], ["dbg_t"])
                    DMA(dbg_t[:, 64:96], max8[:].rearrange("p h n -> p (h n)"), ["max8"], ["dbg_t"])
                    DMA(dbg_t[:, 96:100], thr[:], ["thr"], ["dbg_t"])
                    DMA(dbg_t[:, 128:128 + 4 * NBLK], sel[:].rearrange("p h n -> p (h n)"), ["sel"], ["dbg_t"])
                if KSTOP == 62:
                    continue
                bk = nextbank()
                TR(psbf(bk)[0:4 * NBLK, 0:128], selneg[:], ident_b[:], ["selneg", "C_ident_b"], [f"ps{bk}"])
                CP("act", selT_st[0:4 * NBLK, tt * 128:(tt + 1) * 128], psbf(bk)[0:4 * NBLK, 0:128], [f"ps{bk}"], ["selT_st"])
            if KSTOP == 62:
                return
            DMA(d_sel[b][:, t0:t0 + 512], selT_st[0:4 * NBLK, :], ["selT_st"], [f"d_sel{b}_{g}"])
            if KSTOP == 7:
                return
            for tt in range(4):
                bk = nextbank()
                for j in range(4):
                    TR(psb[bk][:, j * 128:(j + 1) * 128], xsT[:, j, tt * 128:(tt + 1) * 128], ident_f[:], ["xsT", "C_ident_f"], [f"ps{bk}"])
                CP("act" if tt % 2 else "dve", xs_tok[tt][:], psb[bk][:], [f"ps{bk}"], [f"xstok{tt}"])
                bk = nextbank()
                for gi in range(2):
                    TR(psbf(bk)[:, gi * 128:(gi + 1) * 128], BT[:, gi, tt * 128:(tt + 1) * 128], ident_b[:], ["BT", "C_ident_b"], [f"ps{bk}"])
                CP("dve", Btok[:, tt, :, :], psbf(bk)[:, 0:256].rearrange("p (g n) -> p g n", n=128), [f"ps{bk}"], ["Btok"])
            if KSTOP == 8:
                return
            def ssd_chunk(tt):
                tsl = slice(tt * 128, (tt + 1) * 128)
                xs3 = xs_tok[tt][:].rearrange("p (h d) -> p h d", d=64)
                xk = f"xstok{tt}"
                negac, ea, cdb, dtd = negac4[:, tt, :], ea4[:, tt, :], cdb4[:, tt, :], dtd4[:, tt, :]
                nk, ek, ck, dk_ = f"negac{tt}", f"ea{tt}", f"cdb{tt}", f"dtd{tt}"
                TT("dve", rhs_cum[:], tri_incl[:].unsqueeze(1).to_broadcast([128, 8, 128]),
                   adt[:, tt, :].unsqueeze(2).to_broadcast([128, 8, 128]), ALU.mult, ["C_tri_incl", "adt"], ["rhs_cum"])
                rc = rhs_cum[:].rearrange("p h l -> p (h l)")
                for half in range(2):
                    pb = psb[2 + half]
                    MM(pb[:], ones_f[:], rc[:, half * 512:(half + 1) * 512], True, False, ["C_ones_f", "rhs_cum"], [f"ps{2 + half}"])
                    MM(pb[:], ident_b[:], negmask_b[:, half * 512:(half + 1) * 512], False, True, ["C_ident_b", "C_negmask_b"], [f"ps{2 + half}"])
                MM(psb[4][:, 0:8], tri_incl[:], adt[:, tt, :], True, True, ["C_tri_incl", "adt"], ["ps4"])
                yield
                ACT(negac, psb[4][:, 0:8], AF.Copy, ["ps4"], [nk], scale=-1.0)
                ACT(ea, psb[4][:, 0:8], AF.Exp, ["ps4"], [ek])
                for half in range(2):
                    ACT(cdb[:, half * 4:(half + 1) * 4], psb[2 + half][:].rearrange("p (h l) -> p h l", l=128)[:, :, 127], AF.Exp,
                        [f"ps{2 + half}"], [ck])
                for h in range(8):
                    ACT(Eb[:, h, :], psb[2 + h // 4][:, (h % 4) * 128:(h % 4 + 1) * 128], AF.Exp, [f"ps{2 + h // 4}", nk], ["Eb"],
                        bias=negac[:, h:h + 1])
                for gi in range(2):
                    MM(psb[4][:, 128 + gi * 128:256 + gi * 128], BT[:, gi, tsl], CT[:, gi, tsl], True, True, ["BT", "CT"], ["ps4"])
                yield
                for gi in range(2):
                    TT("dve", MT[:, gi * 4:(gi + 1) * 4, :], Eb[:, gi * 4:(gi + 1) * 4, :],
                       psb[4][:, 128 + gi * 128:256 + gi * 128].unsqueeze(1).to_broadcast([128, 4, 128]), ALU.mult,
                       ["Eb", "ps4"], ["MT"])
                TT("dve", dtd, dt_all[:, tt, :], Eb[:, :, 127], ALU.mult, ["dt_all", "Eb"], [dk_])
                TT("dve", xdt[:], xs3, dt_all[:, tt, :].unsqueeze(2).to_broadcast([128, 8, 64]), ALU.mult, [xk, "dt_all"], ["xdt"])
                TT("pool", xdt2[:], xs3, dtd.unsqueeze(2).to_broadcast([128, 8, 64]), ALU.mult, [xk, dk_], ["xdt2"])
                yield
                x2f = xdt2[:].rearrange("p h d -> p (h d)")
                x1f = xdt[:].rearrange("p h d -> p (h d)")
                for gi in range(2):
                    MM(psb[5][:, gi * 256:(gi + 1) * 256], Btok[:, tt, gi, :], x2f[:, gi * 256:(gi + 1) * 256], True, True,
                       ["Btok", "xdt2"], ["ps5"])
                for gi in range(2):
                    MM(psb[6][:, gi * 256:(gi + 1) * 256], CT[:, gi, tsl], Sb[:, gi * 256:(gi + 1) * 256], True, True,
                       ["CT", "Sb"], ["ps6"])
                for h in range(8):
                    MM(psb[7][:, h * 64:(h + 1) * 64], MT[:, h, :], x1f[:, h * 64:(h + 1) * 64], True, True, ["MT", "xdt"], ["ps7"])
                yield
                TT("dve", S32[:].rearrange("p (h d) -> p h d", d=64), S32[:].rearrange("p (h d) -> p h d", d=64),
                   cdb.unsqueeze(2).to_broadcast([128, 8, 64]), ALU.mult, ["S32", ck], ["S32"])
                TT("dve", S32[:], S32[:], psb[5][:], ALU.add, ["S32", "ps5"], ["S32"])
                CP("act", Sb[:], S32[:], ["S32"], ["Sb"])
                TT("dve", y1[:].rearrange("p (h d) -> p h d", d=64), psb[6][:].rearrange("p (h d) -> p h d", d=64),
                   ea.unsqueeze(2).to_broadcast([128, 8, 64]), ALU.mult, ["ps6", ek], ["y1"])
                TT("dve", y1[:], y1[:], psb[7][:], ALU.add, ["y1", "ps7"], ["y1"])
                TT("pool", y2[:].rearrange("p (h d) -> p h d", d=64), xs3, dsk_bc[:], ALU.mult, [xk, "dsk_bc"], ["y2"])
                TT("pool", y2[:], y2[:], y1[:], ALU.add, ["y2", "y1"], ["y2"])
                TT("dve", y3[:], y2[:], zs[:, tt, :], ALU.mult, ["y2", "zs"], ["y3"])
                for gi in range(2):
                    jb, jk = nextjunk()
                    ACT(jb[:, 0:256], y3[:, gi * 256:(gi + 1) * 256], AF.Square, ["y3"], [jk, f"ssq{gi}"], scale=1.0 / 16,
                        accum_out=ssq[:, gi:gi + 1])
                ACT(rsq[:], ssq[:], AF.Ln, ["ssq0", "ssq1"], ["rsq"], bias=EPS)
                ACT(rsq[:], rsq[:], AF.Exp, ["rsq"], ["rsq"], scale=-0.5)
                for gi in range(2):
                    STT("dve", yn[:, gi * 256:(gi + 1) * 256], y3[:, gi * 256:(gi + 1) * 256], rsq[:, gi:gi + 1],
                        ssdn_bc[:, gi * 256:(gi + 1) * 256], ALU.mult, ALU.mult, ["y3", "rsq", "ssdn_bc"], ["yn"])
                yield
                bk = nextbank()
                for j in range(4):
                    TR(psbf(bk)[:, j * 128:(j + 1) * 128], yn[:, j * 128:(j + 1) * 128], ident_b[:], ["yn", "C_ident_b"], [f"ps{bk}"])
                CP("act", mixT_ssd[:, :, tsl], psbf(bk)[:, 0:512].rearrange("p (c k) -> p c k", k=128), [f"ps{bk}"], ["mixT_ssd"])

            for tt in range(4):
                for _ in ssd_chunk(tt):
                    pass
            DMA(d_ssd[b].rearrange("(c p) s -> p c s", p=128)[:, :, t0:t0 + 512], mixT_ssd[:], ["mixT_ssd"], [f"d_ssd{b}_{g}"])

    arena_state["off"] = base_off
    p2_kT = SB([128, 2, S], BF16, "p2_kT")
    p2_v = SB([128, NT, 256], BF16, "p2_v")
    p2_q = [SB([128, 2, 512], BF16, f"p2_q{i}") for i in range(3)]
    p2_nq = [SB([128, 2, 512], BF16, f"p2_nq{i}") for i in range(3)]
    p2_e = [SB([128, 512], F32, f"p2_e{i}") for i in range(2)]
    p2_sp = [SB([128, 512], BF16, f"p2_sp{i}") for i in range(2)]
    p2_S = SB([128, 512], BF16, "p2_S")
    p2_w = [SB([128, 512], BF16, f"p2_w{i}") for i in range(2)]
    p23_y = SB([64, 4, 512], F32, "p23_y")
    p23_sq = SB([64, 4, 512], BF16, "p23_sq")
    p23_rs = SB([64, 512], F32, "p23_rs")
    p23_mix = SB([64, 4, 512], BF16, "p23_mix")
    p23_nw = SB([64, 4], F32, "p23_nw")

    def head_norm_store(l, b, g, dst, nb=6):
        t0 = g * 512
        for h in range(4):
            ACT(p23_sq[:, h, :], p23_y[:, h, :], AF.Square, ["p23_y"], ["p23_sq"])
        for h in range(4):
            MM(psb[nb][0:64, :], ones_b[0:64, 0:64], p23_sq[:, h, :], h == 0, h == 3, ["C_ones_b", "p23_sq"], [f"ps{nb}"])
        ACT(p23_rs[:], psb[nb][0:64, :], AF.Ln, [f"ps{nb}"], ["p23_rs"], scale=1.0 / 256, bias=EPS)
        ACT(p23_rs[:], p23_rs[:], AF.Exp, ["p23_rs"], ["p23_rs"], scale=-0.5)
        for h in range(4):
            STT("dve", p23_mix[:, h, :], p23_y[:, h, :], p23_nw[:, h:h + 1], p23_rs[:], ALU.mult, ALU.mult,
                ["p23_y", "p23_nw", "p23_rs"], ["p23_mix"])
        DMA(dst.rearrange("h d s -> d h s")[:, :, t0:t0 + 512], p23_mix[:], ["p23_mix"], [f"{dst.tensor.name}_{g}"])

    def run_pipeline(gens):
        active = []
        gi = iter(gens)
        more = True
        while True:
            if more:
                try:
                    active.append(next(gi))
                except StopIteration:
                    more = False
            if not active:
                break
            for gen in list(active):
                try:
                    next(gen)
                except StopIteration:
                    active.remove(gen)

    NB2 = 3
    p2_e3 = [SB([128, 512], F32, f"p2_e3{i}") for i in range(NB2)]
    p2_sp3 = [SB([128, 512], BF16, f"p2_sp3{i}") for i in range(NB2)]
    p2_w3 = [SB([128, 512], BF16, f"p2_w3{i}") for i in range(NB2)]
    p2_S2 = [SB([128, 512], BF16, f"p2_S2{i}") for i in range(4)]
    p2_t4 = [SB([128, 512], F32, f"p2_t4{i}") for i in range(NB2)]

    p2set = (p23_y, p23_sq, p23_rs, p23_mix, p23_nw)

    def pass2(l, b):
        nonlocal p23_y, p23_sq, p23_rs, p23_mix, p23_nw
        p23_y, p23_sq, p23_rs, p23_mix, p23_nw = p2set
        DMA(p23_nw[:], sbn[l], (), ["p23_nw"])

        def loads(g):
            t0 = g * 512
            DMA(p2_kT[:, :, t0:t0 + 512], d_sbk[b].rearrange("(r p) s -> p r s", p=128)[:, :, t0:t0 + 512],
                [f"d_sbk{b}_{g}"], [f"p2_kT_{g}"])
            DMA(p2_v[:, 4 * g:4 * g + 4, :], d_sbv[b][t0:t0 + 512, :].rearrange("(t p) c -> p t c", p=128),
                [f"d_sbv{b}_{g}"], [f"p2_v_{g}"])
            q, nq = p2_q[g % 3], p2_nq[g % 3]
            DMA(q[:], d_sbq[b].rearrange("(r p) s -> p r s", p=128)[:, :, t0:t0 + 512], [f"d_sbq{b}_{g}"], [f"p2_q{g % 3}"])

        def it_gen(g, h, kb, n):
            q, nq = p2_q[g % 3], p2_nq[g % 3]
            qk, nqk = f"p2_q{g % 3}", f"p2_nq{g % 3}"
            pr, e_ = h // 2, h % 2
            rows = slice(e_ * 64, e_ * 64 + 64)
            yb = 4 + (h % 2)
            last = 4 * g + 3
            gk = kb // 4
            qoff = max(0, kb - 4 * g) * 128
            W = 512 - qoff
            diag = kb >= 4 * g
            i3 = n % NB2
            b1, b2 = n % 2, 2 + n % 2
            ksl = slice(kb * 128, (kb + 1) * 128)
            e3, sp3, w3 = p2_e3[i3], p2_sp3[i3], p2_w3[i3]
            ek, spk, wk = f"p2_e3{i3}", f"p2_sp3{i3}", f"p2_w3{i3}"
            par = (last - kb) % 2
            S_in, S_out = p2_S2[2 * e_ + par], p2_S2[2 * e_ + 1 - par]
            Sik, Sok = f"p2_S2{2 * e_ + par}", f"p2_S2{2 * e_ + 1 - par}"
            if kb == last:
                if h == 0 and g + 1 < NG:
                    loads(g + 1)
                MM(psb[yb][0:64, :], zeros_b[:, 0:64], ones_b[:, 0:512], True, False, ["C_zeros_b", "C_ones_b"], [f"ps{yb}"])
            MM(psb[b1][:, 0:W], p2_kT[rows, pr, ksl], q[rows, pr, qoff:512], True, True, [f"p2_kT_{gk}", qk], [f"ps{b1}"])
            yield
            ACT(e3[:, 0:W], psb[b1][:, 0:W], AF.Exp, [f"ps{b1}"], [ek])
            ACT(sp3[:, 0:W], e3[:, 0:W], AF.Ln, [ek], [spk], bias=1.0)
            if diag:
                TT("pool", sp3[:, 0:128], sp3[:, 0:128], tri_strict[:], ALU.mult, [spk, "C_tri_strict"], [spk])
            yield
            MM(psb[b2][:, 0:W], ut_b[:], sp3[:, 0:W], True, kb == last, ["C_ut_b", spk], [f"ps{b2}"])
            if kb < last:
                MM(psb[b2][:, 0:W], ones_b[:, 0:128], S_in[:, qoff:512], False, True, ["C_ones_b", Sik], [f"ps{b2}"])
            if kb > 0:
                if kb == last:
                    if qoff > 0:
                        MS("pool", S_out[:, 0:qoff], 0.0, [Sok])
                    CP("dve", S_out[:, qoff:512], sp3[:, 0:W], [spk], [Sok])
                else:
                    if qoff > 0:
                        CP("pool", S_out[:, 0:qoff], S_in[:, 0:qoff], [Sik], [Sok])
                    TT("dve", S_out[:, qoff:512], S_in[:, qoff:512], sp3[:, 0:W], ALU.add, [Sik, spk], [Sok])
            yield
            t4, t4k = p2_t4[i3], f"p2_t4{i3}"
            ACT(t4[:, 0:W], psb[b2][:, 0:W], AF.Exp, [f"ps{b2}"], [t4k], scale=-1.0)
            TT("dve", w3[:, 0:W], t4[:, 0:W], e3[:, 0:W], ALU.mult, [t4k, ek], [wk])
            if diag:
                TT("pool", w3[:, 0:128], w3[:, 0:128], tri_strict[:], ALU.mult, [wk, "C_tri_strict"], [wk])
            yield
            MM(psb[yb][0:64, qoff:512], p2_v[:, kb, h * 64:(h + 1) * 64], w3[:, 0:W], False, kb == 0,
               [f"p2_v_{gk}", wk], [f"ps{yb}"])
            if kb == 0:
                CP("dve", p23_y[:, h, :], psb[yb][0:64, :], [f"ps{yb}"], ["p23_y"])
                if h == 3:
                    head_norm_store(l, b, g, d_msb[b])

        def all_iters():
            n = 0
            for g in range(NG):
                for h in range(4):
                    for kb in range(4 * g + 3, -1, -1):
                        yield it_gen(g, h, kb, n)
                        n += 1

        loads(0)
        run_pipeline(all_iters())

    arena_state["off"] = base_off
    p3_kT = [SB([96, S], BF16, f"p3_kT{h}") for h in range(4)]
    p3_va = SB([128, NT, 4, 128], BF16, "p3_va")
    p3_q = [[SB([96, 512], BF16, f"p3_q{i}_{h}") for h in range(4)] for i in range(3)]
    p3_p = [SB([128, 512], BF16, f"p3_p{i}") for i in range(3)]
    arena_state["off"] = max(arena_state["off"], 0)
    p3_y = SB([64, 4, 512], F32, "p3_y")
    p3_sq = SB([64, 4, 512], BF16, "p3_sq")
    p3_rs = SB([64, 512], F32, "p3_rs")
    p3_mix = SB([64, 4, 512], BF16, "p3_mix")
    p3_nw = SB([64, 4], F32, "p3_nw")

    def pass3(l, b):
        nonlocal p23_y, p23_sq, p23_rs, p23_mix, p23_nw
        p23_y, p23_sq, p23_rs, p23_mix, p23_nw = p3_y, p3_sq, p3_rs, p3_mix, p3_nw
        DMA(p23_nw[:], mon[l], (), ["p23_nw"])
        for h in range(4):
            MS("pool", p3_kT[h][:], 0.0, [f"p3_kT{h}_c"])
            DMA(p3_kT[h][64:64 + NBLK + 1, :], cd["kaug"], [], [f"p3_kT{h}_c"])
            for i in range(3):
                MS("pool", p3_q[i][h][:], 0.0, [f"p3_q{i}_{h}"])
                DMA(p3_q[i][h][64 + NBLK:64 + NBLK + 1, :], cd["alibi_row"][h:h + 1, :], [], [f"p3_q{i}_{h}"])

        for h in range(4):
            MS("pool", p3_va[:, :, h, 64:128], 1.0, ["p3_va_c"])

        def loads(g):
            t0 = g * 512
            for h in range(4):
                DMA(p3_kT[h][0:64, t0:t0 + 512], d_mok[b][h * 64:(h + 1) * 64, t0:t0 + 512], [f"d_mok{b}_{g}", f"p3_kT{h}_c"], [f"p3_kT{h}_{g}"])
            for h in range(4):
                DMA(p3_va[:, 4 * g:4 * g + 4, h, 0:64], d_mov[b][t0:t0 + 512, h * 64:(h + 1) * 64].rearrange("(t p) d -> p t d", p=128),
                    [f"d_mov{b}_{g}", "p3_va_c"], [f"p3_v_{g}"])
            i_ = g % 3
            for h in range(4):
                qk = f"p3_q{i_}_{h}"
                DMA(p3_q[i_][h][0:64, :], d_moq[b][h * 64:(h + 1) * 64, t0:t0 + 512], [f"d_moq{b}_{g}"], [qk])
                DMA(p3_q[i_][h][64:64 + NBLK, :], d_sel[b][h * NBLK:(h + 1) * NBLK, t0:t0 + 512], [f"d_sel{b}_{g}"], [qk])

        def it_gen(g, h, kb, n):
            i_ = g % 3
            qk = f"p3_q{i_}_{h}"
            qa = p3_q[i_][h]
            yb = 4 + (h % 2)
            db = 6 + (h % 2)
            gk = kb // 4
            qoff = max(0, kb - 4 * g) * 128
            W = 512 - qoff
            diag = kb >= 4 * g
            rel = 4 * g + 3 - kb
            i3 = n % 3
            b1 = n % 3
            lastk = kb == 4 * g + 3
            pp, pk = p3_p[i3], f"p3_p{i3}"
            if kb == 0:
                if h == 0 and g + 1 < NG:
                    loads(g + 1)
                MM(psb[yb][:, :], zeros_b[:, 0:128], ones_b[:, 0:512], True, False, ["C_zeros_b", "C_ones_b"], [f"ps{yb}"])
            MM(psb[b1][:, 0:W], p3_kT[h][:, kb * 128:(kb + 1) * 128], qa[:, qoff:512], True, True,
               [f"p3_kT{h}_{gk}", f"p3_kT{h}_c", qk], [f"ps{b1}"])
            yield
            ACT(pp[:, 0:W], psb[b1][:, 0:W], AF.Exp, [f"ps{b1}", "C_alibi_kb"], [pk], bias=alibi_kb[:, h, rel:rel + 1])
            if diag:
                TT("dve", pp[:, 0:128], pp[:, 0:128], tri_incl[:], ALU.mult, [pk, "C_tri_incl"], [pk])
            yield
            MM(psb[yb][:, qoff:512], p3_va[:, kb, h, :], pp[:, 0:W], False, lastk, [f"p3_v_{gk}", "p3_va_c", pk], [f"ps{yb}"])
            if lastk:
                CP("act", p23_y[:, h, :], psb[yb][0:64, :], [f"ps{yb}"], ["p23_y"])
                CP("act", p23_rs[:], psb[yb][64:128, :], [f"ps{yb}"], ["p23_rs"])
                P.add("dve", lambda e: e.reciprocal(out=p23_rs[:], in_=p23_rs[:]), ["p23_rs"], ["p23_rs"])
                TT("dve", p23_y[:, h, :], p23_y[:, h, :], p23_rs[:], ALU.mult, ["p23_y", "p23_rs"], ["p23_y"])
                if h == 3:
                    head_norm_store(l, b, g, d_mmo[b], nb=3)

        def all_iters():
            n = 0
            for g in range(NG):
                for h in range(4):
                    for kb in range(0, 4 * g + 4):
                        yield it_gen(g, h, kb, n)
                        n += 1

        loads(0)
        run_pipeline(all_iters())

    arena_state["off"] = base_off
    p45_tmp = [SB([128, D], F32, f"p45_tmp{i}") for i in range(2)]
    p45_xo = [SB([128, D], F32, f"p45_xo{i}") for i in range(2)]
    p45_ss = SB([128, 4], F32, "p45_ss")
    base45 = arena_state["off"]
    p4_woa = SB([128, 4, D], BF16, "p4_woa")
    p4_wob = SB([64, 8, D], BF16, "p4_wob")
    p4_ms = [SB([128, 4, 512], BF16, f"p4_ms{i}") for i in range(2)]
    p4_mh = [SB([64, 8, 512], BF16, f"p4_mh{i}") for i in range(2)]

    def norm_resid_store(pb, xtile, xk, i2, dst_ap, dkey):
        for nh in range(2):
            jb, jk = nextjunk()
            ACT(jb[:, 0:512], psb[pb + nh][:], AF.Square, [f"ps{pb + nh}"], [jk, f"p45_ss{nh}"], scale=1.0 / 32,
                accum_out=p45_ss[:, nh:nh + 1])
        TT("dve", p45_ss[:, 2:3], p45_ss[:, 0:1], p45_ss[:, 1:2], ALU.add, ["p45_ss0", "p45_ss1"], ["p45_ss2"])
        ACT(p45_ss[:, 3:4], p45_ss[:, 2:3], AF.Ln, ["p45_ss2"], ["p45_ss3"], bias=EPS)
        ACT(p45_ss[:, 3:4], p45_ss[:, 3:4], AF.Exp, ["p45_ss3"], ["p45_ss3"], scale=-0.5)
        tmp, tk = p45_tmp[i2], f"p45_tmp{i2}"
        for nh in range(2):
            STT("dve", tmp[:, nh * 512:(nh + 1) * 512], psb[pb + nh][:], p45_ss[:, 3:4], gain[1][:, nh * 512:(nh + 1) * 512],
                ALU.mult, ALU.mult, [f"ps{pb + nh}", "p45_ss3", "gain1"], [tk])
        xo, xok = p45_xo[i2], f"p45_xo{i2}"
        TT("pool", xo[:], tmp[:], xtile, ALU.add, [tk, xk], [xok])
        DMA(dst_ap, xo[:], [xok], [dkey])

    def pass4(l, b, xsrc, srckey):
        load_gain(1, l, 1)
        DMA(p4_woa[:], w_out[l, 0:512, :].rearrange("(c p) n -> p c n", p=128), (), ["p4_woa"], eng="pool")
        DMA(p4_wob[:], w_out[l, 512:1024, :].rearrange("(j p) n -> p j n", p=64), (), ["p4_wob"], eng="pool")
        n = 0
        for g in range(NG):
            t0 = g * 512
            ms, mh = p4_ms[g % 2], p4_mh[g % 2]
            msk, mhk = f"p4_ms{g % 2}", f"p4_mh{g % 2}"
            DMA(ms[:], d_ssd[b].rearrange("(c p) s -> p c s", p=128)[:, :, t0:t0 + 512], [f"d_ssd{b}_{g}"], [msk])
            DMA(mh[:, 0:4, :], d_msb[b].rearrange("h d s -> d h s")[:, :, t0:t0 + 512], [f"d_msb{b}_{g}"], [mhk])
            DMA(mh[:, 4:8, :], d_mmo[b].rearrange("h d s -> d h s")[:, :, t0:t0 + 512], [f"d_mmo{b}_{g}"], [mhk])
            for tt in range(4):
                tok = slice(t0 + tt * 128, t0 + (tt + 1) * 128)
                xi = n % 4
                DMA(xt[xi][:], xsrc[tok, :], [f"{srckey}_{(t0 + tt * 128) // 1024}"], [f"xt{xi}"])
                pb = 4 + 2 * (n % 2)
                tsl = slice(tt * 128, (tt + 1) * 128)
                for nh in range(2):
                    cs = slice(nh * 512, (nh + 1) * 512)
                    for c in range(4):
                        MM(psb[pb + nh][:], ms[:, c, tsl], p4_woa[:, c, cs], c == 0, False, [msk, "p4_woa"], [f"ps{pb + nh}"])
                    for j in range(8):
                        MM(psb[pb + nh][:], mh[:, j, tsl], p4_wob[:, j, cs], False, j == 7, [mhk, "p4_wob"], [f"ps{pb + nh}"])
                norm_resid_store(pb, xt[xi][:], f"xt{xi}", n % 2, d_x1[b][tok, :], f"d_x1{b}_{(t0 + tt * 128) // 1024}")
                n += 1

    arena_state["off"] = base45
    p5_hT = SB([128, 8, 1024], BF16, "p5_hT")
    p5_act = SB([128, NHC, 1024], BF16, "p5_act")
    p5_wd = SB([128, NHC, D], BF16, "p5_wd")
    p5_wg = [SB([128, 8, 256], BF16, f"p5_wg{i}") for i in range(2)]
    p5_wu = [SB([128, 8, 256], BF16, f"p5_wu{i}") for i in range(2)]
    p5_sg = [SB([128, 512], F32, f"p5_sg{i}") for i in range(2)]

    def pass5(l, b, xdst, dstkey):
        load_gain(0, l, 2)
        load_gain(1, l, 3)
        for (c0, c1) in [(0, 11), (11, 22)]:
            DMA(p5_wd[:, c0:c1, :], w_down[l, c0 * 128:c1 * 128, :].rearrange("(c p) n -> p c n", p=128), (), ["p5_wd"], eng="pool")
        n = 0
        wi = 0
        for G in range(S // 1024 if not ng_limit else max(1, NG // 2)):
            T0 = G * 1024
            for tt in range(8):
                xi = n % 4
                n += 1
                tok = slice(T0 + tt * 128, T0 + (tt + 1) * 128)
                DMA(xt[xi][:], d_x1[b][tok, :], [f"d_x1{b}_{G}"], [f"xt{xi}"])
                jb, jk = nextjunk()
                ACT(jb[:], xt[xi][:], AF.Square, [f"xt{xi}"], [jk, "p5_ssa"], scale=1.0 / 32, accum_out=ss4[:, 0:1])
                ACT(rs4[:, 0:1], ss4[:, 0:1], AF.Ln, ["p5_ssa"], ["p5_rsa"], bias=EPS)
                ACT(rs4[:, 0:1], rs4[:, 0:1], AF.Exp, ["p5_rsa"], ["p5_rsa"], scale=-0.5)
                hbk = f"hb{tt % 2}"
                STT("dve", hb[tt % 2][:], xt[xi][:], rs4[:, 0:1], gain[0][:], ALU.mult, ALU.mult, [f"xt{xi}", "p5_rsa", "gain0"], [hbk])
                bk = 4 + (tt % 4)
                pT = psbf(bk)
                for c in range(8):
                    TR(pT[:, c * 128:(c + 1) * 128], hb[tt % 2][:, c * 128:(c + 1) * 128], ident_b[:], [hbk, "C_ident_b"], [f"ps{bk}"])
                CP("act" if tt % 2 else "dve", p5_hT[:, :, tt * 128:(tt + 1) * 128], pT.rearrange("p (c k) -> p c k", k=128),
                   [f"ps{bk}"], ["p5_hT"])
            it = 0
            for hq in range(0, NHC, 2):
                nq_ = min(2, NHC - hq)
                wg, wu = p5_wg[wi % 2], p5_wu[wi % 2]
                wgk, wuk = f"p5_wg{wi % 2}", f"p5_wu{wi % 2}"
                wi += 1
                cols = slice(hq * 128, (hq + nq_) * 128)
                DMA(wg[:, :, 0:nq_ * 128], w_gate[l, :, cols].rearrange("(c p) n -> p c n", p=128), (), [wgk], eng="pool")
                DMA(wu[:, :, 0:nq_ * 128], w_up[l, :, cols].rearrange("(c p) n -> p c n", p=128), (), [wuk], eng="pool")
                for hl in range(nq_):
                    hcx = hq + hl
                    for half in range(2):
                        i2 = it % 2
                        it += 1
                        ba, bb = 2 * i2, 2 * i2 + 1
                        hs = slice(half * 512, (half + 1) * 512)
                        for c in range(8):
                            MM(psb[ba][:], wg[:, c, hl * 128:(hl + 1) * 128], p5_hT[:, c, hs], c == 0, c == 7, [wgk, "p5_hT"], [f"ps{ba}"])
                        for c in range(8):
                            MM(psb[bb][:], wu[:, c, hl * 128:(hl + 1) * 128], p5_hT[:, c, hs], c == 0, c == 7, [wuk, "p5_hT"], [f"ps{bb}"])
                        ACT(p5_sg[i2][:], psb[ba][:], AF.Silu, [f"ps{ba}"], [f"p5_sg{i2}"])
                        TT("dve", p5_act[:, hcx, hs], p5_sg[i2][:], psb[bb][:], ALU.mult, [f"p5_sg{i2}", f"ps{bb}"], ["p5_act"])
            for tt in range(8):
                tok = slice(T0 + tt * 128, T0 + (tt + 1) * 128)
                xi = n % 4
                n += 1
                DMA(xt[xi][:], d_x1[b][tok, :], [f"d_x1{b}_{G}"], [f"xt{xi}"])
                pb = 4 + 2 * (tt % 2)
                tsl = slice(tt * 128, (tt + 1) * 128)
                for nh in range(2):
                    for hcx in range(NHC):
                        MM(psb[pb + nh][:], p5_act[:, hcx, tsl], p5_wd[:, hcx, nh * 512:(nh + 1) * 512], hcx == 0, hcx == NHC - 1,
                           ["p5_act", "p5_wd"], [f"ps{pb + nh}"])
                norm_resid_store(pb, xt[xi][:], f"xt{xi}", tt % 2, xdst[tok, :], f"{dstkey}_{G}")

    for l in range(DEPTH):
        for b in range(NSEQ):
            xsrc = x_in[b] if l == 0 else d_x2[b]
            srckey = f"xin{b}" if l == 0 else f"d_x2{b}"
            if 1 in passes:
                barrier()
                pass1(l, b, xsrc)
            if 2 in passes:
                barrier()
                pass2(l, b)
            if 3 in passes:
                barrier()
                pass3(l, b)
            if 4 in passes:
                barrier()
                pass4(l, b, xsrc, srckey)
            if 5 in passes:
                barrier()
                if l == DEPTH - 1:
                    pass5(l, b, out[b], f"out{b}")
                else:
                    pass5(l, b, d_x2[b], f"d_x2{b}")
    finals = list(P.dma_prev.values())
    P.emit(final_waits=finals)
    return nc, hc


def prep_inputs(inputs, DEPTH):
    f = lambda a: np.ascontiguousarray(np.asarray(a, dtype=np.float32))
    d = {}
    for k in ["w_in", "w_out", "w_gate", "w_up", "w_down"]:
        d[k] = f(inputs[k])
    d["vec1024"] = f(np.stack([inputs["pre_mix_norm"], inputs["post_mix_norm"], inputs["pre_ffn_norm"], inputs["post_ffn_norm"]], axis=1))
    cwv = np.asarray(inputs["conv_w"], np.float32)
    d["convw"] = f(cwv.reshape(DEPTH, 4, 8, 128).transpose(0, 3, 2, 1))
    d["convb"] = f(np.asarray(inputs["conv_b"], np.float32).reshape(DEPTH, 8, 128).transpose(0, 2, 1))
    d["hv"] = f(np.stack([inputs["dt_bias"], inputs["a_log"], inputs["d_skip"]], axis=1))
    d["ssdn"] = f(inputs["ssd_norm"])
    d["sbn"] = f(np.asarray(inputs["sb_norm"], np.float32).reshape(DEPTH, 4, 64).transpose(0, 2, 1))
    d["mon"] = f(np.asarray(inputs["moba_norm"], np.float32).reshape(DEPTH, 4, 64).transpose(0, 2, 1))
    return d


def kernel(**inputs):
    x = np.asarray(inputs["x"], np.float32)
    B, S, _ = x.shape
    DEPTH = inputs["w_in"].shape[0]
    NCORE = 8
    NSEQ = B // NCORE
    nc, hc = build(S, NSEQ, DEPTH)
    shared = prep_inputs(inputs, DEPTH)
    for k, v in hc.items():
        shared["c_" + k] = v
    in_maps = []
    for c in range(NCORE):
        m = dict(shared)
        m["x"] = np.ascontiguousarray(x[c * NSEQ:(c + 1) * NSEQ])
        in_maps.append(m)
    res = run_bass_kernel_spmd(nc, in_maps, core_ids=list(range(NCORE)))
    return np.concatenate([r["out"] for r in res.results], axis=0).astype(np.float32)
```

```python
import contextlib
import numpy as np
import ml_dtypes
import concourse.bass as bass
import concourse.mybir as mybir
from concourse.bass_utils import run_bass_kernel_spmd

F32 = mybir.dt.float32
BF16 = mybir.dt.bfloat16
AF = mybir.ActivationFunctionType
ALU = mybir.AluOpType
AX = mybir.AxisListType

D = 1024
INC = 3080
FH = 2816
NHC = 22
EPS = 1e-6
BIG = 30000.0
ENGS = ["pe", "act", "dve", "pool", "sp"]
EPOCH = 12000
NDMA = 12


class Prog:
    def __init__(self, nc):
        self.nc = nc
        self.ins = {e: [] for e in ENGS}
        self.lastw = {}
        self.readers = {}
        self.dma_slot = {e: 0 for e in ENGS}
        self.dma_prev = {}

    def add(self, eng, fn, reads=(), writes=(), dma=False):
        lst = self.ins[eng]
        me = dict(eng=eng, idx=len(lst), fn=fn, deps=set(), dma=dma, sig=False)
        deps = me["deps"]
        for r in reads:
            w = self.lastw.get(r)
            if w is not None:
                deps.add((w["eng"], w["idx"]))
        for wr in writes:
            w = self.lastw.get(wr)
            if w is not None and (w["eng"] != eng or w["dma"] or dma or eng != "pe"):
                deps.add((w["eng"], w["idx"]))
            for rd in self.readers.get(wr, ()):
                if rd["eng"] != eng or rd["dma"] or dma or eng != "pe":
                    deps.add((rd["eng"], rd["idx"]))
        if dma:
            slot = self.dma_slot[eng]
            self.dma_slot[eng] = (slot + 1) % NDMA
            me["slot"] = slot
            prev = self.dma_prev.get((eng, slot))
            if prev is not None:
                deps.add((prev["eng"], prev["idx"]))
            self.dma_prev[(eng, slot)] = me
        deps.discard((eng, me["idx"]))
        for r in reads:
            self.readers.setdefault(r, []).append(me)
        for wr in writes:
            self.lastw[wr] = me
            self.readers[wr] = []
        lst.append(me)
        return me

    def emit(self, final_waits=()):
        nc = self.nc
        for e in ENGS:
            for me in self.ins[e]:
                for (pe, pi) in me["deps"]:
                    self.ins[pe][pi]["sig"] = True
        for me in final_waits:
            me["sig"] = True
        nep = {}
        for e in ENGS:
            c = 0
            for me in self.ins[e]:
                if me["dma"]:
                    continue
                if me["sig"]:
                    c += 1
                    me["cnt"] = c
            nep[e] = c // EPOCH + 1
        dcount = {}
        for e in ENGS:
            for me in self.ins[e]:
                if me["dma"]:
                    k = (e, me["slot"])
                    dcount[k] = dcount.get(k, 0) + 16
                    me["cnt"] = dcount[k]
        with contextlib.ExitStack() as st:
            esem = {e: [st.enter_context(nc.semaphore(f"s_{e}_{i}")) for i in range(nep[e])] for e in ENGS}
            dsem = {}
            for k in dcount:
                dsem[k] = st.enter_context(nc.semaphore(f"d_{k[0]}_{k[1]}"))
            block = st.enter_context(nc.Block())

            def target(p):
                if p["dma"]:
                    return dsem[(p["eng"], p["slot"])], p["cnt"]
                c = p["cnt"]
                ep = (c - 1) // EPOCH
                return esem[p["eng"]][ep], c - ep * EPOCH

            def run(e, engobj):
                waited = {}
                for me in self.ins[e]:
                    best = {}
                    for (pe, pi) in me["deps"]:
                        p = self.ins[pe][pi]
                        key = ("d", pe, p["slot"]) if p["dma"] else ("e", pe)
                        if key not in best or best[key]["idx"] < p["idx"]:
                            best[key] = p
                    for key, p in best.items():
                        if waited.get(key, 0) >= p["cnt"]:
                            continue
                        waited[key] = p["cnt"]
                        sem, val = target(p)
                        engobj.wait_ge(sem, val)
                    r = me["fn"](engobj)
                    if me["dma"]:
                        r.then_inc(dsem[(e, me["slot"])], 16)
                    elif me["sig"]:
                        sem, _ = target(me)
                        r.then_inc(sem, 1)
                if e == "sp":
                    for p in final_waits:
                        sem, val = target(p)
                        engobj.wait_ge(sem, val)

            block.tensor(lambda eng: run("pe", eng))
            block.scalar(lambda eng: run("act", eng))
            block.vector(lambda eng: run("dve", eng))
            block.gpsimd(lambda eng: run("pool", eng))
            block.sync(lambda eng: run("sp", eng))


def host_consts(S):
    NBLK = S // 256
    p = np.arange(128)
    c = {}
    c["ident_f"] = np.eye(128, dtype=np.float32)
    c["tri_incl"] = (p[:, None] <= p[None, :]).astype(np.float32)
    c["tri_strict"] = (p[:, None] < p[None, :]).astype(np.float32)
    c["ones_f"] = np.ones((128, 128), np.float32)
    bf = ml_dtypes.bfloat16
    c["ut_b"] = (p[:, None] >= p[None, :]).astype(bf)
    c["ones_b"] = np.ones((128, 512), bf)
    c["zeros_b"] = np.zeros((128, 128), bf)
    nm = np.where(p[None, :] < p[:, None], -BIG, 0.0).astype(np.float32)
    c["negmask_b"] = np.tile(nm[:, None, :], (1, 8, 1)).reshape(128, 1024).astype(bf)
    own = np.arange(NBLK + 1)
    n = np.arange(NBLK)
    vm = np.where(n[None, :] < own[:, None], 0.0, -1e30).astype(np.float32)
    om = (n[None, :] == own[:, None]).astype(np.float32)
    c["vmask"] = np.tile(vm.reshape(1, -1), (128, 1))
    c["ownmask"] = np.tile(om.reshape(1, -1), (128, 1))
    s = np.arange(S)
    kaug = np.zeros((NBLK + 1, S), np.float32)
    kaug[s // 256, s] = 1.0
    kaug[NBLK, :] = 1.0
    c["kaug"] = kaug.astype(bf)
    slopes = 2.0 ** (-8.0 * np.arange(1, 5) / 4)
    c["alibi_row"] = (-slopes[:, None] * np.arange(512)[None, :]).astype(bf)
    rel = np.arange(32)
    ab = slopes[None, :, None] * (p[:, None, None] + 384 - 128 * rel[None, None, :])
    c["alibi_kb"] = ab.astype(np.float32)
    return c


import os as _os
KSTOP = int(_os.environ.get('KSTOP', '0'))


def build(S, NSEQ, DEPTH, dbg=False, passes=(1, 2, 3, 4, 5), ng_limit=None):
    nc = bass.Bass("TRN2", target_bir_lowering=False)
    NG, NT, NBLK = S // 512, S // 128, S // 256
    if ng_limit:
        NG = ng_limit
    KA = 64 + NBLK + 1
    hc = host_consts(S)

    def din(name, shape, dt=F32):
        return nc.dram_tensor(name, list(shape), dt, kind="ExternalInput").ap()

    def dscr(name, shape, dt):
        return nc.dram_tensor(name, list(shape), dt, kind=("ExternalOutput" if dbg else "Internal")).ap()

    x_in = din("x", [NSEQ, S, D])
    w_in = din("w_in", [DEPTH, D, INC])
    w_out = din("w_out", [DEPTH, D, D])
    w_gate = din("w_gate", [DEPTH, D, FH])
    w_up = din("w_up", [DEPTH, D, FH])
    w_down = din("w_down", [DEPTH, FH, D])
    vec1024 = din("vec1024", [DEPTH, 4, D])
    convw = din("convw", [DEPTH, 128, 8, 4])
    convb = din("convb", [DEPTH, 128, 8])
    hv = din("hv", [DEPTH, 3, 8])
    ssdn = din("ssdn", [DEPTH, 512])
    sbn = din("sbn", [DEPTH, 64, 4])
    mon = din("mon", [DEPTH, 64, 4])
    cd = {}
    for k, v in hc.items():
        cd[k] = din("c_" + k, v.shape, BF16 if v.dtype == ml_dtypes.bfloat16 else F32)
    out = nc.dram_tensor("out", [NSEQ, S, D], F32, kind="ExternalOutput").ap()

    d_ssd = [dscr(f"d_ssd{b}", [512, S], BF16) for b in range(NSEQ)]
    d_sbq = [dscr(f"d_sbq{b}", [256, S], BF16) for b in range(NSEQ)]
    d_sbk = [dscr(f"d_sbk{b}", [256, S], BF16) for b in range(NSEQ)]
    d_sbv = [dscr(f"d_sbv{b}", [S, 256], BF16) for b in range(NSEQ)]
    d_moq = [dscr(f"d_moq{b}", [256, S], BF16) for b in range(NSEQ)]
    d_mok = [dscr(f"d_mok{b}", [256, S], BF16) for b in range(NSEQ)]
    d_mov = [dscr(f"d_mov{b}", [S, 256], BF16) for b in range(NSEQ)]
    d_sel = [dscr(f"d_sel{b}", [4 * NBLK, S], BF16) for b in range(NSEQ)]
    d_msb = [dscr(f"d_msb{b}", [4, 64, S], BF16) for b in range(NSEQ)]
    d_mmo = [dscr(f"d_mmo{b}", [4, 64, S], BF16) for b in range(NSEQ)]
    d_x1 = [dscr(f"d_x1{b}", [S, D], F32) for b in range(NSEQ)]
    d_x2 = [dscr(f"d_x2{b}", [S, D], F32) for b in range(NSEQ)]

    dbg_t = dscr("dbg_t", [128, 256], F32)
    P = Prog(nc)
    sb_ = nc.alloc_sbuf_tensor
    _n = [0]
    ARENA_BYTES = 192 * 1024
    arena_state = {"on": False, "off": 0, "t": None}

    def SB(shape, dt, name=None):
        _n[0] += 1
        if not arena_state["on"]:
            return sb_(name or f"t{_n[0]}", list(shape), dt)
        free = int(np.prod(shape[1:]))
        nb = free * (2 if dt == BF16 else 4)
        nb = (nb + 63) // 64 * 64
        off = arena_state["off"]
        assert off + nb <= ARENA_BYTES, (name, off, nb)
        ap = arena_state["t"][0:shape[0], off // 4:(off + nb) // 4]
        if dt == BF16:
            ap = ap.bitcast(BF16)
        ap = ap[:, 0:free]
        if len(shape) == 3:
            ap = ap.rearrange("p (a b) -> p a b", b=shape[2])
        elif len(shape) == 4:
            ap = ap.rearrange("p (a b c) -> p a b c", b=shape[2], c=shape[3])
        arena_state["off"] = off + nb
        return ap

    def DMA(out_, in_, reads, writes, eng="sp"):
        return P.add(eng, lambda e: e.dma_start(out=out_, in_=in_), reads, writes, dma=True)

    def MM(out_, lhsT, rhs, start, stop, reads, writes):
        return P.add("pe", lambda e: e.matmul(out_, lhsT=lhsT, rhs=rhs, start=start, stop=stop), reads, writes)

    def TR(out_, in_, ident, reads, writes):
        return P.add("pe", lambda e: e.transpose(out=out_, in_=in_, identity=ident), reads, writes)

    def ACT(out_, in_, func, reads, writes, **kw):
        return P.add("act", lambda e: e.activation(out=out_, in_=in_, func=func, **kw), reads, writes)

    def TT(eng, out_, in0, in1, op, reads, writes):
        return P.add(eng, lambda e: e.tensor_tensor(out=out_, in0=in0, in1=in1, op=op), reads, writes)

    def TS(eng, out_, in0, s1, s2, op0, op1, reads, writes):
        if s2 is None:
            return P.add(eng, lambda e: e.tensor_scalar(out=out_, in0=in0, scalar1=s1, scalar2=None, op0=op0), reads, writes)
        return P.add(eng, lambda e: e.tensor_scalar(out=out_, in0=in0, scalar1=s1, scalar2=s2, op0=op0, op1=op1), reads, writes)

    def STT(eng, out_, in0, scalar, in1, op0, op1, reads, writes):
        return P.add(eng, lambda e: e.scalar_tensor_tensor(out=out_, in0=in0, scalar=scalar, in1=in1, op0=op0, op1=op1), reads, writes)

    def CP(eng, out_, in_, reads, writes):
        if eng == "act":
            return ACT(out_, in_, AF.Copy, reads, writes)
        return P.add(eng, lambda e: e.tensor_copy(out=out_, in_=in_), reads, writes)

    def MS(eng, ap, val, writes):
        return P.add(eng, lambda e: e.memset(ap, val), (), writes)

    psb = [nc.alloc_psum_tensor(f"ps{i}", [128, 512], F32) for i in range(8)]

    def psbf(i):
        return psb[i][:].bitcast(BF16)

    ident_f = SB([128, 128], F32, "ident_f")
    ident_b = SB([128, 128], BF16, "ident_b")
    tri_incl = SB([128, 128], F32, "tri_incl")
    tri_strict = SB([128, 128], F32, "tri_strict")
    ones_f = SB([128, 128], F32, "ones_f")
    ut_b = SB([128, 128], BF16, "ut_b")
    ones_b = SB([128, 512], BF16, "ones_b")
    zeros_b = SB([128, 128], BF16, "zeros_b")
    negmask_b = SB([128, 1024], BF16, "negmask_b")
    vmask = SB([128, (NBLK + 1) * NBLK], F32, "vmask")
    ownmask = SB([128, (NBLK + 1) * NBLK], F32, "ownmask")
    alibi_kb = SB([128, 4, 32], F32, "alibi_kb")
    for nm_, t in [("ident_f", ident_f), ("tri_incl", tri_incl), ("tri_strict", tri_strict), ("ones_f", ones_f),
                   ("ut_b", ut_b), ("ones_b", ones_b), ("zeros_b", zeros_b), ("negmask_b", negmask_b),
                   ("vmask", vmask), ("ownmask", ownmask), ("alibi_kb", alibi_kb)]:
        DMA(t[:], cd[nm_], (), ["C_" + nm_])
    CP("dve", ident_b[:], ident_f[:], ["C_ident_f"], ["C_ident_b"])
    CONST = ["C_ident_f", "C_ident_b", "C_tri_incl", "C_tri_strict", "C_ones_f", "C_ut_b", "C_ones_b", "C_zeros_b",
             "C_negmask_b", "C_vmask", "C_ownmask", "C_alibi_kb"]

    bar_sb = {e: SB([128, 8], F32, f"bar_{e}") for e in ("act", "dve", "pool")}
    bar_dram = nc.dram_tensor("bar_dram", [2, 64], F32).ap()

    def barrier():
        keys = []
        P.add("pe", lambda e: e.matmul(psb[7][:, 0:8], lhsT=zeros_b[:, 0:128], rhs=zeros_b[:, 0:8], start=True, stop=True),
              ["C_zeros_b"], ["ps7", "bar_pe"])
        for en in ("act", "dve", "pool"):
            CP(en, bar_sb[en][:], ones_f[:, 0:8], ["C_ones_f"], [f"bar_{en}"])
        allk = ["bar_pe", "bar_act", "bar_dve", "bar_pool"]
        dmas = {(d["eng"], d["idx"]) for d in P.dma_prev.values()}
        m = P.add("pe", lambda e: e.matmul(psb[7][:, 0:8], lhsT=zeros_b[:, 0:128], rhs=zeros_b[:, 0:8], start=True, stop=True),
                  ["C_zeros_b"] + allk, ["ps7", "bar2_pe"])
        m["deps"] |= dmas
        for en in ("act", "dve", "pool"):
            m = CP(en, bar_sb[en][:], ones_f[:, 0:8], ["C_ones_f"] + allk, [f"bar2_{en}"])
            m["deps"] |= dmas
        m = DMA(bar_dram[1:2, :], cd["ones_f"][0:1, 0:64], allk, ["bar2_sp"])
        m["deps"] |= {d for d in dmas if d != (m["eng"], m["idx"])}

    arena_state["t"] = sb_("arena", [128, ARENA_BYTES // 4], F32)
    arena_state["on"] = True
    gain = [SB([128, D], F32, f"gain{i}") for i in range(2)]
    xt = [SB([128, D], F32, f"xt{i}") for i in range(4)]
    junks = [SB([128, D], BF16, f"junk{i}") for i in range(4)]
    _jr = [0]

    def nextjunk():
        _jr[0] = (_jr[0] + 1) % 4
        return junks[_jr[0]], f"junk{_jr[0]}"
    ss4 = SB([128, 8], F32, "ss4")
    rs4 = SB([128, 8], F32, "rs4")
    hb = [SB([128, D], BF16, f"hb{i}") for i in range(2)]

    def load_gain(slot, l, which):
        DMA(gain[slot][:], vec1024[l, which].partition_broadcast(128), (), [f"gain{slot}"])

    base_off = arena_state["off"]

    win = SB([128, 8, INC], BF16, "win")
    hT = SB([128, 8, 512], BF16, "hT")
    zs = SB([128, 4, 512], F32, "zs")
    ubuf = [SB([128, 515], F32, f"u{i}") for i in range(2)]
    halo = SB([128, 8, 3], F32, "halo")
    cta = [SB([128, 512], F32, f"cta{i}") for i in range(2)]
    ctb = [SB([128, 512], F32, f"ctb{i}") for i in range(2)]
    xsT = SB([128, 4, 512], F32, "xsT")
    xs_tok = [SB([128, 512], F32, f"xstok{i}") for i in range(4)]
    BT = SB([128, 2, 512], BF16, "BT")
    CT = SB([128, 2, 512], BF16, "CT")
    Btok = SB([128, 4, 2, 128], BF16, "Btok")
    cw = SB([128, 8, 4], F32, "cw")
    cb = SB([128, 8], F32, "cb")
    hv_bc = SB([128, 3, 8], F32, "hv_bc")
    a_bc = SB([128, 8], F32, "a_bc")
    dsk_bc = SB([128, 8, 64], F32, "dsk_bc")
    ssdn_bc = SB([128, 512], F32, "ssdn_bc")
    dtx = SB([128, 4, 8], F32, "dtx")
    dt_all = SB([128, 4, 8], F32, "dt_all")
    adt = SB([128, 4, 8], F32, "adt")
    sbq_st = SB([128, 2, 512], BF16, "sbq_st")
    sbk_st = SB([128, 2, 512], BF16, "sbk_st")
    sbv_st = SB([128, 4, 256], BF16, "sbv_st")
    moq32 = SB([128, 2, 512], F32, "moq32")
    mok32 = SB([128, 2, 512], F32, "mok32")
    moq_st = SB([128, 2, 512], BF16, "moq_st")
    mok_st = SB([128, 2, 512], BF16, "mok_st")
    mov_st = SB([128, 4, 256], BF16, "mov_st")
    kmean = SB([128, 2, 2, NBLK], F32, "kmean")
    gm = SB([128, 4, NBLK], F32, "gm")
    max8 = SB([128, 4, 8], F32, "max8")
    thr = SB([128, 4], F32, "thr")
    sel = SB([128, 4, NBLK], F32, "sel")
    selneg = SB([128, 4 * NBLK], BF16, "selneg")
    selT_st = SB([64, 512], BF16, "selT_st")
    rhs_cum = SB([128, 8, 128], F32, "rhs_cum")
    negac4 = SB([128, 4, 8], F32, "negac4")
    ea4 = SB([128, 4, 8], F32, "ea4")
    cdb4 = SB([128, 4, 8], F32, "cdb4")
    dtd4 = SB([128, 4, 8], F32, "dtd4")
    negac = SB([128, 8], F32, "negac")
    ea = SB([128, 8], F32, "ea")
    cdb = SB([128, 8], F32, "cdb")
    Eb = SB([128, 8, 128], F32, "Eb")
    MT = SB([128, 8, 128], BF16, "MT")
    dtd = SB([128, 8], F32, "dtd")
    xdt = SB([128, 8, 64], BF16, "xdt")
    xdt2 = SB([128, 8, 64], BF16, "xdt2")
    S32 = SB([128, 512], F32, "S32")
    Sb = SB([128, 512], BF16, "Sb")
    y1 = SB([128, 512], F32, "y1")
    y2 = SB([128, 512], F32, "y2")
    y3 = SB([128, 512], F32, "y3")
    ssq = SB([128, 2], F32, "ssq")
    rsq = SB([128, 2], F32, "rsq")
    yn = SB([128, 512], BF16, "yn")
    mixT_ssd = SB([128, 4, 512], BF16, "mixT_ssd")

    rot01 = [0]

    def nextbank():
        rot01[0] ^= 1
        return rot01[0]

    def load_layer_p1(l):
        for (c0, c1) in [(0, 512), (512, 1536), (1536, 2312), (2312, INC)]:
            DMA(win[:, :, c0:c1], w_in[l, :, c0:c1].rearrange("(c p) n -> p c n", p=128), (), ["win"], eng="pool")
        DMA(cw[:], convw[l], (), ["cw"])
        DMA(cb[:], convb[l], (), ["cb"])
        for i in range(3):
            DMA(hv_bc[:, i, :], hv[l, i].partition_broadcast(128), (), ["hv_bc"])
        DMA(ssdn_bc[:], ssdn[l].partition_broadcast(128), (), ["ssdn_bc"])
        ACT(a_bc[:], hv_bc[:, 1, :], AF.Exp, ["hv_bc"], ["a_bc"])
        TS("dve", a_bc[:], a_bc[:], -1.0, None, ALU.mult, None, ["a_bc"], ["a_bc"])
        CP("dve", dsk_bc[:], hv_bc[:, 2, :].unsqueeze(2).to_broadcast([128, 8, 64]), ["hv_bc"], ["dsk_bc"])

    def pass1(l, b, xsrc):
        load_layer_p1(l)
        load_gain(0, l, 0)
        MS("pool", S32[:], 0.0, ["S32"])
        MS("pool", Sb[:], 0.0, ["Sb"])
        MS("pool", halo[:], 0.0, ["halo"])
        MS("pool", kmean[:], 0.0, ["kmean"])
        def load_x(g_):
            for tt_ in range(4):
                DMA(xt[tt_][:], xsrc[g_ * 512 + tt_ * 128:g_ * 512 + (tt_ + 1) * 128, :], (), [f"xt{tt_}"])

        load_x(0)
        for g in range(NG):
            t0 = g * 512
            for tt in range(4):
                jb, jk = nextjunk()
                ACT(jb[:], xt[tt][:], AF.Square, [f"xt{tt}"], [jk, f"ss4_{tt}"], scale=1.0 / 32, accum_out=ss4[:, tt:tt + 1])
            ACT(rs4[:, 0:4], ss4[:, 0:4], AF.Ln, [f"ss4_{i}" for i in range(4)], ["rs4"], bias=EPS)
            ACT(rs4[:, 0:4], rs4[:, 0:4], AF.Exp, ["rs4"], ["rs4"], scale=-0.5)
            for tt in range(4):
                hbk = f"hb{tt % 2}"
                STT("dve", hb[tt % 2][:], xt[tt][:], rs4[:, tt:tt + 1], gain[0][:], ALU.mult, ALU.mult,
                    [f"xt{tt}", "rs4", "gain0"], [hbk])
                bk = nextbank()
                pT = psbf(bk)
                for c in range(8):
                    TR(pT[:, c * 128:(c + 1) * 128], hb[tt % 2][:, c * 128:(c + 1) * 128], ident_b[:], [hbk, "C_ident_b"], [f"ps{bk}"])
                CP("act" if tt % 2 else "dve", hT[:, :, tt * 128:(tt + 1) * 128], pT.rearrange("p (c k) -> p c k", k=128),
                   [f"ps{bk}"], ["hT"])

            if g + 1 < NG:
                load_x(g + 1)

            def proj_fm(col0, ncols=128):
                bk = nextbank()
                for c in range(8):
                    MM(psb[bk][0:ncols, :], win[:, c, col0:col0 + ncols], hT[:, c, :], c == 0, c == 7, ["win", "hT"], [f"ps{bk}"])
                return bk

            def proj_tm(tt, col0, ncols, bk, off):
                for c in range(8):
                    MM(psb[bk][:, off:off + ncols], hT[:, c, tt * 128:(tt + 1) * 128], win[:, c, col0:col0 + ncols], c == 0, c == 7,
                       ["win", "hT"], [f"ps{bk}"])

            if KSTOP == 1:
                return
            for tt in range(4):
                bk = nextbank()
                proj_tm(tt, 0, 512, bk, 0)
                ACT(zs[:, tt, :], psb[bk][:], AF.Silu, [f"ps{bk}"], ["zs"])
            if KSTOP == 2:
                return
            for tt in range(4):
                for c in range(8):
                    MM(psb[4][:, 400 + tt * 8:408 + tt * 8], hT[:, c, tt * 128:(tt + 1) * 128], win[:, c, 1536:1544], c == 0, c == 7,
                       ["win", "hT"], ["ps4"])
            TT("dve", dtx[:], psb[4][:, 400:432].rearrange("p (t h) -> p t h", h=8),
               hv_bc[:, 0, :].unsqueeze(1).to_broadcast([128, 4, 8]), ALU.add, ["ps4", "hv_bc"], ["dtx"])
            ACT(dtx[:], dtx[:], AF.Exp, ["dtx"], ["dtx"])
            ACT(dt_all[:], dtx[:], AF.Ln, ["dtx"], ["dt_all"], bias=1.0)
            TT("dve", adt[:], dt_all[:], a_bc[:].unsqueeze(1).to_broadcast([128, 4, 8]), ALU.mult, ["dt_all", "a_bc"], ["adt"])
            if KSTOP == 3:
                return
            for j in range(8):
                bk = proj_fm(512 + 128 * j)
                u = ubuf[j % 2]
                uk = f"u{j % 2}"
                CP("dve", u[:, 0:3], halo[:, j, :], ["halo"], [uk])
                CP("dve", u[:, 3:515], psb[bk][:], [f"ps{bk}"], [uk])
                CP("pool", halo[:, j, :], u[:, 512:515], [uk], ["halo"])
                ta = cta[j % 2]
                tk = f"cta{j % 2}"
                ACT(ta[:], u[:, 0:512], AF.Copy, [uk, "cw"], [tk], scale=cw[:, j, 0:1])
                for k_ in range(1, 4):
                    STT("dve", ta[:], u[:, k_:k_ + 512], cw[:, j, k_:k_ + 1], ta[:], ALU.mult, ALU.add, [uk, "cw", tk], [tk])
                if j < 4:
                    dst, dk = xsT[:, j, :], "xsT"
                elif j < 6:
                    dst, dk = BT[:, j - 4, :], "BT"
                else:
                    dst, dk = CT[:, j - 6, :], "CT"
                ACT(dst, ta[:], AF.Silu, [tk, "cb"], [dk], bias=cb[:, j:j + 1])
            if KSTOP == 4:
                return
            for pr in range(2):
                bk = proj_fm(1544 + 128 * pr)
                ACT(sbq_st[:, pr, :], psb[bk][:], AF.Copy, [f"ps{bk}"], ["sbq_st"], scale=0.125)
                bk = proj_fm(1800 + 128 * pr)
                CP("dve", sbk_st[:, pr, :], psb[bk][:], [f"ps{bk}"], ["sbk_st"])
            for t2 in range(2):
                bk = nextbank()
                for q in range(2):
                    proj_tm(2 * t2 + q, 2056, 256, bk, q * 256)
                CP("act", sbv_st[:, 2 * t2:2 * t2 + 2, :], psb[bk][:].rearrange("p (t c) -> p t c", c=256), [f"ps{bk}"], ["sbv_st"])
            DMA(d_sbq[b].rearrange("(r p) s -> p r s", p=128)[:, :, t0:t0 + 512], sbq_st[:], ["sbq_st"], [f"d_sbq{b}_{g}"])
            DMA(d_sbk[b].rearrange("(r p) s -> p r s", p=128)[:, :, t0:t0 + 512], sbk_st[:], ["sbk_st"], [f"d_sbk{b}_{g}"])
            DMA(d_sbv[b][t0:t0 + 512, :].rearrange("(t p) c -> p t c", p=128), sbv_st[:], ["sbv_st"], [f"d_sbv{b}_{g}"])
            if KSTOP == 5:
                return
            for pr in range(2):
                bk = proj_fm(2312 + 128 * pr)
                CP("dve", moq32[:, pr, :], psb[bk][:], [f"ps{bk}"], ["moq32"])
                TS("pool", moq_st[:, pr, :], moq32[:, pr, :], 0.125, None, ALU.mult, None, ["moq32"], ["moq_st"])
                bk = proj_fm(2568 + 128 * pr)
                CP("act", mok32[:, pr, :], psb[bk][:], [f"ps{bk}"], ["mok32"])
                CP("pool", mok_st[:, pr, :], mok32[:, pr, :], ["mok32"], ["mok_st"])
                for e_ in range(2):
                    rows = slice(e_ * 64, e_ * 64 + 64)
                    P.add("dve", lambda e, pr=pr, g=g, e_=e_, rows=rows: e.tensor_reduce(
                        out=kmean[rows, pr, e_, 2 * g:2 * g + 2], in_=mok32[rows, pr, :].rearrange("p (b k) -> p b k", k=256),
                        axis=AX.X, op=ALU.add), ["mok32"], ["kmean"])
            for t2 in range(2):
                bk = nextbank()
                for q in range(2):
                    proj_tm(2 * t2 + q, 2824, 256, bk, q * 256)
                CP("dve", mov_st[:, 2 * t2:2 * t2 + 2, :], psb[bk][:].rearrange("p (t c) -> p t c", c=256), [f"ps{bk}"], ["mov_st"])
            DMA(d_moq[b].rearrange("(r p) s -> p r s", p=128)[:, :, t0:t0 + 512], moq_st[:], ["moq_st"], [f"d_moq{b}_{g}"])
            DMA(d_mok[b].rearrange("(r p) s -> p r s", p=128)[:, :, t0:t0 + 512], mok_st[:], ["mok_st"], [f"d_mok{b}_{g}"])
            DMA(d_mov[b][t0:t0 + 512, :].rearrange("(t p) c -> p t c", p=128), mov_st[:], ["mov_st"], [f"d_mov{b}_{g}"])
            if KSTOP == 6:
                return
            gps = psb[5][:, 0:16 * NBLK].rearrange("p (t h n) -> p t h n", t=4, h=4)
            for tt in range(4):
                for pr in range(2):
                    MM(gps[:, tt, 2 * pr:2 * pr + 2, :], moq32[:, pr, tt * 128:(tt + 1) * 128],
                       kmean[:, pr, :, :], True, True, ["moq32", "kmean"], ["ps5"])
            if KSTOP == 61:
                return
            for tt in range(4):
                own = 2 * g + tt // 2
                TT("dve", gm[:], gps[:, tt], vmask[:, own * NBLK:(own + 1) * NBLK].unsqueeze(1).to_broadcast([128, 4, NBLK]),
                   ALU.add, ["ps5", "C_vmask"], ["gm"])
                for h in range(4):
                    P.add("dve", lambda e, h=h: e.max(out=max8[:, h, :], in_=gm[:, h, :]), ["gm"], ["max8"])
                TS("dve", thr[:], max8[:, :, 2], -1e20, None, ALU.max, None, ["max8"], ["thr"])
                TT("dve", sel[:], gm[:], thr[:].unsqueeze(2).to_broadcast([128, 4, NBLK]), ALU.is_ge, ["gm", "thr"], ["sel"])
                TT("dve", sel[:], sel[:], ownmask[:, own * NBLK:(own + 1) * NBLK].unsqueeze(1).to_broadcast([128, 4, NBLK]),
                   ALU.add, ["sel", "C_ownmask"], ["sel"])
                TS("dve", selneg[:], sel[:].rearrange("p h n -> p (h n)"), -1.0, BIG, ALU.add, ALU.mult, ["sel"], ["selneg"])
                if dbg and g == NG - 1 and tt == 3:
                    DMA(dbg_t[:, 0:4 * NBLK], gm[:].rearrange("p h n -> p (h n)"), ["gm"], ["dbg_t"])
                    DMA(dbg_t[:, 64:96], max8[:].rearrange("p h n -> p (h n)"), ["max8"], ["dbg_t"])
                    DMA(dbg_t[:, 96:100], thr[:], ["thr"], ["dbg_t"])
                    DMA(dbg_t[:, 128:128 + 4 * NBLK], sel[:].rearrange("p h n -> p (h n)"), ["sel"], ["dbg_t"])
                if KSTOP == 62:
                    continue
                bk = nextbank()
                TR(psbf(bk)[0:4 * NBLK, 0:128], selneg[:], ident_b[:], ["selneg", "C_ident_b"], [f"ps{bk}"])
                CP("act", selT_st[0:4 * NBLK, tt * 128:(tt + 1) * 128], psbf(bk)[0:4 * NBLK, 0:128], [f"ps{bk}"], ["selT_st"])
            if KSTOP == 62:
                return
            DMA(d_sel[b][:, t0:t0 + 512], selT_st[0:4 * NBLK, :], ["selT_st"], [f"d_sel{b}_{g}"])
            if KSTOP == 7:
                return
            for tt in range(4):
                bk = nextbank()
                for j in range(4):
                    TR(psb[bk][:, j * 128:(j + 1) * 128], xsT[:, j, tt * 128:(tt + 1) * 128], ident_f[:], ["xsT", "C_ident_f"], [f"ps{bk}"])
                CP("act" if tt % 2 else "dve", xs_tok[tt][:], psb[bk][:], [f"ps{bk}"], [f"xstok{tt}"])
                bk = nextbank()
                for gi in range(2):
                    TR(psbf(bk)[:, gi * 128:(gi + 1) * 128], BT[:, gi, tt * 128:(tt + 1) * 128], ident_b[:], ["BT", "C_ident_b"], [f"ps{bk}"])
                CP("dve", Btok[:, tt, :, :], psbf(bk)[:, 0:256].rearrange("p (g n) -> p g n", n=128), [f"ps{bk}"], ["Btok"])
            if KSTOP == 8:
                return
            def ssd_chunk(tt):
                tsl = slice(tt * 128, (tt + 1) * 128)
                xs3 = xs_tok[tt][:].rearrange("p (h d) -> p h d", d=64)
                xk = f"xstok{tt}"
                negac, ea, cdb, dtd = negac4[:, tt, :], ea4[:, tt, :], cdb4[:, tt, :], dtd4[:, tt, :]
                nk, ek, ck, dk_ = f"negac{tt}", f"ea{tt}", f"cdb{tt}", f"dtd{tt}"
                TT("dve", rhs_cum[:], tri_incl[:].unsqueeze(1).to_broadcast([128, 8, 128]),
                   adt[:, tt, :].unsqueeze(2).to_broadcast([128, 8, 128]), ALU.mult, ["C_tri_incl", "adt"], ["rhs_cum"])
                rc = rhs_cum[:].rearrange("p h l -> p (h l)")
                for half in range(2):
                    pb = psb[2 + half]
                    MM(pb[:], ones_f[:], rc[:, half * 512:(half + 1) * 512], True, False, ["C_ones_f", "rhs_cum"], [f"ps{2 + half}"])
                    MM(pb[:], ident_b[:], negmask_b[:, half * 512:(half + 1) * 512], False, True, ["C_ident_b", "C_negmask_b"], [f"ps{2 + half}"])
                MM(psb[4][:, 0:8], tri_incl[:], adt[:, tt, :], True, True, ["C_tri_incl", "adt"], ["ps4"])
                yield
                ACT(negac, psb[4][:, 0:8], AF.Copy, ["ps4"], [nk], scale=-1.0)
                ACT(ea, psb[4][:, 0:8], AF.Exp, ["ps4"], [ek])
                for half in range(2):
                    ACT(cdb[:, half * 4:(half + 1) * 4], psb[2 + half][:].rearrange("p (h l) -> p h l", l=128)[:, :, 127], AF.Exp,
                        [f"ps{2 + half}"], [ck])
                for h in range(8):
                    ACT(Eb[:, h, :], psb[2 + h // 4][:, (h % 4) * 128:(h % 4 + 1) * 128], AF.Exp, [f"ps{2 + h // 4}", nk], ["Eb"],
                        bias=negac[:, h:h + 1])
                for gi in range(2):
                    MM(psb[4][:, 128 + gi * 128:256 + gi * 128], BT[:, gi, tsl], CT[:, gi, tsl], True, True, ["BT", "CT"], ["ps4"])
                yield
                for gi in range(2):
                    TT("dve", MT[:, gi * 4:(gi + 1) * 4, :], Eb[:, gi * 4:(gi + 1) * 4, :],
                       psb[4][:, 128 + gi * 128:256 + gi * 128].unsqueeze(1).to_broadcast([128, 4, 128]), ALU.mult,
                       ["Eb", "ps4"], ["MT"])
                TT("dve", dtd, dt_all[:, tt, :], Eb[:, :, 127], ALU.mult, ["dt_all", "Eb"], [dk_])
                TT("dve", xdt[:], xs3, dt_all[:, tt, :].unsqueeze(2).to_broadcast([128, 8, 64]), ALU.mult, [xk, "dt_all"], ["xdt"])
                TT("pool", xdt2[:], xs3, dtd.unsqueeze(2).to_broadcast([128, 8, 64]), ALU.mult, [xk, dk_], ["xdt2"])
                yield
                x2f = xdt2[:].rearrange("p h d -> p (h d)")
                x1f = xdt[:].rearrange("p h d -> p (h d)")
                for gi in range(2):
                    MM(psb[5][:, gi * 256:(gi + 1) * 256], Btok[:, tt, gi, :], x2f[:, gi * 256:(gi + 1) * 256], True, True,
                       ["Btok", "xdt2"], ["ps5"])
                for gi in range(2):
                    MM(psb[6][:, gi * 256:(gi + 1) * 256], CT[:, gi, tsl], Sb[:, gi * 256:(gi + 1) * 256], True, True,
                       ["CT", "Sb"], ["ps6"])
                for h in range(8):
                    MM(psb[7][:, h * 64:(h + 1) * 64], MT[:, h, :], x1f[:, h * 64:(h + 1) * 64], True, True, ["MT", "xdt"], ["ps7"])
                yield
                TT("dve", S32[:].rearrange("p (h d) -> p h d", d=64), S32[:].rearrange("p (h d) -> p h d", d=64),
                   cdb.unsqueeze(2).to_broadcast([128, 8, 64]), ALU.mult, ["S32", ck], ["S32"])
                TT("dve", S32[:], S32[:], psb[5][:], ALU.add, ["S32", "ps5"], ["S32"])
                CP("act", Sb[:], S32[:], ["S32"], ["Sb"])
                TT("dve", y1[:].rearrange("p (h d) -> p h d", d=64), psb[6][:].rearrange("p (h d) -> p h d", d=64),
                   ea.unsqueeze(2).to_broadcast([128, 8, 64]), ALU.mult, ["ps6", ek], ["y1"])
                TT("dve", y1[:], y1[:], psb[7][:], ALU.add, ["y1", "ps7"], ["y1"])
                TT("pool", y2[:].rearrange("p (h d) -> p h d", d=64), xs3, dsk_bc[:], ALU.mult, [xk, "dsk_bc"], ["y2"])
                TT("pool", y2[:], y2[:], y1[:], ALU.add, ["y2", "y1"], ["y2"])
                TT("dve", y3[:], y2[:], zs[:, tt, :], ALU.mult, ["y2", "zs"], ["y3"])
                for gi in range(2):
                    jb, jk = nextjunk()
                    ACT(jb[:, 0:256], y3[:, gi * 256:(gi + 1) * 256], AF.Square, ["y3"], [jk, f"ssq{gi}"], scale=1.0 / 16,
                        accum_out=ssq[:, gi:gi + 1])
                ACT(rsq[:], ssq[:], AF.Ln, ["ssq0", "ssq1"], ["rsq"], bias=EPS)
                ACT(rsq[:], rsq[:], AF.Exp, ["rsq"], ["rsq"], scale=-0.5)
                for gi in range(2):
                    STT("dve", yn[:, gi * 256:(gi + 1) * 256], y3[:, gi * 256:(gi + 1) * 256], rsq[:, gi:gi + 1],
                        ssdn_bc[:, gi * 256:(gi + 1) * 256], ALU.mult, ALU.mult, ["y3", "rsq", "ssdn_bc"], ["yn"])
                yield
                bk = nextbank()
                for j in range(4):
                    TR(psbf(bk)[:, j * 128:(j + 1) * 128], yn[:, j * 128:(j + 1) * 128], ident_b[:], ["yn", "C_ident_b"], [f"ps{bk}"])
                CP("act", mixT_ssd[:, :, tsl], psbf(bk)[:, 0:512].rearrange("p (c k) -> p c k", k=128), [f"ps{bk}"], ["mixT_ssd"])

            for tt in range(4):
                for _ in ssd_chunk(tt):
                    pass
            DMA(d_ssd[b].rearrange("(c p) s -> p c s", p=128)[:, :, t0:t0 + 512], mixT_ssd[:], ["mixT_ssd"], [f"d_ssd{b}_{g}"])

    arena_state["off"] = base_off
    p2_kT = SB([128, 2, S], BF16, "p2_kT")
    p2_v = SB([128, NT, 256], BF16, "p2_v")
    p2_q = [SB([128, 2, 512], BF16, f"p2_q{i}") for i in range(3)]
    p2_nq = [SB([128, 2, 512], BF16, f"p2_nq{i}") for i in range(3)]
    p2_e = [SB([128, 512], F32, f"p2_e{i}") for i in range(2)]
    p2_sp = [SB([128, 512], BF16, f"p2_sp{i}") for i in range(2)]
    p2_S = SB([128, 512], BF16, "p2_S")
    p2_w = [SB([128, 512], BF16, f"p2_w{i}") for i in range(2)]
    p23_y = SB([64, 4, 512], F32, "p23_y")
    p23_sq = SB([64, 4, 512], BF16, "p23_sq")
    p23_rs = SB([64, 512], F32, "p23_rs")
    p23_mix = SB([64, 4, 512], BF16, "p23_mix")
    p23_nw = SB([64, 4], F32, "p23_nw")

    def head_norm_store(l, b, g, dst, nb=6):
        t0 = g * 512
        for h in range(4):
            ACT(p23_sq[:, h, :], p23_y[:, h, :], AF.Square, ["p23_y"], ["p23_sq"])
        for h in range(4):
            MM(psb[nb][0:64, :], ones_b[0:64, 0:64], p23_sq[:, h, :], h == 0, h == 3, ["C_ones_b", "p23_sq"], [f"ps{nb}"])
        ACT(p23_rs[:], psb[nb][0:64, :], AF.Ln, [f"ps{nb}"], ["p23_rs"], scale=1.0 / 256, bias=EPS)
        ACT(p23_rs[:], p23_rs[:], AF.Exp, ["p23_rs"], ["p23_rs"], scale=-0.5)
        for h in range(4):
            STT("dve", p23_mix[:, h, :], p23_y[:, h, :], p23_nw[:, h:h + 1], p23_rs[:], ALU.mult, ALU.mult,
                ["p23_y", "p23_nw", "p23_rs"], ["p23_mix"])
        DMA(dst.rearrange("h d s -> d h s")[:, :, t0:t0 + 512], p23_mix[:], ["p23_mix"], [f"{dst.tensor.name}_{g}"])

    def run_pipeline(gens):
        active = []
        gi = iter(gens)
        more = True
        while True:
            if more:
                try:
                    active.append(next(gi))
                except StopIteration:
                    more = False
            if not active:
                break
            for gen in list(active):
                try:
                    next(gen)
                except StopIteration:
                    active.remove(gen)

    NB2 = 4
    p2_e3 = [SB([128, 512], F32, f"p2_e3{i}") for i in range(NB2)]
    p2_sp3 = [SB([128, 512], BF16, f"p2_sp3{i}") for i in range(NB2)]
    p2_w3 = [SB([128, 512], BF16, f"p2_w3{i}") for i in range(NB2)]
    p2_S2 = [SB([128, 512], BF16, f"p2_S2{i}") for i in range(4)]
    p2_t4 = [SB([128, 512], F32, f"p2_t4{i}") for i in range(NB2)]

    p2set = (p23_y, p23_sq, p23_rs, p23_mix, p23_nw)

    def pass2(l, b):
        nonlocal p23_y, p23_sq, p23_rs, p23_mix, p23_nw
        p23_y, p23_sq, p23_rs, p23_mix, p23_nw = p2set
        DMA(p23_nw[:], sbn[l], (), ["p23_nw"])

        def loads(g):
            t0 = g * 512
            DMA(p2_kT[:, :, t0:t0 + 512], d_sbk[b].rearrange("(r p) s -> p r s", p=128)[:, :, t0:t0 + 512],
                [f"d_sbk{b}_{g}"], [f"p2_kT_{g}"])
            DMA(p2_v[:, 4 * g:4 * g + 4, :], d_sbv[b][t0:t0 + 512, :].rearrange("(t p) c -> p t c", p=128),
                [f"d_sbv{b}_{g}"], [f"p2_v_{g}"])
            q, nq = p2_q[g % 3], p2_nq[g % 3]
            DMA(q[:], d_sbq[b].rearrange("(r p) s -> p r s", p=128)[:, :, t0:t0 + 512], [f"d_sbq{b}_{g}"], [f"p2_q{g % 3}"])

        def it_gen(g, h, kb, n):
            q, nq = p2_q[g % 3], p2_nq[g % 3]
            qk, nqk = f"p2_q{g % 3}", f"p2_nq{g % 3}"
            pr, e_ = h // 2, h % 2
            rows = slice(e_ * 64, e_ * 64 + 64)
            yb = 4 + (h % 2)
            last = 4 * g + 3
            gk = kb // 4
            qoff = max(0, kb - 4 * g) * 128
            W = 512 - qoff
            diag = kb >= 4 * g
            i3 = n % NB2
            b1, b2 = n % 2, 2 + n % 2
            ksl = slice(kb * 128, (kb + 1) * 128)
            e3, sp3, w3 = p2_e3[i3], p2_sp3[i3], p2_w3[i3]
            ek, spk, wk = f"p2_e3{i3}", f"p2_sp3{i3}", f"p2_w3{i3}"
            par = (last - kb) % 2
            S_in, S_out = p2_S2[2 * e_ + par], p2_S2[2 * e_ + 1 - par]
            Sik, Sok = f"p2_S2{2 * e_ + par}", f"p2_S2{2 * e_ + 1 - par}"
            if kb == last:
                if h == 0 and g + 1 < NG:
                    loads(g + 1)
                MM(psb[yb][0:64, :], zeros_b[:, 0:64], ones_b[:, 0:512], True, False, ["C_zeros_b", "C_ones_b"], [f"ps{yb}"])
            MM(psb[b1][:, 0:W], p2_kT[rows, pr, ksl], q[rows, pr, qoff:512], True, True, [f"p2_kT_{gk}", qk], [f"ps{b1}"])
            yield
            ACT(e3[:, 0:W], psb[b1][:, 0:W], AF.Exp, [f"ps{b1}"], [ek])
            yield
            ACT(sp3[:, 0:W], e3[:, 0:W], AF.Ln, [ek], [spk], bias=1.0)
            if diag:
                TT("pool", sp3[:, 0:128], sp3[:, 0:128], tri_strict[:], ALU.mult, [spk, "C_tri_strict"], [spk])
            yield
            MM(psb[b2][:, 0:W], ut_b[:], sp3[:, 0:W], True, kb == last, ["C_ut_b", spk], [f"ps{b2}"])
            if kb < last:
                MM(psb[b2][:, 0:W], ones_b[:, 0:128], S_in[:, qoff:512], False, True, ["C_ones_b", Sik], [f"ps{b2}"])
            if kb > 0:
                if kb == last:
                    if qoff > 0:
                        MS("pool", S_out[:, 0:qoff], 0.0, [Sok])
                    CP("dve", S_out[:, qoff:512], sp3[:, 0:W], [spk], [Sok])
                else:
                    if qoff > 0:
                        CP("pool", S_out[:, 0:qoff], S_in[:, 0:qoff], [Sik], [Sok])
                    TT("dve", S_out[:, qoff:512], S_in[:, qoff:512], sp3[:, 0:W], ALU.add, [Sik, spk], [Sok])
            yield
            t4, t4k = p2_t4[i3], f"p2_t4{i3}"
            ACT(t4[:, 0:W], psb[b2][:, 0:W], AF.Exp, [f"ps{b2}"], [t4k], scale=-1.0)
            TT("dve", w3[:, 0:W], t4[:, 0:W], e3[:, 0:W], ALU.mult, [t4k, ek], [wk])
            if diag:
                TT("pool", w3[:, 0:128], w3[:, 0:128], tri_strict[:], ALU.mult, [wk, "C_tri_strict"], [wk])
            yield
            MM(psb[yb][0:64, qoff:512], p2_v[:, kb, h * 64:(h + 1) * 64], w3[:, 0:W], False, kb == 0,
               [f"p2_v_{gk}", wk], [f"ps{yb}"])
            if kb == 0:
                CP("dve", p23_y[:, h, :], psb[yb][0:64, :], [f"ps{yb}"], ["p23_y"])
                if h == 3:
                    head_norm_store(l, b, g, d_msb[b])

        def all_iters():
            n = 0
            for g in range(NG):
                for h in range(4):
                    for kb in range(4 * g + 3, -1, -1):
                        yield it_gen(g, h, kb, n)
                        n += 1

        loads(0)
        run_pipeline(all_iters())

    arena_state["off"] = base_off
    p3_kT = [SB([96, S], BF16, f"p3_kT{h}") for h in range(4)]
    p3_va = SB([128, NT, 4, 128], BF16, "p3_va")
    p3_q = [[SB([96, 512], BF16, f"p3_q{i}_{h}") for h in range(4)] for i in range(3)]
    p3_p = [SB([128, 512], BF16, f"p3_p{i}") for i in range(3)]
    arena_state["off"] = max(arena_state["off"], 0)
    p3_y = SB([64, 4, 512], F32, "p3_y")
    p3_sq = SB([64, 4, 512], BF16, "p3_sq")
    p3_rs = SB([64, 512], F32, "p3_rs")
    p3_mix = SB([64, 4, 512], BF16, "p3_mix")
    p3_nw = SB([64, 4], F32, "p3_nw")

    def pass3(l, b):
        nonlocal p23_y, p23_sq, p23_rs, p23_mix, p23_nw
        p23_y, p23_sq, p23_rs, p23_mix, p23_nw = p3_y, p3_sq, p3_rs, p3_mix, p3_nw
        DMA(p23_nw[:], mon[l], (), ["p23_nw"])
        for h in range(4):
            MS("pool", p3_kT[h][:], 0.0, [f"p3_kT{h}_c"])
            DMA(p3_kT[h][64:64 + NBLK + 1, :], cd["kaug"], [], [f"p3_kT{h}_c"])
            for i in range(3):
                MS("pool", p3_q[i][h][:], 0.0, [f"p3_q{i}_{h}"])
                DMA(p3_q[i][h][64 + NBLK:64 + NBLK + 1, :], cd["alibi_row"][h:h + 1, :], [], [f"p3_q{i}_{h}"])

        for h in range(4):
            MS("pool", p3_va[:, :, h, 64:128], 1.0, ["p3_va_c"])

        def loads(g):
            t0 = g * 512
            for h in range(4):
                DMA(p3_kT[h][0:64, t0:t0 + 512], d_mok[b][h * 64:(h + 1) * 64, t0:t0 + 512], [f"d_mok{b}_{g}", f"p3_kT{h}_c"], [f"p3_kT{h}_{g}"])
            for h in range(4):
                DMA(p3_va[:, 4 * g:4 * g + 4, h, 0:64], d_mov[b][t0:t0 + 512, h * 64:(h + 1) * 64].rearrange("(t p) d -> p t d", p=128),
                    [f"d_mov{b}_{g}", "p3_va_c"], [f"p3_v_{g}"])
            i_ = g % 3
            for h in range(4):
                qk = f"p3_q{i_}_{h}"
                DMA(p3_q[i_][h][0:64, :], d_moq[b][h * 64:(h + 1) * 64, t0:t0 + 512], [f"d_moq{b}_{g}"], [qk])
                DMA(p3_q[i_][h][64:64 + NBLK, :], d_sel[b][h * NBLK:(h + 1) * NBLK, t0:t0 + 512], [f"d_sel{b}_{g}"], [qk])

        def it_gen(g, h, kb, n):
            i_ = g % 3
            qk = f"p3_q{i_}_{h}"
            qa = p3_q[i_][h]
            yb = 4 + (h % 2)
            db = 6 + (h % 2)
            gk = kb // 4
            qoff = max(0, kb - 4 * g) * 128
            W = 512 - qoff
            diag = kb >= 4 * g
            rel = 4 * g + 3 - kb
            i3 = n % 3
            b1 = n % 3
            lastk = kb == 4 * g + 3
            pp, pk = p3_p[i3], f"p3_p{i3}"
            if kb == 0:
                if h == 0 and g + 1 < NG:
                    loads(g + 1)
                MM(psb[yb][:, :], zeros_b[:, 0:128], ones_b[:, 0:512], True, False, ["C_zeros_b", "C_ones_b"], [f"ps{yb}"])
            MM(psb[b1][:, 0:W], p3_kT[h][:, kb * 128:(kb + 1) * 128], qa[:, qoff:512], True, True,
               [f"p3_kT{h}_{gk}", f"p3_kT{h}_c", qk], [f"ps{b1}"])
            yield
            ACT(pp[:, 0:W], psb[b1][:, 0:W], AF.Exp, [f"ps{b1}", "C_alibi_kb"], [pk], bias=alibi_kb[:, h, rel:rel + 1])
            if diag:
                TT("dve", pp[:, 0:128], pp[:, 0:128], tri_incl[:], ALU.mult, [pk, "C_tri_incl"], [pk])
            yield
            MM(psb[yb][:, qoff:512], p3_va[:, kb, h, :], pp[:, 0:W], False, lastk, [f"p3_v_{gk}", "p3_va_c", pk], [f"ps{yb}"])
            if lastk:
                CP("act", p23_y[:, h, :], psb[yb][0:64, :], [f"ps{yb}"], ["p23_y"])
                CP("act", p23_rs[:], psb[yb][64:128, :], [f"ps{yb}"], ["p23_rs"])
                P.add("dve", lambda e: e.reciprocal(out=p23_rs[:], in_=p23_rs[:]), ["p23_rs"], ["p23_rs"])
                TT("dve", p23_y[:, h, :], p23_y[:, h, :], p23_rs[:], ALU.mult, ["p23_y", "p23_rs"], ["p23_y"])
                if h == 3:
                    head_norm_store(l, b, g, d_mmo[b], nb=3)

        def all_iters():
            n = 0
            for g in range(NG):
                for h in range(4):
                    for kb in range(0, 4 * g + 4):
                        yield it_gen(g, h, kb, n)
                        n += 1

        loads(0)
        run_pipeline(all_iters())

    arena_state["off"] = base_off
    p45_tmp = [SB([128, D], F32, f"p45_tmp{i}") for i in range(2)]
    p45_xo = [SB([128, D], F32, f"p45_xo{i}") for i in range(2)]
    p45_ss = SB([128, 4], F32, "p45_ss")
    base45 = arena_state["off"]
    p4_woa = SB([128, 4, D], BF16, "p4_woa")
    p4_wob = SB([64, 8, D], BF16, "p4_wob")
    p4_ms = [SB([128, 4, 512], BF16, f"p4_ms{i}") for i in range(2)]
    p4_mh = [SB([64, 8, 512], BF16, f"p4_mh{i}") for i in range(2)]

    def norm_resid_store(pb, xtile, xk, i2, dst_ap, dkey):
        for nh in range(2):
            jb, jk = nextjunk()
            ACT(jb[:, 0:512], psb[pb + nh][:], AF.Square, [f"ps{pb + nh}"], [jk, f"p45_ss{nh}"], scale=1.0 / 32,
                accum_out=p45_ss[:, nh:nh + 1])
        TT("dve", p45_ss[:, 2:3], p45_ss[:, 0:1], p45_ss[:, 1:2], ALU.add, ["p45_ss0", "p45_ss1"], ["p45_ss2"])
        ACT(p45_ss[:, 3:4], p45_ss[:, 2:3], AF.Ln, ["p45_ss2"], ["p45_ss3"], bias=EPS)
        ACT(p45_ss[:, 3:4], p45_ss[:, 3:4], AF.Exp, ["p45_ss3"], ["p45_ss3"], scale=-0.5)
        tmp, tk = p45_tmp[i2], f"p45_tmp{i2}"
        for nh in range(2):
            STT("dve", tmp[:, nh * 512:(nh + 1) * 512], psb[pb + nh][:], p45_ss[:, 3:4], gain[1][:, nh * 512:(nh + 1) * 512],
                ALU.mult, ALU.mult, [f"ps{pb + nh}", "p45_ss3", "gain1"], [tk])
        xo, xok = p45_xo[i2], f"p45_xo{i2}"
        TT("pool", xo[:], tmp[:], xtile, ALU.add, [tk, xk], [xok])
        DMA(dst_ap, xo[:], [xok], [dkey])

    def pass4(l, b, xsrc, srckey):
        load_gain(1, l, 1)
        DMA(p4_woa[:], w_out[l, 0:512, :].rearrange("(c p) n -> p c n", p=128), (), ["p4_woa"], eng="pool")
        DMA(p4_wob[:], w_out[l, 512:1024, :].rearrange("(j p) n -> p j n", p=64), (), ["p4_wob"], eng="pool")
        n = 0
        for g in range(NG):
            t0 = g * 512
            ms, mh = p4_ms[g % 2], p4_mh[g % 2]
            msk, mhk = f"p4_ms{g % 2}", f"p4_mh{g % 2}"
            DMA(ms[:], d_ssd[b].rearrange("(c p) s -> p c s", p=128)[:, :, t0:t0 + 512], [f"d_ssd{b}_{g}"], [msk])
            DMA(mh[:, 0:4, :], d_msb[b].rearrange("h d s -> d h s")[:, :, t0:t0 + 512], [f"d_msb{b}_{g}"], [mhk])
            DMA(mh[:, 4:8, :], d_mmo[b].rearrange("h d s -> d h s")[:, :, t0:t0 + 512], [f"d_mmo{b}_{g}"], [mhk])
            for tt in range(4):
                tok = slice(t0 + tt * 128, t0 + (tt + 1) * 128)
                xi = n % 4
                DMA(xt[xi][:], xsrc[tok, :], [f"{srckey}_{(t0 + tt * 128) // 1024}"], [f"xt{xi}"])
                pb = 4 + 2 * (n % 2)
                tsl = slice(tt * 128, (tt + 1) * 128)
                for nh in range(2):
                    cs = slice(nh * 512, (nh + 1) * 512)
                    for c in range(4):
                        MM(psb[pb + nh][:], ms[:, c, tsl], p4_woa[:, c, cs], c == 0, False, [msk, "p4_woa"], [f"ps{pb + nh}"])
                    for j in range(8):
                        MM(psb[pb + nh][:], mh[:, j, tsl], p4_wob[:, j, cs], False, j == 7, [mhk, "p4_wob"], [f"ps{pb + nh}"])
                norm_resid_store(pb, xt[xi][:], f"xt{xi}", n % 2, d_x1[b][tok, :], f"d_x1{b}_{(t0 + tt * 128) // 1024}")
                n += 1

    arena_state["off"] = base45
    p5_hT = SB([128, 8, 1024], BF16, "p5_hT")
    p5_act = SB([128, NHC, 1024], BF16, "p5_act")
    p5_wd = SB([128, NHC, D], BF16, "p5_wd")
    p5_wg = [SB([128, 8, 256], BF16, f"p5_wg{i}") for i in range(2)]
    p5_wu = [SB([128, 8, 256], BF16, f"p5_wu{i}") for i in range(2)]
    p5_sg = [SB([128, 512], F32, f"p5_sg{i}") for i in range(2)]

    def pass5(l, b, xdst, dstkey):
        load_gain(0, l, 2)
        load_gain(1, l, 3)
        for (c0, c1) in [(0, 11), (11, 22)]:
            DMA(p5_wd[:, c0:c1, :], w_down[l, c0 * 128:c1 * 128, :].rearrange("(c p) n -> p c n", p=128), (), ["p5_wd"], eng="pool")
        n = 0
        wi = 0
        for G in range(S // 1024 if not ng_limit else max(1, NG // 2)):
            T0 = G * 1024
            def ld5(tt_):
                DMA(xt[(n0 + tt_) % 4][:], d_x1[b][T0 + tt_ * 128:T0 + (tt_ + 1) * 128, :], [f"d_x1{b}_{G}"], [f"xt{(n0 + tt_) % 4}"])

            n0 = n
            for tt_ in range(4):
                ld5(tt_)
            for tt in range(8):
                xi = n % 4
                n += 1
                jb, jk = nextjunk()
                ACT(jb[:], xt[xi][:], AF.Square, [f"xt{xi}"], [jk, "p5_ssa"], scale=1.0 / 32, accum_out=ss4[:, 0:1])
                ACT(rs4[:, 0:1], ss4[:, 0:1], AF.Ln, ["p5_ssa"], ["p5_rsa"], bias=EPS)
                ACT(rs4[:, 0:1], rs4[:, 0:1], AF.Exp, ["p5_rsa"], ["p5_rsa"], scale=-0.5)
                hbk = f"hb{tt % 2}"
                STT("dve", hb[tt % 2][:], xt[xi][:], rs4[:, 0:1], gain[0][:], ALU.mult, ALU.mult, [f"xt{xi}", "p5_rsa", "gain0"], [hbk])
                if tt + 4 < 8:
                    ld5(tt + 4)
                bk = 4 + (tt % 4)
                pT = psbf(bk)
                for c in range(8):
                    TR(pT[:, c * 128:(c + 1) * 128], hb[tt % 2][:, c * 128:(c + 1) * 128], ident_b[:], [hbk, "C_ident_b"], [f"ps{bk}"])
                CP("act" if tt % 2 else "dve", p5_hT[:, :, tt * 128:(tt + 1) * 128], pT.rearrange("p (c k) -> p c k", k=128),
                   [f"ps{bk}"], ["p5_hT"])
            it = 0
            for hq in range(0, NHC, 2):
                nq_ = min(2, NHC - hq)
                wg, wu = p5_wg[wi % 2], p5_wu[wi % 2]
                wgk, wuk = f"p5_wg{wi % 2}", f"p5_wu{wi % 2}"
                wi += 1
                cols = slice(hq * 128, (hq + nq_) * 128)
                DMA(wg[:, :, 0:nq_ * 128], w_gate[l, :, cols].rearrange("(c p) n -> p c n", p=128), (), [wgk], eng="pool")
                DMA(wu[:, :, 0:nq_ * 128], w_up[l, :, cols].rearrange("(c p) n -> p c n", p=128), (), [wuk], eng="pool")
                for hl in range(nq_):
                    hcx = hq + hl
                    for half in range(2):
                        i2 = it % 2
                        it += 1
                        ba, bb = 2 * i2, 2 * i2 + 1
                        hs = slice(half * 512, (half + 1) * 512)
                        for c in range(8):
                            MM(psb[ba][:], wg[:, c, hl * 128:(hl + 1) * 128], p5_hT[:, c, hs], c == 0, c == 7, [wgk, "p5_hT"], [f"ps{ba}"])
                        for c in range(8):
                            MM(psb[bb][:], wu[:, c, hl * 128:(hl + 1) * 128], p5_hT[:, c, hs], c == 0, c == 7, [wuk, "p5_hT"], [f"ps{bb}"])
                        ACT(p5_sg[i2][:], psb[ba][:], AF.Silu, [f"ps{ba}"], [f"p5_sg{i2}"])
                        TT("dve", p5_act[:, hcx, hs], p5_sg[i2][:], psb[bb][:], ALU.mult, [f"p5_sg{i2}", f"ps{bb}"], ["p5_act"])
            for tt in range(8):
                tok = slice(T0 + tt * 128, T0 + (tt + 1) * 128)
                xi = n % 4
                n += 1
                DMA(xt[xi][:], d_x1[b][tok, :], [f"d_x1{b}_{G}"], [f"xt{xi}"])
                pb = 4 + 2 * (tt % 2)
                tsl = slice(tt * 128, (tt + 1) * 128)
                for nh in range(2):
                    for hcx in range(NHC):
                        MM(psb[pb + nh][:], p5_act[:, hcx, tsl], p5_wd[:, hcx, nh * 512:(nh + 1) * 512], hcx == 0, hcx == NHC - 1,
                           ["p5_act", "p5_wd"], [f"ps{pb + nh}"])
                norm_resid_store(pb, xt[xi][:], f"xt{xi}", tt % 2, xdst[tok, :], f"{dstkey}_{G}")

    for l in range(DEPTH):
        for b in range(NSEQ):
            xsrc = x_in[b] if l == 0 else d_x2[b]
            srckey = f"xin{b}" if l == 0 else f"d_x2{b}"
            if 1 in passes:
                barrier()
                pass1(l, b, xsrc)
            if 2 in passes:
                barrier()
                pass2(l, b)
            if 3 in passes:
                barrier()
                pass3(l, b)
            if 4 in passes:
                barrier()
                pass4(l, b, xsrc, srckey)
            if 5 in passes:
                barrier()
                if l == DEPTH - 1:
                    pass5(l, b, out[b], f"out{b}")
                else:
                    pass5(l, b, d_x2[b], f"d_x2{b}")
    finals = list(P.dma_prev.values())
    P.emit(final_waits=finals)
    return nc, hc


def prep_inputs(inputs, DEPTH):
    f = lambda a: np.ascontiguousarray(np.asarray(a, dtype=np.float32))
    d = {}
    for k in ["w_in", "w_out", "w_gate", "w_up", "w_down"]:
        d[k] = f(inputs[k])
    d["vec1024"] = f(np.stack([inputs["pre_mix_norm"], inputs["post_mix_norm"], inputs["pre_ffn_norm"], inputs["post_ffn_norm"]], axis=1))
    cwv = np.asarray(inputs["conv_w"], np.float32)
    d["convw"] = f(cwv.reshape(DEPTH, 4, 8, 128).transpose(0, 3, 2, 1))
    d["convb"] = f(np.asarray(inputs["conv_b"], np.float32).reshape(DEPTH, 8, 128).transpose(0, 2, 1))
    d["hv"] = f(np.stack([inputs["dt_bias"], inputs["a_log"], inputs["d_skip"]], axis=1))
    d["ssdn"] = f(inputs["ssd_norm"])
    d["sbn"] = f(np.asarray(inputs["sb_norm"], np.float32).reshape(DEPTH, 4, 64).transpose(0, 2, 1))
    d["mon"] = f(np.asarray(inputs["moba_norm"], np.float32).reshape(DEPTH, 4, 64).transpose(0, 2, 1))
    return d


def kernel(**inputs):
    x = np.asarray(inputs["x"], np.float32)
    B, S, _ = x.shape
    DEPTH = inputs["w_in"].shape[0]
    NCORE = 8
    NSEQ = B // NCORE
    nc, hc = build(S, NSEQ, DEPTH)
    shared = prep_inputs(inputs, DEPTH)
    for k, v in hc.items():
        shared["c_" + k] = v
    in_maps = []
    for c in range(NCORE):
        m = dict(shared)
        m["x"] = np.ascontiguousarray(x[c * NSEQ:(c + 1) * NSEQ])
        in_maps.append(m)
    res = run_bass_kernel_spmd(nc, in_maps, core_ids=list(range(NCORE)))
    return np.concatenate([r["out"] for r in res.results], axis=0).astype(np.float32)
```

```python
import contextlib
import numpy as np
import ml_dtypes
import concourse.bass as bass
import concourse.mybir as mybir
from concourse.bass_utils import run_bass_kernel_spmd

F32 = mybir.dt.float32
BF16 = mybir.dt.bfloat16
AF = mybir.ActivationFunctionType
ALU = mybir.AluOpType
AX = mybir.AxisListType

D = 1024
INC = 3080
FH = 2816
NHC = 22
EPS = 1e-6
BIG = 30000.0
ENGS = ["pe", "act", "dve", "pool", "sp"]
EPOCH = 12000
NDMA = 12


class Prog:
    def __init__(self, nc):
        self.nc = nc
        self.ins = {e: [] for e in ENGS}
        self.lastw = {}
        self.readers = {}
        self.dma_slot = {e: 0 for e in ENGS}
        self.dma_prev = {}

    def add(self, eng, fn, reads=(), writes=(), dma=False):
        lst = self.ins[eng]
        me = dict(eng=eng, idx=len(lst), fn=fn, deps=set(), dma=dma, sig=False)
        deps = me["deps"]
        for r in reads:
            w = self.lastw.get(r)
            if w is not None:
                deps.add((w["eng"], w["idx"]))
        for wr in writes:
            w = self.lastw.get(wr)
            if w is not None and (w["eng"] != eng or w["dma"] or dma or eng != "pe"):
                deps.add((w["eng"], w["idx"]))
            for rd in self.readers.get(wr, ()):
                if rd["eng"] != eng or rd["dma"] or dma or eng != "pe":
                    deps.add((rd["eng"], rd["idx"]))
        if dma:
            slot = self.dma_slot[eng]
            self.dma_slot[eng] = (slot + 1) % NDMA
            me["slot"] = slot
            prev = self.dma_prev.get((eng, slot))
            if prev is not None:
                deps.add((prev["eng"], prev["idx"]))
            self.dma_prev[(eng, slot)] = me
        deps.discard((eng, me["idx"]))
        for r in reads:
            self.readers.setdefault(r, []).append(me)
        for wr in writes:
            self.lastw[wr] = me
            self.readers[wr] = []
        lst.append(me)
        return me

    def emit(self, final_waits=()):
        nc = self.nc
        for e in ENGS:
            for me in self.ins[e]:
                for (pe, pi) in me["deps"]:
                    self.ins[pe][pi]["sig"] = True
        for me in final_waits:
            me["sig"] = True
        nep = {}
        for e in ENGS:
            c = 0
            for me in self.ins[e]:
                if me["dma"]:
                    continue
                if me["sig"]:
                    c += 1
                    me["cnt"] = c
            nep[e] = c // EPOCH + 1
        dcount = {}
        for e in ENGS:
            for me in self.ins[e]:
                if me["dma"]:
                    k = (e, me["slot"])
                    dcount[k] = dcount.get(k, 0) + 16
                    me["cnt"] = dcount[k]
        with contextlib.ExitStack() as st:
            esem = {e: [st.enter_context(nc.semaphore(f"s_{e}_{i}")) for i in range(nep[e])] for e in ENGS}
            dsem = {}
            for k in dcount:
                dsem[k] = st.enter_context(nc.semaphore(f"d_{k[0]}_{k[1]}"))
            block = st.enter_context(nc.Block())

            def target(p):
                if p["dma"]:
                    return dsem[(p["eng"], p["slot"])], p["cnt"]
                c = p["cnt"]
                ep = (c - 1) // EPOCH
                return esem[p["eng"]][ep], c - ep * EPOCH

            def run(e, engobj):
                waited = {}
                for me in self.ins[e]:
                    best = {}
                    for (pe, pi) in me["deps"]:
                        p = self.ins[pe][pi]
                        key = ("d", pe, p["slot"]) if p["dma"] else ("e", pe)
                        if key not in best or best[key]["idx"] < p["idx"]:
                            best[key] = p
                    for key, p in best.items():
                        if waited.get(key, 0) >= p["cnt"]:
                            continue
                        waited[key] = p["cnt"]
                        sem, val = target(p)
                        engobj.wait_ge(sem, val)
                    r = me["fn"](engobj)
                    if me["dma"]:
                        r.then_inc(dsem[(e, me["slot"])], 16)
                    elif me["sig"]:
                        sem, _ = target(me)
                        r.then_inc(sem, 1)
                if e == "sp":
                    for p in final_waits:
                        sem, val = target(p)
                        engobj.wait_ge(sem, val)

            block.tensor(lambda eng: run("pe", eng))
            block.scalar(lambda eng: run("act", eng))
            block.vector(lambda eng: run("dve", eng))
            block.gpsimd(lambda eng: run("pool", eng))
            block.sync(lambda eng: run("sp", eng))


def host_consts(S):
    NBLK = S // 256
    p = np.arange(128)
    c = {}
    c["ident_f"] = np.eye(128, dtype=np.float32)
    c["tri_incl"] = (p[:, None] <= p[None, :]).astype(np.float32)
    c["tri_strict"] = (p[:, None] < p[None, :]).astype(np.float32)
    c["ones_f"] = np.ones((128, 128), np.float32)
    bf = ml_dtypes.bfloat16
    c["ut_b"] = (p[:, None] >= p[None, :]).astype(bf)
    c["ones_b"] = np.ones((128, 512), bf)
    c["zeros_b"] = np.zeros((128, 128), bf)
    nm = np.where(p[None, :] < p[:, None], -BIG, 0.0).astype(np.float32)
    c["negmask_b"] = np.tile(nm[:, None, :], (1, 8, 1)).reshape(128, 1024).astype(bf)
    own = np.arange(NBLK + 1)
    n = np.arange(NBLK)
    vm = np.where(n[None, :] < own[:, None], 0.0, -1e30).astype(np.float32)
    om = (n[None, :] == own[:, None]).astype(np.float32)
    c["vmask"] = np.tile(vm.reshape(1, -1), (128, 1))
    c["ownmask"] = np.tile(om.reshape(1, -1), (128, 1))
    s = np.arange(S)
    kaug = np.zeros((NBLK + 1, S), np.float32)
    kaug[s // 256, s] = 1.0
    kaug[NBLK, :] = 1.0
    c["kaug"] = kaug.astype(bf)
    slopes = 2.0 ** (-8.0 * np.arange(1, 5) / 4)
    c["alibi_row"] = (-slopes[:, None] * np.arange(512)[None, :]).astype(bf)
    rel = np.arange(32)
    ab = slopes[None, :, None] * (p[:, None, None] + 384 - 128 * rel[None, None, :])
    c["alibi_kb"] = ab.astype(np.float32)
    return c


import os as _os
KSTOP = int(_os.environ.get('KSTOP', '0'))


def build(S, NSEQ, DEPTH, dbg=False, passes=(1, 2, 3, 4, 5), ng_limit=None):
    nc = bass.Bass("TRN2", target_bir_lowering=False)
    NG, NT, NBLK = S // 512, S // 128, S // 256
    if ng_limit:
        NG = ng_limit
    KA = 64 + NBLK + 1
    hc = host_consts(S)

    def din(name, shape, dt=F32):
        return nc.dram_tensor(name, list(shape), dt, kind="ExternalInput").ap()

    def dscr(name, shape, dt):
        return nc.dram_tensor(name, list(shape), dt, kind=("ExternalOutput" if dbg else "Internal")).ap()

    x_in = din("x", [NSEQ, S, D])
    w_in = din("w_in", [DEPTH, D, INC])
    w_out = din("w_out", [DEPTH, D, D])
    w_gate = din("w_gate", [DEPTH, D, FH])
    w_up = din("w_up", [DEPTH, D, FH])
    w_down = din("w_down", [DEPTH, FH, D])
    vec1024 = din("vec1024", [DEPTH, 4, D])
    convw = din("convw", [DEPTH, 128, 8, 4])
    convb = din("convb", [DEPTH, 128, 8])
    hv = din("hv", [DEPTH, 3, 8])
    ssdn = din("ssdn", [DEPTH, 512])
    sbn = din("sbn", [DEPTH, 64, 4])
    mon = din("mon", [DEPTH, 64, 4])
    cd = {}
    for k, v in hc.items():
        cd[k] = din("c_" + k, v.shape, BF16 if v.dtype == ml_dtypes.bfloat16 else F32)
    out = nc.dram_tensor("out", [NSEQ, S, D], F32, kind="ExternalOutput").ap()

    d_ssd = [dscr(f"d_ssd{b}", [512, S], BF16) for b in range(NSEQ)]
    d_sbq = [dscr(f"d_sbq{b}", [256, S], BF16) for b in range(NSEQ)]
    d_sbk = [dscr(f"d_sbk{b}", [256, S], BF16) for b in range(NSEQ)]
    d_sbv = [dscr(f"d_sbv{b}", [S, 256], BF16) for b in range(NSEQ)]
    d_moq = [dscr(f"d_moq{b}", [256, S], BF16) for b in range(NSEQ)]
    d_mok = [dscr(f"d_mok{b}", [256, S], BF16) for b in range(NSEQ)]
    d_mov = [dscr(f"d_mov{b}", [S, 256], BF16) for b in range(NSEQ)]
    d_sel = [dscr(f"d_sel{b}", [4 * NBLK, S], BF16) for b in range(NSEQ)]
    d_msb = [dscr(f"d_msb{b}", [4, 64, S], BF16) for b in range(NSEQ)]
    d_mmo = [dscr(f"d_mmo{b}", [4, 64, S], BF16) for b in range(NSEQ)]
    d_x1 = [dscr(f"d_x1{b}", [S, D], F32) for b in range(NSEQ)]
    d_x2 = [dscr(f"d_x2{b}", [S, D], F32) for b in range(NSEQ)]

    dbg_t = dscr("dbg_t", [128, 256], F32)
    P = Prog(nc)
    sb_ = nc.alloc_sbuf_tensor
    _n = [0]
    ARENA_BYTES = 192 * 1024
    arena_state = {"on": False, "off": 0, "t": None}

    def SB(shape, dt, name=None):
        _n[0] += 1
        if not arena_state["on"]:
            return sb_(name or f"t{_n[0]}", list(shape), dt)
        free = int(np.prod(shape[1:]))
        nb = free * (2 if dt == BF16 else 4)
        nb = (nb + 63) // 64 * 64
        off = arena_state["off"]
        assert off + nb <= ARENA_BYTES, (name, off, nb)
        ap = arena_state["t"][0:shape[0], off // 4:(off + nb) // 4]
        if dt == BF16:
            ap = ap.bitcast(BF16)
        ap = ap[:, 0:free]
        if len(shape) == 3:
            ap = ap.rearrange("p (a b) -> p a b", b=shape[2])
        elif len(shape) == 4:
            ap = ap.rearrange("p (a b c) -> p a b c", b=shape[2], c=shape[3])
        arena_state["off"] = off + nb
        return ap

    def DMA(out_, in_, reads, writes, eng="sp"):
        return P.add(eng, lambda e: e.dma_start(out=out_, in_=in_), reads, writes, dma=True)

    def MM(out_, lhsT, rhs, start, stop, reads, writes):
        return P.add("pe", lambda e: e.matmul(out_, lhsT=lhsT, rhs=rhs, start=start, stop=stop), reads, writes)

    def TR(out_, in_, ident, reads, writes):
        return P.add("pe", lambda e: e.transpose(out=out_, in_=in_, identity=ident), reads, writes)

    def ACT(out_, in_, func, reads, writes, **kw):
        return P.add("act", lambda e: e.activation(out=out_, in_=in_, func=func, **kw), reads, writes)

    def TT(eng, out_, in0, in1, op, reads, writes):
        return P.add(eng, lambda e: e.tensor_tensor(out=out_, in0=in0, in1=in1, op=op), reads, writes)

    def TS(eng, out_, in0, s1, s2, op0, op1, reads, writes):
        if s2 is None:
            return P.add(eng, lambda e: e.tensor_scalar(out=out_, in0=in0, scalar1=s1, scalar2=None, op0=op0), reads, writes)
        return P.add(eng, lambda e: e.tensor_scalar(out=out_, in0=in0, scalar1=s1, scalar2=s2, op0=op0, op1=op1), reads, writes)

    def STT(eng, out_, in0, scalar, in1, op0, op1, reads, writes):
        return P.add(eng, lambda e: e.scalar_tensor_tensor(out=out_, in0=in0, scalar=scalar, in1=in1, op0=op0, op1=op1), reads, writes)

    def CP(eng, out_, in_, reads, writes):
        if eng == "act":
            return ACT(out_, in_, AF.Copy, reads, writes)
        return P.add(eng, lambda e: e.tensor_copy(out=out_, in_=in_), reads, writes)

    def MS(eng, ap, val, writes):
        return P.add(eng, lambda e: e.memset(ap, val), (), writes)

    psb = [nc.alloc_psum_tensor(f"ps{i}", [128, 512], F32) for i in range(8)]

    def psbf(i):
        return psb[i][:].bitcast(BF16)

    ident_f = SB([128, 128], F32, "ident_f")
    ident_b = SB([128, 128], BF16, "ident_b")
    tri_incl = SB([128, 128], F32, "tri_incl")
    tri_strict = SB([128, 128], F32, "tri_strict")
    ones_f = SB([128, 128], F32, "ones_f")
    ut_b = SB([128, 128], BF16, "ut_b")
    ones_b = SB([128, 512], BF16, "ones_b")
    zeros_b = SB([128, 128], BF16, "zeros_b")
    negmask_b = SB([128, 1024], BF16, "negmask_b")
    vmask = SB([128, (NBLK + 1) * NBLK], F32, "vmask")
    ownmask = SB([128, (NBLK + 1) * NBLK], F32, "ownmask")
    alibi_kb = SB([128, 4, 32], F32, "alibi_kb")
    for nm_, t in [("ident_f", ident_f), ("tri_incl", tri_incl), ("tri_strict", tri_strict), ("ones_f", ones_f),
                   ("ut_b", ut_b), ("ones_b", ones_b), ("zeros_b", zeros_b), ("negmask_b", negmask_b),
                   ("vmask", vmask), ("ownmask", ownmask), ("alibi_kb", alibi_kb)]:
        DMA(t[:], cd[nm_], (), ["C_" + nm_])
    CP("dve", ident_b[:], ident_f[:], ["C_ident_f"], ["C_ident_b"])
    CONST = ["C_ident_f", "C_ident_b", "C_tri_incl", "C_tri_strict", "C_ones_f", "C_ut_b", "C_ones_b", "C_zeros_b",
             "C_negmask_b", "C_vmask", "C_ownmask", "C_alibi_kb"]

    bar_sb = {e: SB([128, 8], F32, f"bar_{e}") for e in ("act", "dve", "pool")}
    bar_dram = nc.dram_tensor("bar_dram", [2, 64], F32).ap()

    def barrier():
        keys = []
        P.add("pe", lambda e: e.matmul(psb[7][:, 0:8], lhsT=zeros_b[:, 0:128], rhs=zeros_b[:, 0:8], start=True, stop=True),
              ["C_zeros_b"], ["ps7", "bar_pe"])
        for en in ("act", "dve", "pool"):
            CP(en, bar_sb[en][:], ones_f[:, 0:8], ["C_ones_f"], [f"bar_{en}"])
        allk = ["bar_pe", "bar_act", "bar_dve", "bar_pool"]
        dmas = {(d["eng"], d["idx"]) for d in P.dma_prev.values()}
        m = P.add("pe", lambda e: e.matmul(psb[7][:, 0:8], lhsT=zeros_b[:, 0:128], rhs=zeros_b[:, 0:8], start=True, stop=True),
                  ["C_zeros_b"] + allk, ["ps7", "bar2_pe"])
        m["deps"] |= dmas
        for en in ("act", "dve", "pool"):
            m = CP(en, bar_sb[en][:], ones_f[:, 0:8], ["C_ones_f"] + allk, [f"bar2_{en}"])
            m["deps"] |= dmas
        m = DMA(bar_dram[1:2, :], cd["ones_f"][0:1, 0:64], allk, ["bar2_sp"])
        m["deps"] |= {d for d in dmas if d != (m["eng"], m["idx"])}

    arena_state["t"] = sb_("arena", [128, ARENA_BYTES // 4], F32)
    arena_state["on"] = True
    gain = [SB([128, D], F32, f"gain{i}") for i in range(2)]
    xt = [SB([128, D], F32, f"xt{i}") for i in range(4)]
    junks = [SB([128, D], BF16, f"junk{i}") for i in range(4)]
    _jr = [0]

    def nextjunk():
        _jr[0] = (_jr[0] + 1) % 4
        return junks[_jr[0]], f"junk{_jr[0]}"
    ss4 = SB([128, 8], F32, "ss4")
    rs4 = SB([128, 8], F32, "rs4")
    hb = [SB([128, D], BF16, f"hb{i}") for i in range(2)]

    def load_gain(slot, l, which):
        DMA(gain[slot][:], vec1024[l, which].partition_broadcast(128), (), [f"gain{slot}"])

    base_off = arena_state["off"]

    win = SB([128, 8, INC], BF16, "win")
    hT = SB([128, 8, 512], BF16, "hT")
    zs = SB([128, 4, 512], F32, "zs")
    ubuf = [SB([128, 515], F32, f"u{i}") for i in range(2)]
    halo = SB([128, 8, 3], F32, "halo")
    cta = [SB([128, 512], F32, f"cta{i}") for i in range(2)]
    ctb = [SB([128, 512], F32, f"ctb{i}") for i in range(2)]
    xsT = SB([128, 4, 512], F32, "xsT")
    xs_tok = [SB([128, 512], F32, f"xstok{i}") for i in range(4)]
    BT = SB([128, 2, 512], BF16, "BT")
    CT = SB([128, 2, 512], BF16, "CT")
    Btok = SB([128, 4, 2, 128], BF16, "Btok")
    cw = SB([128, 8, 4], F32, "cw")
    cb = SB([128, 8], F32, "cb")
    hv_bc = SB([128, 3, 8], F32, "hv_bc")
    a_bc = SB([128, 8], F32, "a_bc")
    dsk_bc = SB([128, 8, 64], F32, "dsk_bc")
    ssdn_bc = SB([128, 512], F32, "ssdn_bc")
    dtx = SB([128, 4, 8], F32, "dtx")
    dt_all = SB([128, 4, 8], F32, "dt_all")
    adt = SB([128, 4, 8], F32, "adt")
    sbq_st = SB([128, 2, 512], BF16, "sbq_st")
    sbk_st = SB([128, 2, 512], BF16, "sbk_st")
    sbv_st = SB([128, 4, 256], BF16, "sbv_st")
    moq32 = SB([128, 2, 512], F32, "moq32")
    mok32 = SB([128, 2, 512], F32, "mok32")
    moq_st = SB([128, 2, 512], BF16, "moq_st")
    mok_st = SB([128, 2, 512], BF16, "mok_st")
    mov_st = SB([128, 4, 256], BF16, "mov_st")
    kmean = SB([128, 2, 2, NBLK], F32, "kmean")
    gm = SB([128, 4, NBLK], F32, "gm")
    max8 = SB([128, 4, 8], F32, "max8")
    thr = SB([128, 4], F32, "thr")
    sel = SB([128, 4, NBLK], F32, "sel")
    selneg = SB([128, 4 * NBLK], BF16, "selneg")
    selT_st = SB([64, 512], BF16, "selT_st")
    rhs_cum = SB([128, 8, 128], F32, "rhs_cum")
    negac4 = SB([128, 4, 8], F32, "negac4")
    ea4 = SB([128, 4, 8], F32, "ea4")
    cdb4 = SB([128, 4, 8], F32, "cdb4")
    dtd4 = SB([128, 4, 8], F32, "dtd4")
    negac = SB([128, 8], F32, "negac")
    ea = SB([128, 8], F32, "ea")
    cdb = SB([128, 8], F32, "cdb")
    Eb = SB([128, 8, 128], F32, "Eb")
    MT = SB([128, 8, 128], BF16, "MT")
    dtd = SB([128, 8], F32, "dtd")
    xdt = SB([128, 8, 64], BF16, "xdt")
    xdt2 = SB([128, 8, 64], BF16, "xdt2")
    S32 = SB([128, 512], F32, "S32")
    Sb = SB([128, 512], BF16, "Sb")
    y1 = SB([128, 512], F32, "y1")
    y2 = SB([128, 512], F32, "y2")
    y3 = SB([128, 512], F32, "y3")
    ssq = SB([128, 2], F32, "ssq")
    rsq = SB([128, 2], F32, "rsq")
    yn = SB([128, 512], BF16, "yn")
    mixT_ssd = SB([128, 4, 512], BF16, "mixT_ssd")

    rot01 = [0]

    def nextbank():
        rot01[0] ^= 1
        return rot01[0]

    def load_layer_p1(l):
        for (c0, c1) in [(0, 512), (512, 1536), (1536, 2312), (2312, INC)]:
            DMA(win[:, :, c0:c1], w_in[l, :, c0:c1].rearrange("(c p) n -> p c n", p=128), (), ["win"], eng="pool")
        DMA(cw[:], convw[l], (), ["cw"])
        DMA(cb[:], convb[l], (), ["cb"])
        for i in range(3):
            DMA(hv_bc[:, i, :], hv[l, i].partition_broadcast(128), (), ["hv_bc"])
        DMA(ssdn_bc[:], ssdn[l].partition_broadcast(128), (), ["ssdn_bc"])
        ACT(a_bc[:], hv_bc[:, 1, :], AF.Exp, ["hv_bc"], ["a_bc"])
        TS("dve", a_bc[:], a_bc[:], -1.0, None, ALU.mult, None, ["a_bc"], ["a_bc"])
        CP("dve", dsk_bc[:], hv_bc[:, 2, :].unsqueeze(2).to_broadcast([128, 8, 64]), ["hv_bc"], ["dsk_bc"])

    def pass1(l, b, xsrc):
        load_layer_p1(l)
        load_gain(0, l, 0)
        MS("pool", S32[:], 0.0, ["S32"])
        MS("pool", Sb[:], 0.0, ["Sb"])
        MS("pool", halo[:], 0.0, ["halo"])
        MS("pool", kmean[:], 0.0, ["kmean"])
        def load_x(g_):
            for tt_ in range(4):
                DMA(xt[tt_][:], xsrc[g_ * 512 + tt_ * 128:g_ * 512 + (tt_ + 1) * 128, :], (), [f"xt{tt_}"])

        load_x(0)
        for g in range(NG):
            t0 = g * 512
            for tt in range(4):
                jb, jk = nextjunk()
                ACT(jb[:], xt[tt][:], AF.Square, [f"xt{tt}"], [jk, f"ss4_{tt}"], scale=1.0 / 32, accum_out=ss4[:, tt:tt + 1])
            ACT(rs4[:, 0:4], ss4[:, 0:4], AF.Ln, [f"ss4_{i}" for i in range(4)], ["rs4"], bias=EPS)
            ACT(rs4[:, 0:4], rs4[:, 0:4], AF.Exp, ["rs4"], ["rs4"], scale=-0.5)
            for tt in range(4):
                hbk = f"hb{tt % 2}"
                STT("dve", hb[tt % 2][:], xt[tt][:], rs4[:, tt:tt + 1], gain[0][:], ALU.mult, ALU.mult,
                    [f"xt{tt}", "rs4", "gain0"], [hbk])
                bk = nextbank()
                pT = psbf(bk)
                for c in range(8):
                    TR(pT[:, c * 128:(c + 1) * 128], hb[tt % 2][:, c * 128:(c + 1) * 128], ident_b[:], [hbk, "C_ident_b"], [f"ps{bk}"])
                CP("act" if tt % 2 else "dve", hT[:, :, tt * 128:(tt + 1) * 128], pT.rearrange("p (c k) -> p c k", k=128),
                   [f"ps{bk}"], ["hT"])

            if g + 1 < NG:
                load_x(g + 1)

            def proj_fm(col0, ncols=128):
                bk = nextbank()
                for c in range(8):
                    MM(psb[bk][0:ncols, :], win[:, c, col0:col0 + ncols], hT[:, c, :], c == 0, c == 7, ["win", "hT"], [f"ps{bk}"])
                return bk

            def proj_tm(tt, col0, ncols, bk, off):
                for c in range(8):
                    MM(psb[bk][:, off:off + ncols], hT[:, c, tt * 128:(tt + 1) * 128], win[:, c, col0:col0 + ncols], c == 0, c == 7,
                       ["win", "hT"], [f"ps{bk}"])

            if KSTOP == 1:
                return
            for tt in range(4):
                bk = nextbank()
                proj_tm(tt, 0, 512, bk, 0)
                ACT(zs[:, tt, :], psb[bk][:], AF.Silu, [f"ps{bk}"], ["zs"])
            if KSTOP == 2:
                return
            for tt in range(4):
                for c in range(8):
                    MM(psb[4][:, 400 + tt * 8:408 + tt * 8], hT[:, c, tt * 128:(tt + 1) * 128], win[:, c, 1536:1544], c == 0, c == 7,
                       ["win", "hT"], ["ps4"])
            TT("dve", dtx[:], psb[4][:, 400:432].rearrange("p (t h) -> p t h", h=8),
               hv_bc[:, 0, :].unsqueeze(1).to_broadcast([128, 4, 8]), ALU.add, ["ps4", "hv_bc"], ["dtx"])
            ACT(dtx[:], dtx[:], AF.Exp, ["dtx"], ["dtx"])
            ACT(dt_all[:], dtx[:], AF.Ln, ["dtx"], ["dt_all"], bias=1.0)
            TT("dve", adt[:], dt_all[:], a_bc[:].unsqueeze(1).to_broadcast([128, 4, 8]), ALU.mult, ["dt_all", "a_bc"], ["adt"])
            if KSTOP == 3:
                return
            for j in range(8):
                bk = proj_fm(512 + 128 * j)
                u = ubuf[j % 2]
                uk = f"u{j % 2}"
                CP("dve", u[:, 0:3], halo[:, j, :], ["halo"], [uk])
                CP("dve", u[:, 3:515], psb[bk][:], [f"ps{bk}"], [uk])
                CP("pool", halo[:, j, :], u[:, 512:515], [uk], ["halo"])
                ta = cta[j % 2]
                tk = f"cta{j % 2}"
                ACT(ta[:], u[:, 0:512], AF.Copy, [uk, "cw"], [tk], scale=cw[:, j, 0:1])
                for k_ in range(1, 4):
                    STT("dve", ta[:], u[:, k_:k_ + 512], cw[:, j, k_:k_ + 1], ta[:], ALU.mult, ALU.add, [uk, "cw", tk], [tk])
                if j < 4:
                    dst, dk = xsT[:, j, :], "xsT"
                elif j < 6:
                    dst, dk = BT[:, j - 4, :], "BT"
                else:
                    dst, dk = CT[:, j - 6, :], "CT"
                ACT(dst, ta[:], AF.Silu, [tk, "cb"], [dk], bias=cb[:, j:j + 1])
            if KSTOP == 4:
                return
            for pr in range(2):
                bk = proj_fm(1544 + 128 * pr)
                ACT(sbq_st[:, pr, :], psb[bk][:], AF.Copy, [f"ps{bk}"], ["sbq_st"], scale=0.125)
                bk = proj_fm(1800 + 128 * pr)
                CP("dve", sbk_st[:, pr, :], psb[bk][:], [f"ps{bk}"], ["sbk_st"])
            for t2 in range(2):
                bk = nextbank()
                for q in range(2):
                    proj_tm(2 * t2 + q, 2056, 256, bk, q * 256)
                CP("act", sbv_st[:, 2 * t2:2 * t2 + 2, :], psb[bk][:].rearrange("p (t c) -> p t c", c=256), [f"ps{bk}"], ["sbv_st"])
            DMA(d_sbq[b].rearrange("(r p) s -> p r s", p=128)[:, :, t0:t0 + 512], sbq_st[:], ["sbq_st"], [f"d_sbq{b}_{g}"])
            DMA(d_sbk[b].rearrange("(r p) s -> p r s", p=128)[:, :, t0:t0 + 512], sbk_st[:], ["sbk_st"], [f"d_sbk{b}_{g}"])
            DMA(d_sbv[b][t0:t0 + 512, :].rearrange("(t p) c -> p t c", p=128), sbv_st[:], ["sbv_st"], [f"d_sbv{b}_{g}"])
            if KSTOP == 5:
                return
            for pr in range(2):
                bk = proj_fm(2312 + 128 * pr)
                CP("dve", moq32[:, pr, :], psb[bk][:], [f"ps{bk}"], ["moq32"])
                TS("pool", moq_st[:, pr, :], moq32[:, pr, :], 0.125, None, ALU.mult, None, ["moq32"], ["moq_st"])
                bk = proj_fm(2568 + 128 * pr)
                CP("act", mok32[:, pr, :], psb[bk][:], [f"ps{bk}"], ["mok32"])
                CP("pool", mok_st[:, pr, :], mok32[:, pr, :], ["mok32"], ["mok_st"])
                for e_ in range(2):
                    rows = slice(e_ * 64, e_ * 64 + 64)
                    P.add("dve", lambda e, pr=pr, g=g, e_=e_, rows=rows: e.tensor_reduce(
                        out=kmean[rows, pr, e_, 2 * g:2 * g + 2], in_=mok32[rows, pr, :].rearrange("p (b k) -> p b k", k=256),
                        axis=AX.X, op=ALU.add), ["mok32"], ["kmean"])
            for t2 in range(2):
                bk = nextbank()
                for q in range(2):
                    proj_tm(2 * t2 + q, 2824, 256, bk, q * 256)
                CP("dve", mov_st[:, 2 * t2:2 * t2 + 2, :], psb[bk][:].rearrange("p (t c) -> p t c", c=256), [f"ps{bk}"], ["mov_st"])
            DMA(d_moq[b].rearrange("(r p) s -> p r s", p=128)[:, :, t0:t0 + 512], moq_st[:], ["moq_st"], [f"d_moq{b}_{g}"])
            DMA(d_mok[b].rearrange("(r p) s -> p r s", p=128)[:, :, t0:t0 + 512], mok_st[:], ["mok_st"], [f"d_mok{b}_{g}"])
            DMA(d_mov[b][t0:t0 + 512, :].rearrange("(t p) c -> p t c", p=128), mov_st[:], ["mov_st"], [f"d_mov{b}_{g}"])
            if KSTOP == 6:
                return
            gps = psb[5][:, 0:16 * NBLK].rearrange("p (t h n) -> p t h n", t=4, h=4)
            for tt in range(4):
                for pr in range(2):
                    MM(gps[:, tt, 2 * pr:2 * pr + 2, :], moq32[:, pr, tt * 128:(tt + 1) * 128],
                       kmean[:, pr, :, :], True, True, ["moq32", "kmean"], ["ps5"])
            if KSTOP == 61:
                return
            for tt in range(4):
                own = 2 * g + tt // 2
                TT("dve", gm[:], gps[:, tt], vmask[:, own * NBLK:(own + 1) * NBLK].unsqueeze(1).to_broadcast([128, 4, NBLK]),
                   ALU.add, ["ps5", "C_vmask"], ["gm"])
                for h in range(4):
                    P.add("dve", lambda e, h=h: e.max(out=max8[:, h, :], in_=gm[:, h, :]), ["gm"], ["max8"])
                TS("dve", thr[:], max8[:, :, 2], -1e20, None, ALU.max, None, ["max8"], ["thr"])
                TT("dve", sel[:], gm[:], thr[:].unsqueeze(2).to_broadcast([128, 4, NBLK]), ALU.is_ge, ["gm", "thr"], ["sel"])
                TT("dve", sel[:], sel[:], ownmask[:, own * NBLK:(own + 1) * NBLK].unsqueeze(1).to_broadcast([128, 4, NBLK]),
                   ALU.add, ["sel", "C_ownmask"], ["sel"])
                TS("dve", selneg[:], sel[:].rearrange("p h n -> p (h n)"), -1.0, BIG, ALU.add, ALU.mult, ["sel"], ["selneg"])
                if dbg and g == NG - 1 and tt == 3:
                    DMA(dbg_t[:, 0:4 * NBLK], gm[:].rearrange("p h n -> p (h n)"), ["gm"], ["dbg_t"])
                    DMA(dbg_t[:, 64:96], max8[:].rearrange("p h n -> p (h n)"), ["max8"], ["dbg_t"])
                    DMA(dbg_t[:, 96:100], thr[:], ["thr"], ["dbg_t"])
                    DMA(dbg_t[:, 128:128 + 4 * NBLK], sel[:].rearrange("p h n -> p (h n)"), ["sel"], ["dbg_t"])
                if KSTOP == 62:
                    continue
                bk = nextbank()
                TR(psbf(bk)[0:4 * NBLK, 0:128], selneg[:], ident_b[:], ["selneg", "C_ident_b"], [f"ps{bk}"])
                CP("act", selT_st[0:4 * NBLK, tt * 128:(tt + 1) * 128], psbf(bk)[0:4 * NBLK, 0:128], [f"ps{bk}"], ["selT_st"])
            if KSTOP == 62:
                return
            DMA(d_sel[b][:, t0:t0 + 512], selT_st[0:4 * NBLK, :], ["selT_st"], [f"d_sel{b}_{g}"])
            if KSTOP == 7:
                return
            for tt in range(4):
                bk = nextbank()
                for j in range(4):
                    TR(psb[bk][:, j * 128:(j + 1) * 128], xsT[:, j, tt * 128:(tt + 1) * 128], ident_f[:], ["xsT", "C_ident_f"], [f"ps{bk}"])
                CP("act" if tt % 2 else "dve", xs_tok[tt][:], psb[bk][:], [f"ps{bk}"], [f"xstok{tt}"])
                bk = nextbank()
                for gi in range(2):
                    TR(psbf(bk)[:, gi * 128:(gi + 1) * 128], BT[:, gi, tt * 128:(tt + 1) * 128], ident_b[:], ["BT", "C_ident_b"], [f"ps{bk}"])
                CP("dve", Btok[:, tt, :, :], psbf(bk)[:, 0:256].rearrange("p (g n) -> p g n", n=128), [f"ps{bk}"], ["Btok"])
            if KSTOP == 8:
                return
            def ssd_chunk(tt):
                tsl = slice(tt * 128, (tt + 1) * 128)
                xs3 = xs_tok[tt][:].rearrange("p (h d) -> p h d", d=64)
                xk = f"xstok{tt}"
                negac, ea, cdb, dtd = negac4[:, tt, :], ea4[:, tt, :], cdb4[:, tt, :], dtd4[:, tt, :]
                nk, ek, ck, dk_ = f"negac{tt}", f"ea{tt}", f"cdb{tt}", f"dtd{tt}"
                TT("pool", y2[:].rearrange("p (h d) -> p h d", d=64), xs3, dsk_bc[:], ALU.mult, [xk, "dsk_bc"], ["y2"])
                TT("dve", rhs_cum[:], tri_incl[:].unsqueeze(1).to_broadcast([128, 8, 128]),
                   adt[:, tt, :].unsqueeze(2).to_broadcast([128, 8, 128]), ALU.mult, ["C_tri_incl", "adt"], ["rhs_cum"])
                rc = rhs_cum[:].rearrange("p h l -> p (h l)")
                for half in range(2):
                    pb = psb[2 + half]
                    MM(pb[:], ones_f[:], rc[:, half * 512:(half + 1) * 512], True, False, ["C_ones_f", "rhs_cum"], [f"ps{2 + half}"])
                    MM(pb[:], ident_b[:], negmask_b[:, half * 512:(half + 1) * 512], False, True, ["C_ident_b", "C_negmask_b"], [f"ps{2 + half}"])
                MM(psb[4][:, 0:8], tri_incl[:], adt[:, tt, :], True, True, ["C_tri_incl", "adt"], ["ps4"])
                yield
                ACT(negac, psb[4][:, 0:8], AF.Copy, ["ps4"], [nk], scale=-1.0)
                ACT(ea, psb[4][:, 0:8], AF.Exp, ["ps4"], [ek])
                for half in range(2):
                    ACT(cdb[:, half * 4:(half + 1) * 4], psb[2 + half][:].rearrange("p (h l) -> p h l", l=128)[:, :, 127], AF.Exp,
                        [f"ps{2 + half}"], [ck])
                for h in range(8):
                    ACT(Eb[:, h, :], psb[2 + h // 4][:, (h % 4) * 128:(h % 4 + 1) * 128], AF.Exp, [f"ps{2 + h // 4}", nk], ["Eb"],
                        bias=negac[:, h:h + 1])
                for gi in range(2):
                    MM(psb[4][:, 128 + gi * 128:256 + gi * 128], BT[:, gi, tsl], CT[:, gi, tsl], True, True, ["BT", "CT"], ["ps4"])
                yield
                for gi in range(2):
                    TT("dve", MT[:, gi * 4:(gi + 1) * 4, :], Eb[:, gi * 4:(gi + 1) * 4, :],
                       psb[4][:, 128 + gi * 128:256 + gi * 128].unsqueeze(1).to_broadcast([128, 4, 128]), ALU.mult,
                       ["Eb", "ps4"], ["MT"])
                TT("dve", dtd, dt_all[:, tt, :], Eb[:, :, 127], ALU.mult, ["dt_all", "Eb"], [dk_])
                TT("dve", xdt[:], xs3, dt_all[:, tt, :].unsqueeze(2).to_broadcast([128, 8, 64]), ALU.mult, [xk, "dt_all"], ["xdt"])
                TT("dve", xdt2[:], xs3, dtd.unsqueeze(2).to_broadcast([128, 8, 64]), ALU.mult, [xk, dk_], ["xdt2"])
                yield
                x2f = xdt2[:].rearrange("p h d -> p (h d)")
                x1f = xdt[:].rearrange("p h d -> p (h d)")
                for gi in range(2):
                    MM(psb[5][:, gi * 256:(gi + 1) * 256], Btok[:, tt, gi, :], x2f[:, gi * 256:(gi + 1) * 256], True, True,
                       ["Btok", "xdt2"], ["ps5"])
                for gi in range(2):
                    MM(psb[6][:, gi * 256:(gi + 1) * 256], CT[:, gi, tsl], Sb[:, gi * 256:(gi + 1) * 256], True, True,
                       ["CT", "Sb"], ["ps6"])
                for h in range(8):
                    MM(psb[7][:, h * 64:(h + 1) * 64], MT[:, h, :], x1f[:, h * 64:(h + 1) * 64], True, True, ["MT", "xdt"], ["ps7"])
                yield
                TT("dve", S32[:].rearrange("p (h d) -> p h d", d=64), S32[:].rearrange("p (h d) -> p h d", d=64),
                   cdb.unsqueeze(2).to_broadcast([128, 8, 64]), ALU.mult, ["S32", ck], ["S32"])
                TT("dve", S32[:], S32[:], psb[5][:], ALU.add, ["S32", "ps5"], ["S32"])
                CP("act", Sb[:], S32[:], ["S32"], ["Sb"])
                TT("dve", y1[:].rearrange("p (h d) -> p h d", d=64), psb[6][:].rearrange("p (h d) -> p h d", d=64),
                   ea.unsqueeze(2).to_broadcast([128, 8, 64]), ALU.mult, ["ps6", ek], ["y1"])
                TT("dve", y1[:], y1[:], psb[7][:], ALU.add, ["y1", "ps7"], ["y1"])
                TT("dve", y2[:], y2[:], y1[:], ALU.add, ["y2", "y1"], ["y2"])
                TT("dve", y3[:], y2[:], zs[:, tt, :], ALU.mult, ["y2", "zs"], ["y3"])
                for gi in range(2):
                    jb, jk = nextjunk()
                    ACT(jb[:, 0:256], y3[:, gi * 256:(gi + 1) * 256], AF.Square, ["y3"], [jk, f"ssq{gi}"], scale=1.0 / 16,
                        accum_out=ssq[:, gi:gi + 1])
                ACT(rsq[:], ssq[:], AF.Ln, ["ssq0", "ssq1"], ["rsq"], bias=EPS)
                ACT(rsq[:], rsq[:], AF.Exp, ["rsq"], ["rsq"], scale=-0.5)
                for gi in range(2):
                    STT("dve", yn[:, gi * 256:(gi + 1) * 256], y3[:, gi * 256:(gi + 1) * 256], rsq[:, gi:gi + 1],
                        ssdn_bc[:, gi * 256:(gi + 1) * 256], ALU.mult, ALU.mult, ["y3", "rsq", "ssdn_bc"], ["yn"])
                yield
                bk = nextbank()
                for j in range(4):
                    TR(psbf(bk)[:, j * 128:(j + 1) * 128], yn[:, j * 128:(j + 1) * 128], ident_b[:], ["yn", "C_ident_b"], [f"ps{bk}"])
                CP("act", mixT_ssd[:, :, tsl], psbf(bk)[:, 0:512].rearrange("p (c k) -> p c k", k=128), [f"ps{bk}"], ["mixT_ssd"])

            for tt in range(4):
                for _ in ssd_chunk(tt):
                    pass
            DMA(d_ssd[b].rearrange("(c p) s -> p c s", p=128)[:, :, t0:t0 + 512], mixT_ssd[:], ["mixT_ssd"], [f"d_ssd{b}_{g}"])

    arena_state["off"] = base_off
    p2_kT = SB([128, 2, S], BF16, "p2_kT")
    p2_v = SB([128, NT, 256], BF16, "p2_v")
    p2_q = [SB([128, 2, 512], BF16, f"p2_q{i}") for i in range(3)]
    p2_nq = [SB([128, 2, 512], BF16, f"p2_nq{i}") for i in range(3)]
    p2_e = [SB([128, 512], F32, f"p2_e{i}") for i in range(2)]
    p2_sp = [SB([128, 512], BF16, f"p2_sp{i}") for i in range(2)]
    p2_S = SB([128, 512], BF16, "p2_S")
    p2_w = [SB([128, 512], BF16, f"p2_w{i}") for i in range(2)]
    p23_y = SB([64, 4, 512], F32, "p23_y")
    p23_sq = SB([64, 4, 512], BF16, "p23_sq")
    p23_rs = SB([64, 512], F32, "p23_rs")
    p23_mix = SB([64, 4, 512], BF16, "p23_mix")
    p23_nw = SB([64, 4], F32, "p23_nw")

    def head_norm_store(l, b, g, dst, nb=6):
        t0 = g * 512
        for h in range(4):
            ACT(p23_sq[:, h, :], p23_y[:, h, :], AF.Square, ["p23_y"], ["p23_sq"])
        for h in range(4):
            MM(psb[nb][0:64, :], ones_b[0:64, 0:64], p23_sq[:, h, :], h == 0, h == 3, ["C_ones_b", "p23_sq"], [f"ps{nb}"])
        ACT(p23_rs[:], psb[nb][0:64, :], AF.Ln, [f"ps{nb}"], ["p23_rs"], scale=1.0 / 256, bias=EPS)
        ACT(p23_rs[:], p23_rs[:], AF.Exp, ["p23_rs"], ["p23_rs"], scale=-0.5)
        for h in range(4):
            STT("dve", p23_mix[:, h, :], p23_y[:, h, :], p23_nw[:, h:h + 1], p23_rs[:], ALU.mult, ALU.mult,
                ["p23_y", "p23_nw", "p23_rs"], ["p23_mix"])
        DMA(dst.rearrange("h d s -> d h s")[:, :, t0:t0 + 512], p23_mix[:], ["p23_mix"], [f"{dst.tensor.name}_{g}"])

    def run_pipeline(gens):
        active = []
        gi = iter(gens)
        more = True
        while True:
            if more:
                try:
                    active.append(next(gi))
                except StopIteration:
                    more = False
            if not active:
                break
            for gen in list(active):
                try:
                    next(gen)
                except StopIteration:
                    active.remove(gen)

    NB2 = 4
    p2_e3 = [SB([128, 512], F32, f"p2_e3{i}") for i in range(NB2)]
    p2_sp3 = [SB([128, 512], BF16, f"p2_sp3{i}") for i in range(NB2)]
    p2_w3 = [SB([128, 512], BF16, f"p2_w3{i}") for i in range(NB2)]
    p2_S2 = [SB([128, 512], BF16, f"p2_S2{i}") for i in range(4)]
    p2_t4 = [SB([128, 512], F32, f"p2_t4{i}") for i in range(NB2)]

    p2set = (p23_y, p23_sq, p23_rs, p23_mix, p23_nw)

    def pass2(l, b):
        nonlocal p23_y, p23_sq, p23_rs, p23_mix, p23_nw
        p23_y, p23_sq, p23_rs, p23_mix, p23_nw = p2set
        DMA(p23_nw[:], sbn[l], (), ["p23_nw"])

        def loads(g):
            t0 = g * 512
            DMA(p2_kT[:, :, t0:t0 + 512], d_sbk[b].rearrange("(r p) s -> p r s", p=128)[:, :, t0:t0 + 512],
                [f"d_sbk{b}_{g}"], [f"p2_kT_{g}"])
            DMA(p2_v[:, 4 * g:4 * g + 4, :], d_sbv[b][t0:t0 + 512, :].rearrange("(t p) c -> p t c", p=128),
                [f"d_sbv{b}_{g}"], [f"p2_v_{g}"])
            q, nq = p2_q[g % 3], p2_nq[g % 3]
            DMA(q[:], d_sbq[b].rearrange("(r p) s -> p r s", p=128)[:, :, t0:t0 + 512], [f"d_sbq{b}_{g}"], [f"p2_q{g % 3}"])

        def it_gen(g, h, kb, n):
            q, nq = p2_q[g % 3], p2_nq[g % 3]
            qk, nqk = f"p2_q{g % 3}", f"p2_nq{g % 3}"
            pr, e_ = h // 2, h % 2
            rows = slice(e_ * 64, e_ * 64 + 64)
            yb = 4 + (h % 2)
            last = 4 * g + 3
            gk = kb // 4
            qoff = max(0, kb - 4 * g) * 128
            W = 512 - qoff
            diag = kb >= 4 * g
            i3 = n % NB2
            b1, b2 = n % 2, 2 + n % 2
            ksl = slice(kb * 128, (kb + 1) * 128)
            e3, sp3, w3 = p2_e3[i3], p2_sp3[i3], p2_w3[i3]
            ek, spk, wk = f"p2_e3{i3}", f"p2_sp3{i3}", f"p2_w3{i3}"
            par = (last - kb) % 2
            S_in, S_out = p2_S2[2 * e_ + par], p2_S2[2 * e_ + 1 - par]
            Sik, Sok = f"p2_S2{2 * e_ + par}", f"p2_S2{2 * e_ + 1 - par}"
            if kb == last:
                if h == 0 and g + 1 < NG:
                    loads(g + 1)
                MM(psb[yb][0:64, :], zeros_b[:, 0:64], ones_b[:, 0:512], True, False, ["C_zeros_b", "C_ones_b"], [f"ps{yb}"])
            MM(psb[b1][:, 0:W], p2_kT[rows, pr, ksl], q[rows, pr, qoff:512], True, True, [f"p2_kT_{gk}", qk], [f"ps{b1}"])
            yield
            ACT(e3[:, 0:W], psb[b1][:, 0:W], AF.Exp, [f"ps{b1}"], [ek])
            yield
            ACT(sp3[:, 0:W], e3[:, 0:W], AF.Ln, [ek], [spk], bias=1.0)
            if diag:
                TT("pool", sp3[:, 0:128], sp3[:, 0:128], tri_strict[:], ALU.mult, [spk, "C_tri_strict"], [spk])
            yield
            MM(psb[b2][:, 0:W], ut_b[:], sp3[:, 0:W], True, kb == last, ["C_ut_b", spk], [f"ps{b2}"])
            if kb < last:
                MM(psb[b2][:, 0:W], ones_b[:, 0:128], S_in[:, qoff:512], False, True, ["C_ones_b", Sik], [f"ps{b2}"])
            if kb > 0:
                if kb == last:
                    if qoff > 0:
                        MS("pool", S_out[:, 0:qoff], 0.0, [Sok])
                    CP("dve", S_out[:, qoff:512], sp3[:, 0:W], [spk], [Sok])
                else:
                    if qoff > 0:
                        CP("pool", S_out[:, 0:qoff], S_in[:, 0:qoff], [Sik], [Sok])
                    TT("dve", S_out[:, qoff:512], S_in[:, qoff:512], sp3[:, 0:W], ALU.add, [Sik, spk], [Sok])
            yield
            t4, t4k = p2_t4[i3], f"p2_t4{i3}"
            ACT(t4[:, 0:W], psb[b2][:, 0:W], AF.Exp, [f"ps{b2}"], [t4k], scale=-1.0)
            TT("dve", w3[:, 0:W], t4[:, 0:W], e3[:, 0:W], ALU.mult, [t4k, ek], [wk])
            if diag:
                TT("pool", w3[:, 0:128], w3[:, 0:128], tri_strict[:], ALU.mult, [wk, "C_tri_strict"], [wk])
            yield
            MM(psb[yb][0:64, qoff:512], p2_v[:, kb, h * 64:(h + 1) * 64], w3[:, 0:W], False, kb == 0,
               [f"p2_v_{gk}", wk], [f"ps{yb}"])
            if kb == 0:
                CP("dve", p23_y[:, h, :], psb[yb][0:64, :], [f"ps{yb}"], ["p23_y"])
                if h == 3:
                    head_norm_store(l, b, g, d_msb[b])

        def all_iters():
            n = 0
            for g in range(NG):
                for h in range(4):
                    for kb in range(4 * g + 3, -1, -1):
                        yield it_gen(g, h, kb, n)
                        n += 1

        loads(0)
        run_pipeline(all_iters())

    arena_state["off"] = base_off
    p3_kT = [SB([96, S], BF16, f"p3_kT{h}") for h in range(4)]
    p3_va = SB([128, NT, 4, 128], BF16, "p3_va")
    p3_q = [[SB([96, 512], BF16, f"p3_q{i}_{h}") for h in range(4)] for i in range(3)]
    p3_p = [SB([128, 512], BF16, f"p3_p{i}") for i in range(3)]
    arena_state["off"] = max(arena_state["off"], 0)
    p3_y = SB([64, 4, 512], F32, "p3_y")
    p3_sq = SB([64, 4, 512], BF16, "p3_sq")
    p3_rs = SB([64, 512], F32, "p3_rs")
    p3_mix = SB([64, 4, 512], BF16, "p3_mix")
    p3_nw = SB([64, 4], F32, "p3_nw")

    def pass3(l, b):
        nonlocal p23_y, p23_sq, p23_rs, p23_mix, p23_nw
        p23_y, p23_sq, p23_rs, p23_mix, p23_nw = p3_y, p3_sq, p3_rs, p3_mix, p3_nw
        DMA(p23_nw[:], mon[l], (), ["p23_nw"])
        for h in range(4):
            MS("pool", p3_kT[h][:], 0.0, [f"p3_kT{h}_c"])
            DMA(p3_kT[h][64:64 + NBLK + 1, :], cd["kaug"], [], [f"p3_kT{h}_c"])
            for i in range(3):
                MS("pool", p3_q[i][h][:], 0.0, [f"p3_q{i}_{h}"])
                DMA(p3_q[i][h][64 + NBLK:64 + NBLK + 1, :], cd["alibi_row"][h:h + 1, :], [], [f"p3_q{i}_{h}"])

        for h in range(4):
            MS("pool", p3_va[:, :, h, 64:128], 1.0, ["p3_va_c"])

        def loads(g):
            t0 = g * 512
            for h in range(4):
                DMA(p3_kT[h][0:64, t0:t0 + 512], d_mok[b][h * 64:(h + 1) * 64, t0:t0 + 512], [f"d_mok{b}_{g}", f"p3_kT{h}_c"], [f"p3_kT{h}_{g}"])
            for h in range(4):
                DMA(p3_va[:, 4 * g:4 * g + 4, h, 0:64], d_mov[b][t0:t0 + 512, h * 64:(h + 1) * 64].rearrange("(t p) d -> p t d", p=128),
                    [f"d_mov{b}_{g}", "p3_va_c"], [f"p3_v_{g}"])
            i_ = g % 3
            for h in range(4):
                qk = f"p3_q{i_}_{h}"
                DMA(p3_q[i_][h][0:64, :], d_moq[b][h * 64:(h + 1) * 64, t0:t0 + 512], [f"d_moq{b}_{g}"], [qk])
                DMA(p3_q[i_][h][64:64 + NBLK, :], d_sel[b][h * NBLK:(h + 1) * NBLK, t0:t0 + 512], [f"d_sel{b}_{g}"], [qk])

        def it_gen(g, h, kb, n):
            i_ = g % 3
            qk = f"p3_q{i_}_{h}"
            qa = p3_q[i_][h]
            yb = 4 + (h % 2)
            db = 6 + (h % 2)
            gk = kb // 4
            qoff = max(0, kb - 4 * g) * 128
            W = 512 - qoff
            diag = kb >= 4 * g
            rel = 4 * g + 3 - kb
            i3 = n % 3
            b1 = n % 3
            lastk = kb == 4 * g + 3
            pp, pk = p3_p[i3], f"p3_p{i3}"
            if kb == 0:
                if h == 0 and g + 1 < NG:
                    loads(g + 1)
                MM(psb[yb][:, :], zeros_b[:, 0:128], ones_b[:, 0:512], True, False, ["C_zeros_b", "C_ones_b"], [f"ps{yb}"])
            MM(psb[b1][:, 0:W], p3_kT[h][:, kb * 128:(kb + 1) * 128], qa[:, qoff:512], True, True,
               [f"p3_kT{h}_{gk}", f"p3_kT{h}_c", qk], [f"ps{b1}"])
            yield
            ACT(pp[:, 0:W], psb[b1][:, 0:W], AF.Exp, [f"ps{b1}", "C_alibi_kb"], [pk], bias=alibi_kb[:, h, rel:rel + 1])
            if diag:
                TT("dve", pp[:, 0:128], pp[:, 0:128], tri_incl[:], ALU.mult, [pk, "C_tri_incl"], [pk])
            yield
            MM(psb[yb][:, qoff:512], p3_va[:, kb, h, :], pp[:, 0:W], False, lastk, [f"p3_v_{gk}", "p3_va_c", pk], [f"ps{yb}"])
            if lastk:
                CP("act", p23_y[:, h, :], psb[yb][0:64, :], [f"ps{yb}"], ["p23_y"])
                CP("act", p23_rs[:], psb[yb][64:128, :], [f"ps{yb}"], ["p23_rs"])
                P.add("dve", lambda e: e.reciprocal(out=p23_rs[:], in_=p23_rs[:]), ["p23_rs"], ["p23_rs"])
                TT("dve", p23_y[:, h, :], p23_y[:, h, :], p23_rs[:], ALU.mult, ["p23_y", "p23_rs"], ["p23_y"])
                if h == 3:
                    head_norm_store(l, b, g, d_mmo[b], nb=3)

        def all_iters():
            n = 0
            for g in range(NG):
                for h in range(4):
                    for kb in range(0, 4 * g + 4):
                        yield it_gen(g, h, kb, n)
                        n += 1

        loads(0)
        run_pipeline(all_iters())

    arena_state["off"] = base_off
    p45_tmp = [SB([128, D], F32, f"p45_tmp{i}") for i in range(2)]
    p45_xo = [SB([128, D], F32, f"p45_xo{i}") for i in range(2)]
    p45_ss = SB([128, 4], F32, "p45_ss")
    base45 = arena_state["off"]
    p4_woa = SB([128, 4, D], BF16, "p4_woa")
    p4_wob = SB([64, 8, D], BF16, "p4_wob")
    p4_ms = [SB([128, 4, 512], BF16, f"p4_ms{i}") for i in range(2)]
    p4_mh = [SB([64, 8, 512], BF16, f"p4_mh{i}") for i in range(2)]

    def norm_resid_store(pb, xtile, xk, i2, dst_ap, dkey):
        for nh in range(2):
            jb, jk = nextjunk()
            ACT(jb[:, 0:512], psb[pb + nh][:], AF.Square, [f"ps{pb + nh}"], [jk, f"p45_ss{nh}"], scale=1.0 / 32,
                accum_out=p45_ss[:, nh:nh + 1])
        TT("dve", p45_ss[:, 2:3], p45_ss[:, 0:1], p45_ss[:, 1:2], ALU.add, ["p45_ss0", "p45_ss1"], ["p45_ss2"])
        ACT(p45_ss[:, 3:4], p45_ss[:, 2:3], AF.Ln, ["p45_ss2"], ["p45_ss3"], bias=EPS)
        ACT(p45_ss[:, 3:4], p45_ss[:, 3:4], AF.Exp, ["p45_ss3"], ["p45_ss3"], scale=-0.5)
        tmp, tk = p45_tmp[i2], f"p45_tmp{i2}"
        for nh in range(2):
            STT("dve", tmp[:, nh * 512:(nh + 1) * 512], psb[pb + nh][:], p45_ss[:, 3:4], gain[1][:, nh * 512:(nh + 1) * 512],
                ALU.mult, ALU.mult, [f"ps{pb + nh}", "p45_ss3", "gain1"], [tk])
        xo, xok = p45_xo[i2], f"p45_xo{i2}"
        TT("pool", xo[:], tmp[:], xtile, ALU.add, [tk, xk], [xok])
        DMA(dst_ap, xo[:], [xok], [dkey])

    def pass4(l, b, xsrc, srckey):
        load_gain(1, l, 1)
        DMA(p4_woa[:], w_out[l, 0:512, :].rearrange("(c p) n -> p c n", p=128), (), ["p4_woa"], eng="pool")
        DMA(p4_wob[:], w_out[l, 512:1024, :].rearrange("(j p) n -> p j n", p=64), (), ["p4_wob"], eng="pool")
        n = 0
        for g in range(NG):
            t0 = g * 512
            ms, mh = p4_ms[g % 2], p4_mh[g % 2]
            msk, mhk = f"p4_ms{g % 2}", f"p4_mh{g % 2}"
            DMA(ms[:], d_ssd[b].rearrange("(c p) s -> p c s", p=128)[:, :, t0:t0 + 512], [f"d_ssd{b}_{g}"], [msk])
            DMA(mh[:, 0:4, :], d_msb[b].rearrange("h d s -> d h s")[:, :, t0:t0 + 512], [f"d_msb{b}_{g}"], [mhk])
            DMA(mh[:, 4:8, :], d_mmo[b].rearrange("h d s -> d h s")[:, :, t0:t0 + 512], [f"d_mmo{b}_{g}"], [mhk])
            for tt in range(4):
                tok = slice(t0 + tt * 128, t0 + (tt + 1) * 128)
                xi = n % 4
                DMA(xt[xi][:], xsrc[tok, :], [f"{srckey}_{(t0 + tt * 128) // 1024}"], [f"xt{xi}"])
                pb = 4 + 2 * (n % 2)
                tsl = slice(tt * 128, (tt + 1) * 128)
                for nh in range(2):
                    cs = slice(nh * 512, (nh + 1) * 512)
                    for c in range(4):
                        MM(psb[pb + nh][:], ms[:, c, tsl], p4_woa[:, c, cs], c == 0, False, [msk, "p4_woa"], [f"ps{pb + nh}"])
                    for j in range(8):
                        MM(psb[pb + nh][:], mh[:, j, tsl], p4_wob[:, j, cs], False, j == 7, [mhk, "p4_wob"], [f"ps{pb + nh}"])
                norm_resid_store(pb, xt[xi][:], f"xt{xi}", n % 2, d_x1[b][tok, :], f"d_x1{b}_{(t0 + tt * 128) // 1024}")
                n += 1

    arena_state["off"] = base45
    p5_hT = SB([128, 8, 1024], BF16, "p5_hT")
    p5_act = SB([128, NHC, 1024], BF16, "p5_act")
    p5_wd = SB([128, NHC, D], BF16, "p5_wd")
    p5_wg = [SB([128, 8, 256], BF16, f"p5_wg{i}") for i in range(2)]
    p5_wu = [SB([128, 8, 256], BF16, f"p5_wu{i}") for i in range(2)]
    p5_sg = [SB([128, 512], F32, f"p5_sg{i}") for i in range(2)]

    def pass5(l, b, xdst, dstkey):
        load_gain(0, l, 2)
        load_gain(1, l, 3)
        for (c0, c1) in [(0, 11), (11, 22)]:
            DMA(p5_wd[:, c0:c1, :], w_down[l, c0 * 128:c1 * 128, :].rearrange("(c p) n -> p c n", p=128), (), ["p5_wd"], eng="pool")
        n = 0
        wi = 0
        for G in range(S // 1024 if not ng_limit else max(1, NG // 2)):
            T0 = G * 1024
            def ld5(tt_):
                DMA(xt[(n0 + tt_) % 4][:], d_x1[b][T0 + tt_ * 128:T0 + (tt_ + 1) * 128, :], [f"d_x1{b}_{G}"], [f"xt{(n0 + tt_) % 4}"])

            n0 = n
            for tt_ in range(4):
                ld5(tt_)
            for tt in range(8):
                xi = n % 4
                n += 1
                jb, jk = nextjunk()
                ACT(jb[:], xt[xi][:], AF.Square, [f"xt{xi}"], [jk, "p5_ssa"], scale=1.0 / 32, accum_out=ss4[:, 0:1])
                ACT(rs4[:, 0:1], ss4[:, 0:1], AF.Ln, ["p5_ssa"], ["p5_rsa"], bias=EPS)
                ACT(rs4[:, 0:1], rs4[:, 0:1], AF.Exp, ["p5_rsa"], ["p5_rsa"], scale=-0.5)
                hbk = f"hb{tt % 2}"
                STT("dve", hb[tt % 2][:], xt[xi][:], rs4[:, 0:1], gain[0][:], ALU.mult, ALU.mult, [f"xt{xi}", "p5_rsa", "gain0"], [hbk])
                if tt + 4 < 8:
                    ld5(tt + 4)
                bk = 4 + (tt % 4)
                pT = psbf(bk)
                for c in range(8):
                    TR(pT[:, c * 128:(c + 1) * 128], hb[tt % 2][:, c * 128:(c + 1) * 128], ident_b[:], [hbk, "C_ident_b"], [f"ps{bk}"])
                CP("act" if tt % 2 else "dve", p5_hT[:, :, tt * 128:(tt + 1) * 128], pT.rearrange("p (c k) -> p c k", k=128),
                   [f"ps{bk}"], ["p5_hT"])
            it = 0
            for hq in range(0, NHC, 2):
                nq_ = min(2, NHC - hq)
                wg, wu = p5_wg[wi % 2], p5_wu[wi % 2]
                wgk, wuk = f"p5_wg{wi % 2}", f"p5_wu{wi % 2}"
                wi += 1
                cols = slice(hq * 128, (hq + nq_) * 128)
                DMA(wg[:, :, 0:nq_ * 128], w_gate[l, :, cols].rearrange("(c p) n -> p c n", p=128), (), [wgk], eng="pool")
                DMA(wu[:, :, 0:nq_ * 128], w_up[l, :, cols].rearrange("(c p) n -> p c n", p=128), (), [wuk], eng="pool")
                for hl in range(nq_):
                    hcx = hq + hl
                    for half in range(2):
                        i2 = it % 2
                        it += 1
                        ba, bb = 2 * i2, 2 * i2 + 1
                        hs = slice(half * 512, (half + 1) * 512)
                        for c in range(8):
                            MM(psb[ba][:], wg[:, c, hl * 128:(hl + 1) * 128], p5_hT[:, c, hs], c == 0, c == 7, [wgk, "p5_hT"], [f"ps{ba}"])
                        for c in range(8):
                            MM(psb[bb][:], wu[:, c, hl * 128:(hl + 1) * 128], p5_hT[:, c, hs], c == 0, c == 7, [wuk, "p5_hT"], [f"ps{bb}"])
                        ACT(p5_sg[i2][:], psb[ba][:], AF.Silu, [f"ps{ba}"], [f"p5_sg{i2}"])
                        TT("dve", p5_act[:, hcx, hs], p5_sg[i2][:], psb[bb][:], ALU.mult, [f"p5_sg{i2}", f"ps{bb}"], ["p5_act"])
            for tt in range(8):
                tok = slice(T0 + tt * 128, T0 + (tt + 1) * 128)
                xi = n % 4
                n += 1
                DMA(xt[xi][:], d_x1[b][tok, :], [f"d_x1{b}_{G}"], [f"xt{xi}"])
                pb = 4 + 2 * (tt % 2)
                tsl = slice(tt * 128, (tt + 1) * 128)
                for nh in range(2):
                    for hcx in range(NHC):
                        MM(psb[pb + nh][:], p5_act[:, hcx, tsl], p5_wd[:, hcx, nh * 512:(nh + 1) * 512], hcx == 0, hcx == NHC - 1,
                           ["p5_act", "p5_wd"], [f"ps{pb + nh}"])
                norm_resid_store(pb, xt[xi][:], f"xt{xi}", tt % 2, xdst[tok, :], f"{dstkey}_{G}")

    for l in range(DEPTH):
        for b in range(NSEQ):
            xsrc = x_in[b] if l == 0 else d_x2[b]
            srckey = f"xin{b}" if l == 0 else f"d_x2{b}"
            if 1 in passes:
                barrier()
                pass1(l, b, xsrc)
            if 2 in passes:
                barrier()
                pass2(l, b)
            if 3 in passes:
                barrier()
                pass3(l, b)
            if 4 in passes:
                barrier()
                pass4(l, b, xsrc, srckey)
            if 5 in passes:
                barrier()
                if l == DEPTH - 1:
                    pass5(l, b, out[b], f"out{b}")
                else:
                    pass5(l, b, d_x2[b], f"d_x2{b}")
    finals = list(P.dma_prev.values())
    P.emit(final_waits=finals)
    return nc, hc


def prep_inputs(inputs, DEPTH):
    f = lambda a: np.ascontiguousarray(np.asarray(a, dtype=np.float32))
    d = {}
    for k in ["w_in", "w_out", "w_gate", "w_up", "w_down"]:
        d[k] = f(inputs[k])
    d["vec1024"] = f(np.stack([inputs["pre_mix_norm"], inputs["post_mix_norm"], inputs["pre_ffn_norm"], inputs["post_ffn_norm"]], axis=1))
    cwv = np.asarray(inputs["conv_w"], np.float32)
    d["convw"] = f(cwv.reshape(DEPTH, 4, 8, 128).transpose(0, 3, 2, 1))
    d["convb"] = f(np.asarray(inputs["conv_b"], np.float32).reshape(DEPTH, 8, 128).transpose(0, 2, 1))
    d["hv"] = f(np.stack([inputs["dt_bias"], inputs["a_log"], inputs["d_skip"]], axis=1))
    d["ssdn"] = f(inputs["ssd_norm"])
    d["sbn"] = f(np.asarray(inputs["sb_norm"], np.float32).reshape(DEPTH, 4, 64).transpose(0, 2, 1))
    d["mon"] = f(np.asarray(inputs["moba_norm"], np.float32).reshape(DEPTH, 4, 64).transpose(0, 2, 1))
    return d


def kernel(**inputs):
    x = np.asarray(inputs["x"], np.float32)
    B, S, _ = x.shape
    DEPTH = inputs["w_in"].shape[0]
    NCORE = 8
    NSEQ = B // NCORE
    nc, hc = build(S, NSEQ, DEPTH)
    shared = prep_inputs(inputs, DEPTH)
    for k, v in hc.items():
        shared["c_" + k] = v
    in_maps = []
    for c in range(NCORE):
        m = dict(shared)
        m["x"] = np.ascontiguousarray(x[c * NSEQ:(c + 1) * NSEQ])
        in_maps.append(m)
    res = run_bass_kernel_spmd(nc, in_maps, core_ids=list(range(NCORE)))
    return np.concatenate([r["out"] for r in res.results], axis=0).astype(np.float32)
```
